# Optimizing a Trainium2 kernel written in Bass

```python
import math
import jax, jax.numpy as jnp
from jax import lax
import numpy as np

D_MODEL = 1024
BATCH = 16
SEQ = 2048
DEPTH = 2

N_ATTN_HEADS = 4
QK_NOPE_DIM = 128
QK_ROPE_DIM = 64
QK_HEAD_DIM = QK_NOPE_DIM + QK_ROPE_DIM
V_HEAD_DIM = 128
Q_RANK = 256
KV_RANK = 128
ATTN_WIDTH = N_ATTN_HEADS * V_HEAD_DIM
ROPE_THETA = 10000.0
Q_BLOCK = 128
CONV_CH = 512
CONV_WIDTH = 31
CONV_PAD = CONV_WIDTH // 2
D_MIX = ATTN_WIDTH + CONV_CH
IN_WIDTH = Q_RANK + KV_RANK + QK_ROPE_DIM + 2 * CONV_CH
N_EXPERTS = 16
CAPACITY_FACTOR = 2
EXPERT_FF = 1024
N_MOD = 6
EPS = 1e-6

kernel_name = "hybrid_mla_conformer_ecmoe_encoder"


def rms_norm(x, g):
    xf = x.astype(jnp.float32)
    y = xf * lax.rsqrt(jnp.mean(xf * xf, axis=-1, keepdims=True) + EPS)
    return (y * g.astype(jnp.float32)).astype(x.dtype)


def layer_norm(x, g, b):
    xf = x.astype(jnp.float32)
    mu = jnp.mean(xf, axis=-1, keepdims=True)
    var = jnp.mean(jnp.square(xf - mu), axis=-1, keepdims=True)
    y = (xf - mu) * lax.rsqrt(var + EPS)
    return (y * g.astype(jnp.float32) + b.astype(jnp.float32)).astype(x.dtype)


def rope_tables(positions):
    inv_freq = 1.0 / (ROPE_THETA ** (jnp.arange(0, QK_ROPE_DIM, 2, dtype=jnp.float32) / QK_ROPE_DIM))
    ang = positions.astype(jnp.float32)[..., None] * inv_freq
    return jnp.cos(ang), jnp.sin(ang)


def apply_rope(x, cos, sin):
    half = x.shape[-1] // 2
    x1 = x[..., :half].astype(jnp.float32)
    x2 = x[..., half:].astype(jnp.float32)
    c = cos[:, :, None, :]
    s = sin[:, :, None, :]
    return jnp.concatenate([x1 * c - x2 * s, x2 * c + x1 * s], axis=-1).astype(x.dtype)


def mla_group(c_q, c_kv, k_rope, cos, sin, q_latent_g, w_uq, kv_latent_g, w_ukv, q_head_g, k_head_g):
    B, S, _ = c_q.shape
    q = jnp.einsum('bsr,rk->bsk', rms_norm(c_q, q_latent_g), w_uq).reshape(B, S, N_ATTN_HEADS, QK_HEAD_DIM)
    kv = jnp.einsum('bsr,rk->bsk', rms_norm(c_kv, kv_latent_g), w_ukv).reshape(
        B, S, N_ATTN_HEADS, QK_NOPE_DIM + V_HEAD_DIM)
    k_nope, v = kv[..., :QK_NOPE_DIM], kv[..., QK_NOPE_DIM:]
    k_r = jnp.broadcast_to(k_rope[:, :, None, :], (B, S, N_ATTN_HEADS, QK_ROPE_DIM))
    k = jnp.concatenate([k_nope, k_r], axis=-1)
    q = rms_norm(q, q_head_g)
    k = rms_norm(k, k_head_g)
    q = jnp.concatenate([q[..., :QK_NOPE_DIM], apply_rope(q[..., QK_NOPE_DIM:], cos, sin)], axis=-1)
    k = jnp.concatenate([k[..., :QK_NOPE_DIM], apply_rope(k[..., QK_NOPE_DIM:], cos, sin)], axis=-1)
    scale = 1.0 / math.sqrt(QK_HEAD_DIM)
    n_blocks = S // Q_BLOCK
    qb = q.reshape(B, n_blocks, Q_BLOCK, N_ATTN_HEADS, QK_HEAD_DIM).transpose(1, 0, 3, 2, 4)
    kt = k.transpose(0, 2, 1, 3)
    vt = v.transpose(0, 2, 1, 3)

    def attend(q_blk):
        s = jnp.einsum('bhqd,bhkd->bhqk', q_blk, kt, preferred_element_type=jnp.float32) * scale
        p = jax.nn.softmax(s, axis=-1).astype(vt.dtype)
        return jnp.einsum('bhqk,bhkd->bhqd', p, vt)

    o = lax.map(attend, qb)
    return o.transpose(1, 0, 3, 2, 4).reshape(B, S, ATTN_WIDTH)


def conv_group(u, conv_w, conv_b, conv_norm_g, conv_norm_b):
    a, g = u[..., :CONV_CH], u[..., CONV_CH:]
    y = a * jax.nn.sigmoid(g)
    y = lax.conv_general_dilated(
        y, conv_w[:, None, :].astype(y.dtype), window_strides=(1,), padding=[(CONV_PAD, CONV_PAD)],
        dimension_numbers=('NWC', 'WIO', 'NWC'), feature_group_count=CONV_CH)
    y = y + conv_b
    return jax.nn.silu(layer_norm(y, conv_norm_g, conv_norm_b))


def expert_choice_ffn(h, w_router, w_gate, w_up, w_down):
    B, S, D = h.shape
    cap = CAPACITY_FACTOR * S // N_EXPERTS
    logits = jnp.einsum('bsd,de->bse', h, w_router).astype(jnp.float32)
    aff = jax.nn.softmax(logits, axis=-1)
    gate, idx = lax.top_k(aff.transpose(0, 2, 1), cap)
    xe = jax.vmap(lambda hb, ib: hb[ib])(h, idx)
    hid = jax.nn.silu(jnp.einsum('becd,edf->becf', xe, w_gate)) * jnp.einsum('becd,edf->becf', xe, w_up)
    ye = jnp.einsum('becf,efd->becd', hid, w_down) * gate[..., None].astype(h.dtype)
    return jax.vmap(lambda ib, yb: jnp.zeros((S, D), yb.dtype).at[ib.reshape(-1)].add(yb.reshape(-1, D)))(idx, ye)


def setup_inputs(seed: int = 0) -> dict:
    key = jax.random.key(seed)
    ks = jax.random.split(key, 24)
    f32 = jnp.float32
    nrm = lambda k, shape, s: jax.random.normal(k, shape, f32) * s
    gain = lambda k, shape: 1.0 + 0.05 * jax.random.normal(k, shape, f32)
    L = DEPTH
    offset = jax.random.randint(ks[2], (BATCH, 1), 0, 1024, dtype=jnp.int32)
    positions = offset + jnp.arange(SEQ, dtype=jnp.int32)[None, :]
    return {
        "x": nrm(ks[0], (BATCH, SEQ, D_MODEL), 1.0),
        "c": nrm(ks[1], (BATCH, D_MODEL), 1.0),
        "positions": positions,
        "norm1_g": gain(ks[3], (L, D_MODEL)),
        "w_ada": nrm(ks[4], (L, D_MODEL, N_MOD * D_MODEL), 0.5 * D_MODEL ** -0.5),
        "b_ada": nrm(ks[5], (L, N_MOD * D_MODEL), 0.01),
        "w_in": nrm(ks[6], (L, D_MODEL, IN_WIDTH), D_MODEL ** -0.5),
        "q_latent_g": gain(ks[7], (L, Q_RANK)),
        "w_uq": nrm(ks[8], (L, Q_RANK, N_ATTN_HEADS * QK_HEAD_DIM), Q_RANK ** -0.5),
        "kv_latent_g": gain(ks[9], (L, KV_RANK)),
        "w_ukv": nrm(ks[10], (L, KV_RANK, N_ATTN_HEADS * (QK_NOPE_DIM + V_HEAD_DIM)), KV_RANK ** -0.5),
        "q_head_g": gain(ks[11], (L, QK_HEAD_DIM)),
        "k_head_g": gain(ks[12], (L, QK_HEAD_DIM)),
        "conv_w": nrm(ks[13], (L, CONV_WIDTH, CONV_CH), CONV_WIDTH ** -0.5),
        "conv_b": nrm(ks[14], (L, CONV_CH), 0.01),
        "conv_norm_g": gain(ks[15], (L, CONV_CH)),
        "conv_norm_b": nrm(ks[16], (L, CONV_CH), 0.01),
        "w_out": nrm(ks[17], (L, D_MIX, D_MODEL), D_MIX ** -0.5),
        "norm2_g": gain(ks[18], (L, D_MODEL)),
        "w_router": nrm(ks[19], (L, D_MODEL, N_EXPERTS), D_MODEL ** -0.5),
        "w_gate": nrm(ks[20], (L, N_EXPERTS, D_MODEL, EXPERT_FF), D_MODEL ** -0.5),
        "w_up": nrm(ks[21], (L, N_EXPERTS, D_MODEL, EXPERT_FF), D_MODEL ** -0.5),
        "w_down": nrm(ks[22], (L, N_EXPERTS, EXPERT_FF, D_MODEL), EXPERT_FF ** -0.5),
    }


def reference(x, c, positions, norm1_g, w_ada, b_ada, w_in, q_latent_g, w_uq, kv_latent_g, w_ukv,
              q_head_g, k_head_g, conv_w, conv_b, conv_norm_g, conv_norm_b, w_out, norm2_g,
              w_router, w_gate, w_up, w_down):
    cos, sin = rope_tables(positions)
    c_act = jax.nn.silu(c)
    o1 = Q_RANK
    o2 = o1 + KV_RANK
    o3 = o2 + QK_ROPE_DIM
    for l in range(DEPTH):
        mod = jnp.einsum('bd,dk->bk', c_act, w_ada[l]) + b_ada[l]
        shift1, scale1, gate1, shift2, scale2, gate2 = [m[:, None, :] for m in jnp.split(mod, N_MOD, axis=-1)]
        h = rms_norm(x, norm1_g[l]) * (1.0 + scale1) + shift1
        proj = jnp.einsum('bsd,dk->bsk', h, w_in[l])
        c_q, c_kv = proj[..., :o1], proj[..., o1:o2]
        k_rope = proj[..., o2:o3]
        conv_u = proj[..., o3:]
        attn_out = mla_group(c_q, c_kv, k_rope, cos, sin, q_latent_g[l], w_uq[l], kv_latent_g[l], w_ukv[l],
                             q_head_g[l], k_head_g[l])
        conv_out = conv_group(conv_u, conv_w[l], conv_b[l], conv_norm_g[l], conv_norm_b[l])
        mix = jnp.einsum('bsk,kd->bsd', jnp.concatenate([attn_out, conv_out], axis=-1), w_out[l])
        x = x + gate1 * mix
        h2 = rms_norm(x, norm2_g[l]) * (1.0 + scale2) + shift2
        x = x + gate2 * expert_choice_ffn(h2, w_router[l], w_gate[l], w_up[l], w_down[l])
    return x
```

```python
import contextlib
import math
import numpy as np
import concourse.bass as bass
import concourse.mybir as mybir
from concourse.bass_utils import run_bass_kernel_spmd

F32 = mybir.dt.float32
BF16 = mybir.dt.bfloat16
I32 = mybir.dt.int32
ALU = mybir.AluOpType
AF = mybir.ActivationFunctionType

PE, ACT, DVE, POOL, SP = "pe", "act", "dve", "pool", "sp"
ENGS = [PE, ACT, DVE, POOL, SP]

S_LEN = 2048
D = 1024
NH = 4
NE = 16
CAP = 256
EPS = 1e-6
NV = 161
NCST = 520 + 256


class _Op:
    __slots__ = ("eng", "idx", "fn", "waits", "sig", "sigval", "dma", "clock", "dclock")


class DmaSlot:
    def __init__(self, name):
        self.name = name
        self.count = 0
        self.sem = None
        self.last_op = None


class _Rec:
    def __getattr__(self, name):
        return lambda *a, **k: (name, a, k)


_REC = _Rec()


class Tracker:
    def __init__(self):
        self.ops = {e: [] for e in ENGS}
        self.state = {}
        self.clock = {e: {} for e in ENGS}
        self.dclock = {e: {} for e in ENGS}
        self.pending = {e: [] for e in ENGS}
        self.slots = []

    def slot(self, name):
        s = DmaSlot(name)
        self.slots.append(s)
        return s

    def _collect(self, key, is_write, deps):
        buf, sub = key if isinstance(key, tuple) else (key, None)
        st = self.state.setdefault(buf, {})
        if sub is None:
            ents = list(st.values())
        else:
            ents = [st[k] for k in (sub, None) if k in st]
        for ent in ents:
            if ent[0] is not None:
                deps.append(ent[0])
            if is_write:
                deps.extend(ent[1].values())
                deps.extend(ent[2])

    def _update(self, key, is_write, tok):
        buf, sub = key if isinstance(key, tuple) else (key, None)
        st = self.state.setdefault(buf, {})
        if is_write:
            if sub is None:
                st.clear()
                st[None] = [tok, {}, []]
            else:
                st[sub] = [tok, {}, []]
        else:
            ent = st.setdefault(sub, [None, {}, []])
            if tok[0] == "op":
                ent[1][tok[1]] = tok
            else:
                ent[2].append(tok)
                if len(ent[2]) > 8:
                    ent[2] = ent[2][-8:]

    def op(self, eng, fn, reads=(), writes=(), dma=None):
        o = _Op()
        o.eng = eng
        o.idx = len(self.ops[eng])
        o.fn = fn(_REC)
        o.sig = False
        o.sigval = None
        o.dma = None
        deps = list(self.pending[eng])
        self.pending[eng] = []
        for k in reads:
            self._collect(k, False, deps)
        for k in writes:
            self._collect(k, True, deps)
        clock = self.clock[eng]
        dclock = self.dclock[eng]
        waits = []
        for d in deps:
            if d[0] == "op":
                _, e2, i2 = d
                if e2 == eng:
                    if eng in (PE, SP):
                        continue
                    if i2 < o.idx - 3:
                        continue
                    if clock.get(e2, -1) >= i2:
                        continue
                    src = self.ops[e2][i2]
                    src.sig = True
                    waits.append(("op", src))
                    clock[e2] = i2
                    continue
                if clock.get(e2, -1) >= i2:
                    continue
                src = self.ops[e2][i2]
                src.sig = True
                waits.append(("op", src))
                clock[e2] = i2
                for k, v in src.clock.items():
                    if k != eng and clock.get(k, -1) < v:
                        clock[k] = v
                for k, v in src.dclock.items():
                    if dclock.get(k, -1) < v:
                        dclock[k] = v
            else:
                _, slot, val, src = d
                val = slot.count
                src = slot.last_op
                if dclock.get(slot, -1) >= val:
                    continue
                waits.append(("dma", slot, val))
                dclock[slot] = val
                for k, v in src.clock.items():
                    if k != eng and clock.get(k, -1) < v:
                        clock[k] = v
                for k, v in src.dclock.items():
                    if dclock.get(k, -1) < v:
                        dclock[k] = v
        o.waits = waits
        o.clock = dict(clock)
        o.dclock = dict(dclock)
        if dma is not None:
            dma.count += 16
            o.dma = (dma, dma.count)
            dma.last_op = o
            tok = ("dma", dma, dma.count, o)
        else:
            tok = ("op", eng, o.idx)
        self.ops[eng].append(o)
        for k in reads:
            self._update(k, False, tok)
        for k in writes:
            self._update(k, True, tok)
        return o

    def barrier(self):
        toks = []
        for e in (PE, ACT, DVE, POOL):
            for o in reversed(self.ops[e]):
                if o.dma is None:
                    toks.append(("op", e, o.idx))
                    break
        for s in self.slots:
            if s.last_op is not None:
                toks.append(("dma", s, s.count, s.last_op))
        for e in ENGS:
            self.pending[e] = list(toks)

    def emit(self, nc):
        stack = contextlib.ExitStack()
        with stack:
            esem = {}
            for e in (PE, ACT, DVE, POOL):
                esem[e] = stack.enter_context(nc.semaphore("s_" + e))
            for i, s in enumerate(self.slots):
                if s.count > 0:
                    s.sem = stack.enter_context(nc.semaphore("d%d_%s" % (i, s.name)))
            for e in (PE, ACT, DVE, POOL):
                c = 0
                for o in self.ops[e]:
                    if o.sig:
                        c += 1
                        o.sigval = c
            block = stack.enter_context(nc.Block())

            def run(engobj, e):
                bcreg = None
                if e == POOL and any(o.fn[2].get("bounds_check") == "BCREG" for o in self.ops[e]):
                    bcreg = engobj.alloc_register("bcreg")
                    engobj.reg_mov(bcreg, S_LEN - 1)
                for o in self.ops[e]:
                    for w in o.waits:
                        if w[0] == "op":
                            engobj.wait_ge(esem[w[1].eng], w[1].sigval)
                        else:
                            engobj.wait_ge(w[1].sem, w[2])
                    name, a_, k_ = o.fn
                    if k_.get("bounds_check") == "BCREG":
                        k_ = dict(k_)
                        k_["bounds_check"] = bcreg
                    ins = getattr(engobj, name)(*a_, **k_)
                    if o.dma is not None:
                        ins.then_inc(o.dma[0].sem, 16)
                    elif o.sig:
                        ins.then_inc(esem[e], 1)

            @block.tensor
            def _(t):
                run(t, PE)

            @block.scalar
            def _(a):
                run(a, ACT)

            @block.vector
            def _(v):
                run(v, DVE)

            @block.gpsimd
            def _(g):
                run(g, POOL)

            @block.sync
            def _(s):
                run(s, SP)
                for sl in self.slots:
                    if sl.count > 0:
                        s.wait_ge(sl.sem, sl.count)


LAY = []


class _Stop(Exception):
    pass


def build_program(n_seq=2, n_layers=2, stop=None):
    import inspect
    del LAY[:]
    nc = bass.Bass("TRN2", target_bir_lowering=False)
    L = n_layers
    xT_d = nc.dram_tensor("xT", [n_seq, D, S_LEN], F32, kind="ExternalInput").ap()
    out_d = nc.dram_tensor("outT", [n_seq, D, S_LEN], F32, kind="ExternalOutput").ap()
    cT_d = nc.dram_tensor("cT", [128, 8 * n_seq], F32, kind="ExternalInput").ap()
    pos_d = nc.dram_tensor("posr", [n_seq, 64, S_LEN], I32, kind="ExternalInput").ap()
    cst_d = nc.dram_tensor("cst", [128, NCST], F32, kind="ExternalInput").ap()
    vec_d = nc.dram_tensor("vec", [L, 128, NV], F32, kind="ExternalInput").ap()
    bada_d = nc.dram_tensor("bada", [L, 128, 48], F32, kind="ExternalInput").ap()
    wada_d = nc.dram_tensor("w_ada", [L, D, 6 * D], F32, kind="ExternalInput").ap()
    win_d = nc.dram_tensor("w_in", [L, D, 1472], F32, kind="ExternalInput").ap()
    wuq_d = nc.dram_tensor("w_uq", [L, 256, 768], F32, kind="ExternalInput").ap()
    wukv_d = nc.dram_tensor("w_ukv", [L, 128, 1024], F32, kind="ExternalInput").ap()
    wout_d = nc.dram_tensor("w_out", [L, D, D], F32, kind="ExternalInput").ap()
    wr_d = nc.dram_tensor("w_router", [L, 128, 8 * NE], F32, kind="ExternalInput").ap()
    wg_d = nc.dram_tensor("w_gate", [L, NE, D, D], F32, kind="ExternalInput").ap()
    wu_d = nc.dram_tensor("w_up", [L, NE, D, D], F32, kind="ExternalInput").ap()
    wd_d = nc.dram_tensor("w_down", [L, NE, D, D], F32, kind="ExternalInput").ap()

    h2_dram = [nc.dram_tensor("h2s%d" % i, [S_LEN, D], BF16, kind="Internal").ap() for i in range(n_seq)]
    acc_dram = [[nc.dram_tensor("accs%d_%d" % (i, ct), [S_LEN, D], F32, kind="Internal").ap() for ct in range(2)] for i in range(n_seq)]
    xs_dram = nc.dram_tensor("xss", [n_seq, D, S_LEN], F32, kind="Internal").ap()
    tr = Tracker()
    TOTW = 53000
    dump_d = nc.dram_tensor("dump", [128, TOTW], F32, kind="ExternalOutput").ap() if stop is not None else None
    stack = contextlib.ExitStack()
    with stack:
        arena = stack.enter_context(nc.sbuf_tensor("arena", [128, TOTW], F32))
        ps = [stack.enter_context(nc.psum_tensor("ps%d" % i, [128, 512], F32)) for i in range(8)]

        def view(off, shape, dt):
            n = int(np.prod(shape[1:]))
            nb = n * (2 if dt == BF16 else 4)
            nw = (nb + 3) // 4
            assert off + nw <= TOTW, (off, nw)
            if stop is not None:
                LAY.append((inspect.stack()[2].lineno, off, tuple(shape), "bf16" if dt == BF16 else ("i32" if dt == I32 else "f32")))
            v = arena[:, off:off + nw]
            if dt != F32:
                v = v.bitcast(dt)
            if len(shape) == 3:
                v = v.rearrange("p (a b) -> p a b", a=shape[1])
            elif len(shape) == 4:
                v = v.rearrange("p (a b c) -> p a b c", a=shape[1], b=shape[2])
            return v, off + nw

        class Region:
            def __init__(self, start):
                self.off = start

            def a(self, shape, dt):
                v, self.off = view(self.off, shape, dt)
                return v

        def chk(name):
            if stop == name:
                tr.barrier()
                tr.op(SP, lambda e: e.dma_start(out=dump_d, in_=arena[:, :]), dma=s_out)
                raise _Stop()

        V = lambda fn, r=(), w=(): tr.op(DVE, fn, r, w)
        A = lambda fn, r=(), w=(): tr.op(ACT, fn, r, w)
        G = lambda fn, r=(), w=(): tr.op(POOL, fn, r, w)
        T = lambda fn, r=(), w=(): tr.op(PE, fn, r, w)
        bank_ctr = [0]

        def nb(pool=(0, 1, 2, 3, 4, 5, 6, 7)):
            b = pool[bank_ctr[0] % len(pool)]
            bank_ctr[0] += 1
            return b

        def pk(b):
            return "ps%d" % b

        P = Region(0)
        xT = P.a([128, 8, S_LEN], F32)
        identb = P.a([128, 128], BF16)
        onesb = P.a([128, 128], BF16)
        BIG = P.a([128, 1024], BF16)
        jrow = P.a([128, 256], F32)
        identf = P.a([128, 128], F32)
        iota1 = P.a([128, 256], F32)
        ccols = P.a([128, 8], F32)
        onesf = P.a([128, 16], F32)
        modc = P.a([128, L * 48 * n_seq], F32)
        mcs = [P.a([128, 48], F32) for _ in range(n_seq)]
        pos1tm_s = [P.a([128, 256], F32) for _ in range(n_seq)]
        G4_s = [P.a([128, 256, 4], BF16) for _ in range(n_seq)]
        vecs = P.a([128, NV], F32)
        cact = P.a([128, 8 * n_seq], F32)
        bada = P.a([128, L * 48], F32)
        PH0 = P.off
        epsc = ccols[:, 4:5]

        s_x = tr.slot("x")
        s_out = tr.slot("out")
        s_misc = tr.slot("misc")
        s_ada = [tr.slot("ada0"), tr.slot("ada1")]
        s_w = [tr.slot("w%d" % i) for i in range(12)]
        s_tw = [tr.slot("tw%d" % i) for i in range(4)]
        s_g = [tr.slot("g0"), tr.slot("g1")]
        s_acc = [[tr.slot("acc%d_%d" % (i, ct)) for ct in range(2)] for i in range(n_seq)]
        s_zero = tr.slot("zero")
        s_sp = tr.slot("sp")
        s_xc = [tr.slot("xc0"), tr.slot("xc1")]
        s_h2 = tr.slot("h2")
        s_ab = [tr.slot("ab%d" % i) for i in range(4)]
        s_ab2 = [[tr.slot("ab%d_%d" % (i, ct)) for ct in range(2)] for i in range(4)]

        def load_x(l, s):
            xsrc = xT_d[s] if l == 0 else xs_dram[s]
            for c in range(8):
                tr.op(SP, lambda e, c=c, xsrc=xsrc: e.dma_start(out=xT[:, c, :], in_=xsrc[c * 128:(c + 1) * 128, :]),
                      reads=["xs%d" % s], writes=[("xT", (c, t)) for t in range(4)], dma=s_x)

        load_x(0, 0)

        R = Region(PH0)
        cstf = R.a([128, NCST], F32)
        slab = [R.a([128, 8, 512], F32) for _ in range(2)]
        tr.op(SP, lambda e: e.dma_start(out=cstf, in_=cst_d), writes=["cstf"], dma=s_misc)
        tr.op(SP, lambda e: e.dma_start(out=cact, in_=cT_d), writes=["cact"], dma=s_misc)
        for l in range(L):
            tr.op(SP, lambda e, l=l: e.dma_start(out=bada[:, l * 48:(l + 1) * 48], in_=bada_d[l]), writes=["bada"], dma=s_misc)
        V(lambda e: e.tensor_copy(identf, cstf[:, 0:128]), ["cstf"], ["identf"])
        V(lambda e: e.tensor_copy(identb, cstf[:, 0:128]), ["cstf"], ["identb"])
        V(lambda e: e.memset(onesb, 1.0), [], ["onesb"])
        V(lambda e: e.memset(BIG[:, 0:384], 0.0), [], ["BIG"])
        V(lambda e: e.tensor_copy(BIG[:, 384:512], cstf[:, 128:256]), ["cstf"], ["BIG"])
        V(lambda e: e.memset(BIG[:, 512:1024], 1.0), [], ["BIG"])
        V(lambda e: e.tensor_copy(jrow, cstf[:, 520:776]), ["cstf"], ["jrow"])
        V(lambda e: e.tensor_copy(iota1, cstf[:, 256:512]), ["cstf"], ["iota1"])
        V(lambda e: e.tensor_copy(ccols, cstf[:, 512:520]), ["cstf"], ["ccols"])
        V(lambda e: e.memset(onesf, 1.0), [], ["onesf"])
        A(lambda e: e.activation(cact, cact, AF.Silu), ["cact"], ["cact"])
        def emit_mod(l, slab, sbs=range(12)):
            wv = wada_d[l].rearrange("(c p) n -> p c n", p=128)
            for sb in sbs:
                bi = sb % 2
                tr.op(SP, lambda e, bi=bi, wv=wv, sb=sb: e.dma_start(out=slab[bi], in_=wv[:, :, sb * 512:(sb + 1) * 512]),
                      writes=["slab%d" % bi], dma=s_ada[bi])
                b = nb()
                for j in range(4):
                    for kc in range(8):
                        T(lambda e, b=b, bi=bi, j=j, kc=kc: e.matmul(
                            ps[b][:, j * n_seq:(j + 1) * n_seq], slab[bi][:, kc, j * 128:(j + 1) * 128],
                            cact[:, kc * n_seq:(kc + 1) * n_seq], start=(kc == 0), stop=(kc == 7)),
                          ["slab%d" % bi, "cact"], [pk(b)])
                for s in range(n_seq):
                    base = l * 48 * n_seq
                    mview = modc[:, base:base + 48 * n_seq].rearrange("p (j s) -> p j s", s=n_seq)
                    pview = ps[b][:, 0:4 * n_seq].rearrange("p (j s) -> p j s", s=n_seq)
                    V(lambda e, mview=mview, pview=pview, s=s, sb=sb, l=l: e.tensor_tensor(
                        mview[:, sb * 4:(sb + 1) * 4, s], pview[:, :, s], bada[:, l * 48 + sb * 4:l * 48 + (sb + 1) * 4], ALU.add),
                      [pk(b), "bada"], ["modc"])

        emit_mod(0, slab)
        tr.barrier()

        def rms_a(tc, sq_b):
            xs = xT[:, :, tc * 512:(tc + 1) * 512]
            xk = [("xT", (c, tc)) for c in range(8)]
            A(lambda e: e.activation(sq_b, xs, AF.Square), xk, ["sq_b"])

        def rms_chunk(tc, acols, shcols, sq_b, hT_b, xn, rstd, tag, hkey="hT", do_a=True):
            if do_a:
                rms_a(tc, sq_b)
            b = nb()
            for c in range(8):
                T(lambda e, c=c, b=b: e.matmul(ps[b][:, :], onesb, sq_b[:, c, :], start=(c == 0), stop=(c == 7)),
                  ["sq_b", "onesb"], [pk(b)])
            A(lambda e, b=b: e.activation(rstd, ps[b][:, :], AF.Sqrt, bias=epsc, scale=1.0 / D), [pk(b), "ccols"], [tag + "rstd"])
            V(lambda e: e.reciprocal(rstd, rstd), [tag + "rstd"], [tag + "rstd"])
            for c in range(8):
                xi = xn[c % 2]
                V(lambda e, c=c, xi=xi: e.scalar_tensor_tensor(xi, xT[:, c, tc * 512:(tc + 1) * 512], acols[:, c:c + 1], rstd, ALU.mult, ALU.mult),
                  [("xT", (c, tc)), "mc", tag + "rstd"], ["xn%d" % (c % 2)])
                A(lambda e, c=c, xi=xi: e.activation(hT_b[:, c, :], xi, AF.Identity, bias=shcols[:, c:c + 1], scale=1.0),
                  ["xn%d" % (c % 2), "mc"], [(hkey, c)])

        def rstd_from(bank, out, width, tag, npart=128):
            A(lambda e: e.activation(out[0:npart], ps[bank][0:npart, :], AF.Sqrt, bias=epsc[0:npart], scale=1.0 / width), [pk(bank), "ccols"], [tag])
            V(lambda e: e.reciprocal(out[0:npart], out[0:npart]), [tag], [tag])

        RA = Region(PH0)
        cqn_b = RA.a([128, 2, S_LEN], BF16)
        catC = RA.a([128, 4, S_LEN], BF16)
        QT0 = RA.off
        ckvn_b = RA.a([128, S_LEN], BF16)
        kr_f = RA.a([128, S_LEN], F32)
        krs_f = RA.a([128, S_LEN], F32)
        YP0 = RA.off
        ypad = RA.a([128, 4, 2080], BF16)
        PB0 = RA.off
        RY = Region(YP0)
        cos2 = RY.a([128, S_LEN], F32)
        sin2 = RY.a([128, S_LEN], F32)
        assert RY.off <= PB0

        def main_body():
            chk("setup")
            for l in range(L):
                tr.barrier()
                tr.op(SP, lambda e, l=l: e.dma_start(out=vecs, in_=vec_d[l]), writes=["vecs"], dma=s_misc)
                for s in range(n_seq):
                    tr.barrier()
                    if s == 0 and l > 0:
                        load_x(l, 0)
                    mc = mcs[s]
                    pos1tm = pos1tm_s[s]
                    G4 = G4_s[s]
                    mbase = l * 48 * n_seq
                    mv = modc[:, mbase:mbase + 48 * n_seq].rearrange("p (j s) -> p j s", s=n_seq)
                    V(lambda e, mv=mv, s=s: e.scalar_tensor_tensor(mc[:, 0:8], mv[:, 8:16, s], 1.0, vecs[:, 0:8], ALU.add, ALU.mult), ["modc", "vecs"], ["mc"])
                    V(lambda e, mv=mv, s=s: e.tensor_copy(mc[:, 8:16], mv[:, 0:8, s]), ["modc"], ["mc"])
                    V(lambda e, mv=mv, s=s: e.tensor_copy(mc[:, 16:24], mv[:, 16:24, s]), ["modc"], ["mc"])
                    V(lambda e, mv=mv, s=s: e.scalar_tensor_tensor(mc[:, 24:32], mv[:, 32:40, s], 1.0, vecs[:, 8:16], ALU.add, ALU.mult), ["modc", "vecs"], ["mc"])
                    V(lambda e, mv=mv, s=s: e.tensor_copy(mc[:, 32:40], mv[:, 24:32, s]), ["modc"], ["mc"])
                    V(lambda e, mv=mv, s=s: e.tensor_copy(mc[:, 40:48], mv[:, 40:48, s]), ["modc"], ["mc"])

                    R = Region(PB0)
                    w_in_b = R.a([128, 8, 1472], BF16)
                    wkrs_b = R.a([128, 8, 64], BF16)
                    sq_b = R.a([128, 8, 512], BF16)
                    hTs = [R.a([128, 8, 512], BF16) for _ in range(2)]
                    xn = [R.a([128, 512], F32) for _ in range(2)]
                    rstd = R.a([128, 512], F32)
                    sig = [R.a([128, 512], F32) for _ in range(2)]
                    cq_f = R.a([128, 2, 512], F32)
                    sqc = R.a([128, 3, 512], BF16)
                    ckv_f = R.a([128, 512], F32)
                    rs2_ = R.a([128, 512], F32)
                    rs2 = [rs2_, rs2_]
                    wv = win_d[l].rearrange("(c p) n -> p c n", p=128)
                    for q in range(4):
                        tr.op(POOL, lambda e, q=q, wv=wv: e.dma_start(out=w_in_b[:, 2 * q:2 * q + 2, :], in_=wv[:, 2 * q:2 * q + 2, :]),
                              writes=[("w_in", q)], dma=s_tw[q])
                    G(lambda e: e.tensor_copy(wkrs_b[:, :, 0:32], w_in_b[:, :, 416:448]), ["w_in"], ["wkrs"])
                    G(lambda e: e.tensor_copy(wkrs_b[:, :, 32:64], w_in_b[:, :, 384:416]), ["w_in"], ["wkrs"])
                    G(lambda e: e.memset(ypad[:, :, 0:16], 0.0), [], ["ypad"])
                    G(lambda e: e.memset(ypad[:, :, 2064:2080], 0.0), [], ["ypad"])
                    rms_chunk(0, mc[:, 0:8], mc[:, 8:16], sq_b, hTs[0], xn, rstd, "t1", hkey="hT0")
                    for tc in range(4):
                        tsl = slice(tc * 512, (tc + 1) * 512)
                        hT_b = hTs[tc % 2]
                        hk = "hT%d" % (tc % 2)
                        if tc + 1 < 4:
                            rms_a(tc + 1, sq_b)

                        def proj(b, lhs_fn, mrows=128):
                            for c in range(8):
                                T(lambda e, c=c: e.matmul(ps[b][0:mrows, :], lhs_fn(c), hT_b[:, c, :], start=(c == 0), stop=(c == 7)),
                                  [(hk, c), "w_in", "wkrs"], [pk(b)])
                        for i in range(3):
                            b = nb()
                            proj(b, lambda c, i=i: w_in_b[:, c, i * 128:(i + 1) * 128])
                            dst = cq_f[:, i, :] if i < 2 else ckv_f
                            A(lambda e, b=b, dst=dst: e.activation(dst, ps[b][:, :], AF.Copy), [pk(b)], [("cqf", i)])
                            A(lambda e, b=b, i=i: e.activation(sqc[:, i, :], ps[b][:, :], AF.Square), [pk(b)], [("sqc", i)])
                        b = nb()
                        for i in range(2):
                            T(lambda e, i=i, b=b: e.matmul(ps[b][:, :], onesb, sqc[:, i, :], start=(i == 0), stop=(i == 1)), [("sqc", i)], [pk(b)])
                        rstd_from(b, rs2[0], 256.0, "rs2")
                        for i in range(2):
                            V(lambda e, i=i: e.scalar_tensor_tensor(cqn_b[:, i, tsl], cq_f[:, i, :], vecs[:, 16 + i:17 + i], rs2[0], ALU.mult, ALU.mult),
                              [("cqf", i), "vecs", "rs2"], [("cqn", tc)])
                        b = nb()
                        T(lambda e, b=b: e.matmul(ps[b][:, :], onesb, sqc[:, 2, :], start=True, stop=True), [("sqc", 2)], [pk(b)])
                        rstd_from(b, rs2[1], 128.0, "rs2")
                        V(lambda e: e.scalar_tensor_tensor(ckvn_b[:, tsl], ckv_f, vecs[:, 18:19], rs2[1], ALU.mult, ALU.mult),
                          [("cqf", 2), "vecs", "rs2"], [("ckvn", tc)])
                        if tc + 1 < 4:
                            rms_chunk(tc + 1, mc[:, 0:8], mc[:, 8:16], sq_b, hTs[(tc + 1) % 2], xn, rstd, "t1", hkey="hT%d" % ((tc + 1) % 2), do_a=False)
                        b = nb()
                        proj(b, lambda c: w_in_b[:, c, 384:448], 64)
                        A(lambda e, b=b: e.activation(kr_f[0:64, tsl], ps[b][0:64, :], AF.Copy), [pk(b)], [("krf", tc)])
                        b = nb()
                        proj(b, lambda c: wkrs_b[:, c, :], 64)
                        A(lambda e, b=b: e.activation(krs_f[0:64, tsl], ps[b][0:64, :], AF.Copy), [pk(b)], [("krsf", tc)])
                        for cc in range(4):
                            bg = nb()
                            proj(bg, lambda c, cc=cc: w_in_b[:, c, 960 + cc * 128:960 + (cc + 1) * 128])
                            ba = nb()
                            proj(ba, lambda c, cc=cc: w_in_b[:, c, 448 + cc * 128:448 + (cc + 1) * 128])
                            sg = sig[cc % 2]
                            A(lambda e, bg=bg, sg=sg: e.activation(sg, ps[bg][:, :], AF.Sigmoid), [pk(bg)], ["sig%d" % (cc % 2)])
                            V(lambda e, ba=ba, sg=sg, cc=cc: e.tensor_tensor(ypad[:, cc, 16 + tc * 512:16 + (tc + 1) * 512], ps[ba][:, :], sg, ALU.mult),
                              [pk(ba), "sig%d" % (cc % 2)], [("ypad", (cc, tc))])

                    chk("T1")
                    tr.barrier()
                    R = Region(PB0)
                    dg = R.a([128, 4, 31, 128], BF16)
                    yc = R.a([128, 4, 512], F32)
                    ycb = R.a([128, 4, 512], BF16)
                    sqy = R.a([128, 4, 512], BF16)
                    mean = R.a([128, 512], F32)
                    var = R.a([128, 512], F32)
                    msq = R.a([128, 512], F32)
                    tt = [R.a([128, 512], F32) for _ in range(2)]
                    for cc in range(4):
                        for j in range(31):
                            if j % 2 == 0:
                                V(lambda e, cc=cc, j=j: e.tensor_scalar(dg[:, cc, j, :], identf, vecs[:, 37 + cc * 31 + j:38 + cc * 31 + j], None, ALU.mult),
                                  ["identf", "vecs"], [("dg", cc)])
                            else:
                                A(lambda e, cc=cc, j=j: e.activation(dg[:, cc, j, :], identf, AF.Copy, scale=vecs[:, 37 + cc * 31 + j:38 + cc * 31 + j]),
                                  ["identf", "vecs"], [("dg", cc)])
                    for tc in range(4):
                        tsl = slice(tc * 512, (tc + 1) * 512)
                        for cc in range(4):
                            b = nb()
                            for j in range(31):
                                T(lambda e, b=b, cc=cc, j=j: e.matmul(ps[b][:, :], dg[:, cc, j, :], ypad[:, cc, tc * 512 + j + 1:tc * 512 + j + 513],
                                                                      start=(j == 0), stop=(j == 30)),
                                  [("dg", cc), "ypad"], [pk(b)])
                            A(lambda e, b=b, cc=cc: e.activation(yc[:, cc, :], ps[b][:, :], AF.Identity, bias=vecs[:, 25 + cc:26 + cc], scale=1.0),
                              [pk(b), "vecs"], [("yc", cc)])
                            A(lambda e, b=b, cc=cc: e.activation(sqy[:, cc, :], ps[b][:, :], AF.Square, bias=vecs[:, 25 + cc:26 + cc], scale=1.0),
                              [pk(b), "vecs"], [("sqy", cc)])
                            V(lambda e, cc=cc: e.tensor_copy(ycb[:, cc, :], yc[:, cc, :]), [("yc", cc)], [("ycb", cc)])
                        b1 = nb()
                        for cc in range(4):
                            T(lambda e, cc=cc, b1=b1: e.matmul(ps[b1][:, :], onesb, ycb[:, cc, :], start=(cc == 0), stop=(cc == 3)), [("ycb", cc)], [pk(b1)])
                        b2 = nb()
                        for cc in range(4):
                            T(lambda e, cc=cc, b2=b2: e.matmul(ps[b2][:, :], onesb, sqy[:, cc, :], start=(cc == 0), stop=(cc == 3)), [("sqy", cc)], [pk(b2)])
                        A(lambda e, b1=b1: e.activation(mean, ps[b1][:, :], AF.Copy, scale=1.0 / 512), [pk(b1)], ["mean"])
                        V(lambda e: e.tensor_tensor(msq, mean, mean, ALU.mult), ["mean"], ["msq"])
                        V(lambda e, b2=b2: e.scalar_tensor_tensor(var, ps[b2][:, :], 1.0 / 512, msq, ALU.mult, ALU.subtract), [pk(b2), "msq"], ["var"])
                        A(lambda e: e.activation(var, var, AF.Sqrt, bias=epsc, scale=1.0), ["var", "ccols"], ["var"])
                        V(lambda e: e.reciprocal(var, var), ["var"], ["var"])
                        for cc in range(4):
                            t = tt[cc % 2]
                            V(lambda e, cc=cc, t=t: e.tensor_tensor(t, yc[:, cc, :], mean, ALU.subtract), [("yc", cc), "mean"], ["tt%d" % (cc % 2)])
                            V(lambda e, t=t, cc=cc: e.tensor_tensor(t, t, var, ALU.mult), ["tt%d" % (cc % 2), "var"], ["tt%d" % (cc % 2)])
                            A(lambda e, cc=cc, t=t: e.activation(catC[:, cc, tsl], t, AF.Silu, bias=vecs[:, 33 + cc:34 + cc], scale=vecs[:, 29 + cc:30 + cc]),
                              ["tt%d" % (cc % 2), "vecs"], [("catC", tc)])

                    chk("T2")
                    tr.barrier()
                    R = Region(PB0)
                    w_uq_b = R.a([128, 2, 768], BF16)
                    w_uqs = R.a([128, 2, 256], BF16)
                    w_ukv_b = R.a([128, 1024], BF16)
                    w_out_b = R.a([128, 8, 1024], BF16)
                    KnT = R.a([128, 4, S_LEN], BF16)
                    KrT = R.a([128, S_LEN], BF16)
                    Vb = R.a([128, 16, 512], BF16)
                    scl = R.a([128, 64], F32)
                    sskr = R.a([128, 16], F32)
                    ssk = R.a([128, 16], F32)
                    KT0 = R.off
                    sqk = [R.a([128, 512], BF16) for _ in range(2)]
                    kt1 = R.a([128, 512], F32)
                    kt2 = R.a([128, 512], F32)
                    sqkr = R.a([128, 512], BF16)
                    tr.op(POOL, lambda e, l=l: e.dma_start(out=w_uq_b, in_=wuq_d[l].rearrange("(c p) n -> p c n", p=128)), writes=["w_uq"], dma=s_tw[0])
                    tr.op(POOL, lambda e, l=l: e.dma_start(out=w_ukv_b, in_=wukv_d[l]), writes=["w_ukv"], dma=s_tw[1])
                    wv = wout_d[l].rearrange("(c p) n -> p c n", p=128)
                    for q in range(2):
                        tr.op(POOL, lambda e, q=q, wv=wv: e.dma_start(out=w_out_b[:, 4 * q:4 * q + 4, :], in_=wv[:, 4 * q:4 * q + 4, :]),
                              writes=[("w_out", q)], dma=s_tw[2 + q])
                    for h in range(NH):
                        G(lambda e, h=h: e.tensor_copy(w_uqs[:, :, h * 64:h * 64 + 32], w_uq_b[:, :, h * 192 + 160:h * 192 + 192]), ["w_uq"], ["w_uqs"])
                        G(lambda e, h=h: e.tensor_copy(w_uqs[:, :, h * 64 + 32:h * 64 + 64], w_uq_b[:, :, h * 192 + 128:h * 192 + 160]), ["w_uq"], ["w_uqs"])
                    RT = Region(PB0 + (768 + 256 + 512 + 4096))
                    pos_i = RT.a([128, 1024], I32)
                    ang = RT.a([128, 1024], F32)
                    kf = RT.a([128, 1024], F32)
                    ki = RT.a([128, 1024], I32)
                    C1 = 6.28125
                    C2 = 2.0 * math.pi - 6.28125
                    for hf in range(2):
                        hs = slice(hf * 1024, (hf + 1) * 1024)
                        tr.op(SP, lambda e, s=s, hs=hs: e.dma_start(out=pos_i[0:64, :], in_=pos_d[s, :, hs]), writes=["pos_i"], dma=s_misc)
                        V(lambda e: e.tensor_copy(ang[0:64], pos_i[0:64]), ["pos_i"], ["ang"])
                        V(lambda e: e.tensor_scalar(ang[0:64], ang[0:64], ccols[0:64, 2:3], None, ALU.mult), ["ang", "ccols"], ["ang"])
                        V(lambda e: e.tensor_scalar(kf[0:64], ang[0:64], 1.0 / (2.0 * math.pi), None, ALU.mult), ["ang"], ["kf"])
                        V(lambda e: e.tensor_copy(ki[0:64], kf[0:64]), ["kf"], ["ki"])
                        V(lambda e: e.tensor_copy(kf[0:64], ki[0:64]), ["ki"], ["kf"])
                        V(lambda e: e.scalar_tensor_tensor(ang[0:64], kf[0:64], -C1, ang[0:64], ALU.mult, ALU.add), ["kf", "ang"], ["ang"])
                        V(lambda e: e.scalar_tensor_tensor(ang[0:64], kf[0:64], -C2, ang[0:64], ALU.mult, ALU.add), ["kf", "ang"], ["ang"])
                        V(lambda e: e.tensor_scalar(ang[0:64], ang[0:64], 3.1415925, -3.1415925, ALU.min, ALU.max), ["ang"], ["ang"])
                        A(lambda e, hs=hs: e.activation(sin2[0:64, hs], ang[0:64], AF.Sin, scale=ccols[0:64, 3:4]), ["ang", "ccols"], ["sin2"])
                        V(lambda e: e.scalar_tensor_tensor(kf[0:64], ang[0:64], -1.0, ang[0:64], ALU.mult, ALU.max), ["ang"], ["kf"])
                        V(lambda e: e.tensor_scalar(kf[0:64], kf[0:64], -1.0, math.pi / 2, ALU.mult, ALU.add), ["kf"], ["kf"])
                        A(lambda e, hs=hs: e.activation(cos2[0:64, hs], kf[0:64], AF.Sin), ["kf"], ["cos2"])
                    tr.barrier()
                    chk("tab")
                    wv3 = w_ukv_b.rearrange("p (h c) -> p h c", h=4)
                    for j in range(16):
                        b = nb()
                        T(lambda e, b=b, j=j: e.matmul(ps[b][:, :], ckvn_b[:, j * 128:(j + 1) * 128], wv3[:, :, 128:256], start=True, stop=True),
                          ["ckvn", "w_ukv"], [pk(b)])
                        if j % 2 == 0:
                            A(lambda e, b=b, j=j: e.activation(Vb[:, j, :], ps[b][:, :], AF.Copy), [pk(b)], [("Vb", j)])
                        else:
                            V(lambda e, b=b, j=j: e.tensor_copy(Vb[:, j, :], ps[b][:, :]), [pk(b)], [("Vb", j)])
                    bS = nb()
                    for tc in range(4):
                        tsl = slice(tc * 512, (tc + 1) * 512)
                        V(lambda e, tsl=tsl: e.scalar_tensor_tensor(kt1[0:64], kr_f[0:64, tsl], vecs[0:64, 23:24], cos2[0:64, tsl], ALU.mult, ALU.mult),
                          ["krf", "vecs", "cos2"], ["kt1"])
                        V(lambda e, tsl=tsl: e.scalar_tensor_tensor(kt2[0:64], krs_f[0:64, tsl], vecs[0:64, 24:25], sin2[0:64, tsl], ALU.mult, ALU.mult),
                          ["krsf", "vecs", "sin2"], ["kt2"])
                        V(lambda e, tsl=tsl: e.tensor_tensor(KrT[0:64, tsl], kt1[0:64], kt2[0:64], ALU.add), ["kt1", "kt2"], [("KrT", tc)])
                        A(lambda e, tsl=tsl: e.activation(sqkr[0:64], kr_f[0:64, tsl], AF.Square), ["krf"], ["sqkr"])
                        for j in range(4):
                            T(lambda e, tc=tc, j=j: e.matmul(ps[bS][:, tc * 4 + j:tc * 4 + j + 1], sqkr[0:64, j * 128:(j + 1) * 128], onesb[0:64, 0:1],
                                                           start=True, stop=True), ["sqkr", "onesb"], [pk(bS)])
                    V(lambda e: e.tensor_copy(sskr, ps[bS][:, 0:16]), [pk(bS)], ["sskr"])
                    for h in range(NH):
                        bS = nb()
                        for tc in range(4):
                            tsl = slice(tc * 512, (tc + 1) * 512)
                            b = nb()
                            T(lambda e, b=b, h=h, tsl=tsl: e.matmul(ps[b][:, :], w_ukv_b[:, h * 256:h * 256 + 128], ckvn_b[:, tsl], start=True, stop=True),
                              ["ckvn", "w_ukv"], [pk(b)])
                            A(lambda e, b=b, h=h, tsl=tsl: e.activation(KnT[:, h, tsl], ps[b][:, :], AF.Copy, scale=vecs[:, 22:23]), [pk(b), "vecs"], [("KnT", h)])
                            sq = sqk[tc % 2]
                            A(lambda e, b=b, sq=sq: e.activation(sq, ps[b][:, :], AF.Square), [pk(b)], ["sqk%d" % (tc % 2)])
                            for j in range(4):
                                T(lambda e, tc=tc, j=j, sq=sq, bS=bS: e.matmul(ps[bS][:, tc * 4 + j:tc * 4 + j + 1], sq[:, j * 128:(j + 1) * 128], onesb[:, 0:1],
                                                                               start=True, stop=True), ["sqk%d" % (tc % 2), "onesb"], [pk(bS)])
                        V(lambda e, bS=bS: e.tensor_tensor(ssk, ps[bS][:, 0:16], sskr, ALU.add), [pk(bS), "sskr"], ["ssk"])
                        A(lambda e: e.activation(ssk, ssk, AF.Sqrt, bias=epsc, scale=1.0 / 192), ["ssk", "ccols"], ["ssk"])
                        V(lambda e: e.reciprocal(ssk, ssk), ["ssk"], ["ssk"])
                        V(lambda e, h=h: e.tensor_scalar(scl[:, h * 16:(h + 1) * 16], ssk, 1.0 / math.sqrt(192.0), None, ALU.mult), ["ssk"], [("scl", h)])
                    tr.barrier()
                    chk("kprep")
                    RQ = Region(QT0)
                    catA = RQ.a([128, 4, 512], BF16)
                    QnT = [RQ.a([128, 512], BF16) for _ in range(2)]
                    QrT = [RQ.a([128, 512], BF16) for _ in range(2)]
                    PT = [RQ.a([128, 512], BF16) for _ in range(3)]
                    sqn = RQ.a([128, 512], BF16)
                    sqr = RQ.a([128, 512], BF16)
                    rD = RQ.a([128, 512], F32)
                    assert RQ.off <= YP0
                    RK = Region(KT0)
                    rq = RK.a([128, 512], F32)
                    qt1 = RK.a([128, 512], F32)
                    qt2 = RK.a([128, 512], F32)
                    assert RK.off <= TOTW
                    SB = (0, 1, 2)
                    QB = (3, 4, 5)
                    bO, bD = 6, 7
                    pt_state = {"n": 0}

                    def q_prep(qc, h, qb):
                        qsl = slice(qc * 512, (qc + 1) * 512)
                        bqn, bqr, bqs = QB
                        for k in range(2):
                            T(lambda e, k=k: e.matmul(ps[bqn][:, :], w_uq_b[:, k, h * 192:h * 192 + 128], cqn_b[:, k, qsl], start=(k == 0), stop=(k == 1)),
                              ["w_uq", "cqn"], [pk(bqn)])
                        for k in range(2):
                            T(lambda e, k=k: e.matmul(ps[bqr][0:64, :], w_uq_b[:, k, h * 192 + 128:h * 192 + 192], cqn_b[:, k, qsl], start=(k == 0), stop=(k == 1)),
                              ["w_uq", "cqn"], [pk(bqr)])
                        for k in range(2):
                            T(lambda e, k=k: e.matmul(ps[bqs][0:64, :], w_uqs[:, k, h * 64:(h + 1) * 64], cqn_b[:, k, qsl], start=(k == 0), stop=(k == 1)),
                              ["w_uqs", "cqn"], [pk(bqs)])
                        A(lambda e: e.activation(sqn, ps[bqn][:, :], AF.Square), [pk(bqn)], ["sqn"])
                        A(lambda e: e.activation(sqr[0:64], ps[bqr][0:64, :], AF.Square), [pk(bqr)], ["sqr"])
                        bss = nb(SB)
                        T(lambda e: e.matmul(ps[bss][:, :], onesb, sqn, start=True, stop=False), ["sqn", "onesb"], [pk(bss)])
                        T(lambda e: e.matmul(ps[bss][:, :], onesb[0:64, :], sqr[0:64], start=False, stop=True), ["sqr", "onesb"], [pk(bss)])
                        rstd_from(bss, rq, 192.0, "rq")
                        V(lambda e: e.scalar_tensor_tensor(QnT[qb], ps[bqn][:, :], vecs[:, 19:20], rq, ALU.mult, ALU.mult), [pk(bqn), "vecs", "rq"], ["QnT%d" % qb])
                        V(lambda e: e.scalar_tensor_tensor(qt1[0:64], ps[bqr][0:64, :], vecs[0:64, 20:21], cos2[0:64, qsl], ALU.mult, ALU.mult),
                          [pk(bqr), "vecs", "cos2"], ["qt1"])
                        V(lambda e: e.scalar_tensor_tensor(qt2[0:64], ps[bqs][0:64, :], vecs[0:64, 21:22], sin2[0:64, qsl], ALU.mult, ALU.mult),
                          [pk(bqs), "vecs", "sin2"], ["qt2"])
                        V(lambda e: e.tensor_tensor(qt1[0:64], qt1[0:64], qt2[0:64], ALU.add), ["qt1", "qt2"], ["qt1"])
                        V(lambda e: e.tensor_tensor(QrT[qb][0:64], qt1[0:64], rq[0:64], ALU.mult), ["qt1", "rq"], ["QrT%d" % qb])

                    def core(qc, h, qb, hook):
                        def S_mm(kt):
                            b = nb(SB)
                            ksl = slice(kt * 128, (kt + 1) * 128)
                            T(lambda e: e.matmul(ps[b][:, :], KnT[:, h, ksl], QnT[qb], start=True, stop=False), [("KnT", h), "QnT%d" % qb], [pk(b)])
                            T(lambda e: e.matmul(ps[b][:, :], KrT[0:64, ksl], QrT[qb][0:64], start=False, stop=True), ["KrT", "QrT%d" % qb], [pk(b)])
                            return b
                        sb_cur = S_mm(0)
                        for kt in range(16):
                            sb_next = S_mm(kt + 1) if kt < 15 else None
                            pi = pt_state["n"] % 3
                            pt_state["n"] += 1
                            p_t = PT[pi]
                            A(lambda e: e.activation(p_t, ps[sb_cur][:, :], AF.Exp, scale=scl[:, h * 16 + kt:h * 16 + kt + 1]),
                              [pk(sb_cur), ("scl", h)], ["PT%d" % pi])
                            T(lambda e: e.matmul(ps[bO][:, :], Vb[:, kt, h * 128:(h + 1) * 128], p_t, start=(kt == 0), stop=(kt == 15)),
                              ["PT%d" % pi, ("Vb", kt)], [pk(bO)])
                            T(lambda e: e.matmul(ps[bD][:, :], onesb, p_t, start=(kt == 0), stop=(kt == 15)),
                              ["PT%d" % pi, "onesb"], [pk(bD)])
                            sb_cur = sb_next
                            if kt == 3:
                                hook()
                        V(lambda e: e.reciprocal(rD, ps[bD][:, :]), [pk(bD)], ["rD"])
                        V(lambda e: e.tensor_tensor(catA[:, h, :], ps[bO][:, :], rD, ALU.mult), [pk(bO), "rD"], [("catA", h)])

                    pairs = [(qc, h) for qc in range(4) for h in range(NH)]
                    q_prep(0, 0, 0)
                    for i, (qc, h) in enumerate(pairs):
                        qsl = slice(qc * 512, (qc + 1) * 512)
                        if i + 1 < len(pairs):
                            nqc, nh = pairs[i + 1]
                            hook = (lambda nqc=nqc, nh=nh, i=i: q_prep(nqc, nh, (i + 1) % 2))
                        else:
                            hook = (lambda: None)
                        core(qc, h, i % 2, hook)
                        if h == NH - 1:
                            for m in range(8):
                                b = nb(SB)
                                for k in range(8):
                                    rhs = catA[:, k, :] if k < 4 else catC[:, k - 4, qsl]
                                    rk = ("catA", k) if k < 4 else ("catC", qc)
                                    T(lambda e, b=b, k=k, m=m, rhs=rhs: e.matmul(ps[b][:, :], w_out_b[:, k, m * 128:(m + 1) * 128], rhs, start=(k == 0), stop=(k == 7)),
                                      ["w_out", rk], [pk(b)])
                                V(lambda e, b=b, m=m: e.scalar_tensor_tensor(xT[:, m, qsl], ps[b][:, :], mc[:, 16 + m:17 + m], xT[:, m, qsl], ALU.mult, ALU.add),
                                  [pk(b), "mc", ("xT", (m, qc))], [("xT", (m, qc))])

                    chk("T3")
                    tr.barrier()
                    M = Region(PH0)
                    zer = M.a([128, 1024], F32)
                    h2st = [M.a([128, 1024], BF16) for _ in range(4)]
                    M1 = M
                    sq_b = M1.a([128, 8, 512], BF16)
                    h2T = M1.a([128, 8, 512], BF16)
                    xn = [M1.a([128, 512], F32) for _ in range(2)]
                    rstd2 = M1.a([128, 512], F32)
                    affT = M1.a([128, S_LEN], F32)
                    wr_f = M1.a([128, 8, NE], F32)
                    awr = M1.a([128, 8, NE], F32)
                    lg = M1.a([128, 512], F32)
                    rden = M1.a([128, 512], F32)
                    cstc = M1.a([128, 1], F32)
                    m8 = M1.a([128, 8], F32)
                    masktm_b = M1.a([128, 256], BF16)
                    masktm_f = M1.a([128, 256], F32)
                    afftm = M1.a([128, 256], F32)
                    hi_f = M1.a([128, 256], F32)
                    maskT = M1.a([128, S_LEN], BF16)
                    ET = M1.a([128, S_LEN], F32)
                    work = ET
                    mod_slab = [M1.a([128, 8, 512], F32) for _ in range(2)]

                    tr.op(SP, lambda e, l=l: e.dma_start(out=wr_f, in_=wr_d[l].rearrange("p (c n) -> p c n", c=8)), writes=["wr_f"], dma=s_misc)
                    V(lambda e: e.memset(zer, 0.0), [], ["zer"])
                    for c in range(8):
                        V(lambda e, c=c: e.tensor_scalar(awr[:, c, :], wr_f[:, c, :], mc[:, 24 + c:25 + c], None, ALU.mult), ["wr_f", "mc"], ["awr"])
                    b = nb()
                    for c in range(8):
                        T(lambda e, c=c, b=b: e.matmul(ps[b][0:16, 0:1], wr_f[:, c, :], mc[:, 32 + c:33 + c], start=(c == 0), stop=(c == 7)), ["wr_f", "mc"], [pk(b)])
                    V(lambda e, b=b: e.tensor_copy(cstc[0:16], ps[b][0:16, 0:1]), [pk(b)], ["cstc"])
                    h2v = h2_dram[s].rearrange("(j p) d -> p j d", p=128)
                    for tc in range(4):
                        tsl = slice(tc * 512, (tc + 1) * 512)
                        rms_chunk(tc, mc[:, 24:32], mc[:, 32:40], sq_b, h2T, xn, rstd2, "m1")
                        for j4 in range(4):
                            jt = tc * 4 + j4
                            hst = h2st[jt % 4]
                            for hf in range(2):
                                b = nb()
                                for c4 in range(4):
                                    c = hf * 4 + c4
                                    T(lambda e, b=b, c=c, c4=c4, j4=j4: e.matmul(ps[b][:, c4 * 128:(c4 + 1) * 128], h2T[:, c, j4 * 128:(j4 + 1) * 128], identb,
                                                                              start=True, stop=True), [("hT", c), "identb"], [pk(b)])
                                dst = hst[:, hf * 512:(hf + 1) * 512]
                                if (j4 + hf) % 2 == 0:
                                    A(lambda e, b=b, dst=dst: e.activation(dst, ps[b][:, :], AF.Copy), [pk(b)], [("h2st", jt % 4)])
                                else:
                                    V(lambda e, b=b, dst=dst: e.tensor_copy(dst, ps[b][:, :]), [pk(b)], [("h2st", jt % 4)])
                            tr.op(SP, lambda e, jt=jt, hst=hst: e.dma_start(out=h2v[:, jt, :], in_=hst), reads=[("h2st", jt % 4)], writes=["h2d%d" % s], dma=s_h2)
                        b = nb()
                        for c in range(8):
                            T(lambda e, b=b, c=c, tsl=tsl: e.matmul(ps[b][0:16, :], awr[:, c, :], xT[:, c, tsl], start=(c == 0), stop=(c == 7)),
                              ["awr", ("xT", (c, tc))], [pk(b)])
                        V(lambda e, b=b: e.tensor_tensor(lg[0:16], ps[b][0:16, :], rstd2[0:16], ALU.mult), [pk(b), "m1rstd"], ["lg"])
                        A(lambda e, tsl=tsl: e.activation(ET[0:16, tsl], lg[0:16], AF.Exp, bias=cstc[0:16], scale=1.0), ["lg", "cstc"], [("ET", tc)])
                        b = nb()
                        T(lambda e, b=b, tsl=tsl: e.matmul(ps[b][0:16, :], onesf[0:16, 0:16], ET[0:16, tsl], start=True, stop=True), [("ET", tc), "onesf"], [pk(b)])
                        V(lambda e, b=b: e.reciprocal(rden[0:16], ps[b][0:16, :]), [pk(b)], ["rden"])
                        V(lambda e, tsl=tsl: e.tensor_tensor(affT[0:16, tsl], ET[0:16, tsl], rden[0:16], ALU.mult), [("ET", tc), "rden"], [("affT", tc)])
                        if l + 1 < L:
                            per = (12 + n_seq - 1) // n_seq
                            mine = list(range(s * per, min(12, (s + 1) * per)))
                            q4 = (len(mine) + 3) // 4
                            emit_mod(l + 1, mod_slab, mine[tc * q4:(tc + 1) * q4])
                    for c in range(8):
                        tr.op(SP, lambda e, s=s, c=c: e.dma_start(out=xs_dram[s, c * 128:(c + 1) * 128, :], in_=xT[:, c, :]),
                              reads=[("xT", (c, t)) for t in range(4)], writes=["xs%d" % s], dma=s_sp)
                    if s + 1 < n_seq:
                        load_x(l, s + 1)
                    for ct in range(2):
                        accv = acc_dram[s][ct].rearrange("(j p) d -> p j d", p=128)
                        for j in range(16):
                            tr.op(SP, lambda e, j=j, accv=accv: e.dma_start(out=accv[:, j, :], in_=zer), reads=["zer"], writes=["accd%d%d" % (s, ct)], dma=s_zero)
                    ba = nb()
                    for j in range(16):
                        jsl = slice(j * 128, (j + 1) * 128)
                        T(lambda e, j=j, jsl=jsl: e.matmul(ps[ba][:, j * 16:(j + 1) * 16], affT[0:16, jsl], identf[0:16, 0:16], start=True, stop=True),
                          ["affT", "identf"], [pk(ba)])
                    V(lambda e: e.tensor_copy(afftm, ps[ba][:, 0:256]), [pk(ba)], ["afftm"])
                    lo16 = M1.a([128, 16], F32)
                    mid16 = M1.a([128, 16], F32)
                    cnt16 = M1.a([128, 16], F32)
                    ge16 = M1.a([128, 16], F32)
                    a3 = afftm.rearrange("p (j e) -> p j e", e=16)
                    m3 = masktm_b.rearrange("p (j e) -> p j e", e=16)
                    V(lambda e: e.memset(lo16, 0.0), [], ["lo16"])
                    NBIS = 30
                    for k in range(NBIS):
                        wk = 2.0 ** (-(k + 1))
                        V(lambda e, wk=wk: e.tensor_scalar(mid16, lo16, wk, None, ALU.add), ["lo16"], ["mid16"])
                        midb = mid16.rearrange("p (o e) -> p o e", o=1).to_broadcast([128, 16, 16])
                        V(lambda e, midb=midb: e.tensor_tensor(m3, a3, midb, ALU.is_ge), ["afftm", "mid16"], ["masktm_b"])
                        bc = nb()
                        T(lambda e, bc=bc: e.matmul(ps[bc][:, 0:256], onesb, masktm_b, start=True, stop=True), ["masktm_b", "onesb"], [pk(bc)])
                        pv = ps[bc][:, 0:256].rearrange("p (j e) -> p e j", e=16)
                        V(lambda e, pv=pv: e.tensor_reduce(out=cnt16, in_=pv, axis=mybir.AxisListType.X, op=ALU.add), [pk(bc)], ["cnt16"])
                        V(lambda e, wk=wk: e.tensor_scalar(ge16, cnt16, float(CAP) - 0.5, wk, ALU.is_ge, ALU.mult), ["cnt16"], ["ge16"])
                        V(lambda e: e.tensor_tensor(lo16, lo16, ge16, ALU.add), ["lo16", "ge16"], ["lo16"])
                    lob = lo16.rearrange("p (o e) -> p o e", o=1).to_broadcast([128, 16, 16])
                    mf3 = masktm_f.rearrange("p (j e) -> p j e", e=16)
                    V(lambda e: e.tensor_tensor(mf3, a3, lob, ALU.is_ge), ["afftm", "lo16"], ["masktm_f"])
                    V(lambda e: e.tensor_copy(masktm_b, masktm_f), ["masktm_f"], ["masktm_b"])
                    V(lambda e: e.tensor_copy(G4[:, :, 0], afftm), ["afftm"], ["G4%d" % s])
                    V(lambda e: e.tensor_copy(hi_f, G4[:, :, 0]), ["G4%d" % s], ["hi_f"])
                    V(lambda e: e.tensor_tensor(G4[:, :, 1], afftm, hi_f, ALU.subtract), ["afftm", "hi_f"], ["G4%d" % s])
                    V(lambda e: e.tensor_scalar(G4[:, :, 2], jrow, 0.0, ccols[:, 5:6], ALU.mult, ALU.add), ["jrow", "ccols"], ["G4%d" % s])
                    V(lambda e: e.tensor_copy(G4[:, :, 3], jrow), ["jrow"], ["G4%d" % s])
                    bp = nb()
                    for j in range(16):
                        for i in range(j):
                            T(lambda e, i=i, j=j: e.matmul(ps[bp][:, j * 16:(j + 1) * 16], onesb, masktm_b[:, i * 16:(i + 1) * 16], start=(i == 0), stop=False),
                              ["masktm_b", "onesb"], [pk(bp)])
                        T(lambda e, j=j: e.matmul(ps[bp][:, j * 16:(j + 1) * 16], BIG[:, 384:512], masktm_b[:, j * 16:(j + 1) * 16], start=(j == 0), stop=True),
                          ["masktm_b", "BIG"], [pk(bp)])
                    V(lambda e: e.scalar_tensor_tensor(pos1tm, ps[bp][:, 0:256], 1.0, masktm_f, ALU.add, ALU.mult), [pk(bp), "masktm_f"], ["pos1tm%d" % s])

                    chk("M1")

                tr.barrier()
                NSL = 256 * n_seq
                EA = Region(0)
                yef = [[EA.a([128, 1024], F32) for _ in range(2 * n_seq)] for _ in range(2)]
                xe_tm = [[EA.a([128, 2, 1024], BF16) for _ in range(n_seq)] for _ in range(2)]
                xeT = [EA.a([128, 8, NSL], BF16) for _ in range(2)]
                assert EA.off <= 16384, EA.off
                EB = Region(PH0)
                NSLOT = 12
                wsl = [EB.a([128, 4, 1024], BF16) for _ in range(NSLOT)]
                Sel = [EB.a([128, 16, 256], BF16) for _ in range(n_seq)]
                hidT = EB.a([128, 8, NSL], BF16)
                sgt = [EB.a([128, NSL], F32) for _ in range(2)]
                gcol = [[EB.a([128, 2], F32) for _ in range(n_seq)] for _ in range(3)]
                idxi = [[EB.a([128, 2], I32) for _ in range(n_seq)] for _ in range(3)]
                gtmp = EB.a([128, 8], F32)
                idxf = EB.a([128, 2], F32)

                wl = []
                for ex in range(NE):
                    wl += [(wg_d, ex, 0), (wg_d, ex, 1), (wu_d, ex, 0), (wu_d, ex, 1), (wd_d, ex, 0), (wd_d, ex, 1)]
                wstate = {"n": 0}

                def issue_w(upto):
                    while wstate["n"] < min(upto, len(wl)):
                        k = wstate["n"]
                        src, ex, hf = wl[k]
                        sl = k % NSLOT
                        sv = src[l, ex].rearrange("(c p) f -> p c f", p=128)[:, hf * 4:(hf + 1) * 4, :]
                        tr.op(POOL, lambda e, sl=sl, sv=sv: e.dma_start(out=wsl[sl], in_=sv), writes=["wsl%d" % sl], dma=s_w[sl])
                        wstate["n"] += 1

                issue_w(NSLOT - 1)
                wix = {"i": 0}

                def prep_sel(ex):
                    for s in range(n_seq):
                        for j in range(16):
                            V(lambda e, j=j, ex=ex, s=s: e.tensor_scalar(Sel[s][:, j, :], iota1, pos1tm_s[s][:, j * 16 + ex:j * 16 + ex + 1], None, ALU.is_equal),
                              ["iota1", "pos1tm%d" % s], [("Sel%d" % s, j)])

                def prep_idx(ex):
                    pb = ex % 3
                    for s in range(n_seq):
                        b = nb()
                        for ct in range(2):
                            for j in range(16):
                                T(lambda e, b=b, ct=ct, j=j, ex=ex, s=s: e.matmul(ps[b][:, ct * 4:ct * 4 + 4], Sel[s][:, j, ct * 128:(ct + 1) * 128], G4_s[s][:, j * 16 + ex, :],
                                                                                 start=(j == 0), stop=(j == 15)), [("Sel%d" % s, j), "G4%d" % s], [pk(b)])
                        V(lambda e, b=b: e.tensor_copy(gtmp, ps[b][:, 0:8]), [pk(b)], ["gtmp"])
                        g3 = gtmp.rearrange("p (a b) -> p a b", b=4)
                        V(lambda e, g3=g3, pb=pb, s=s: e.tensor_tensor(gcol[pb][s], g3[:, :, 0], g3[:, :, 1], ALU.add), ["gtmp"], ["gcol%d%d" % (pb, s)])
                        V(lambda e, g3=g3: e.scalar_tensor_tensor(idxf, g3[:, :, 3], 128.0, g3[:, :, 2], ALU.mult, ALU.add), ["gtmp"], ["idxf"])
                        V(lambda e, pb=pb, s=s: e.tensor_copy(idxi[pb][s], idxf), ["idxf"], ["idxi%d%d" % (pb, s)])

                def gather(ex):
                    pb, p3 = ex % 2, ex % 3
                    for s in range(n_seq):
                        for ct in range(2):
                            tr.op(POOL, lambda e, ct=ct, pb=pb, p3=p3, s=s: e.indirect_dma_start(
                                out=xe_tm[pb][s][:, ct, :], out_offset=None, in_=h2_dram[s],
                                in_offset=bass.IndirectOffsetOnAxis(ap=idxi[p3][s][:, ct:ct + 1], axis=0)),
                                reads=["h2d%d" % s, "idxi%d%d" % (p3, s)], writes=[("xe_tm%d%d" % (pb, s), ct)], dma=s_g[pb])

                def prep_tr(ex):
                    pb = ex % 2
                    for s in range(n_seq):
                        for cp in range(4):
                            b = nb()
                            for c in (2 * cp, 2 * cp + 1):
                                for ct in range(2):
                                    T(lambda e, b=b, c=c, ct=ct, pb=pb, s=s: e.matmul(ps[b][:, (c % 2) * 256 + ct * 128:(c % 2) * 256 + (ct + 1) * 128],
                                                                                   xe_tm[pb][s][:, ct, c * 128:(c + 1) * 128], identb, start=True, stop=True),
                                      [("xe_tm%d%d" % (pb, s), ct), "identb"], [pk(b)])
                            dst = xeT[pb][:, 2 * cp:2 * cp + 2, s * 256:(s + 1) * 256]
                            if cp % 2 == 0:
                                A(lambda e, b=b, dst=dst: e.activation(dst, ps[b][:, :].rearrange("p (a b) -> p a b", a=2), AF.Copy), [pk(b)], [("xeT%d" % pb, cp)])
                            else:
                                V(lambda e, b=b, dst=dst: e.tensor_copy(dst, ps[b][:, :].rearrange("p (a b) -> p a b", a=2)), [pk(b)], [("xeT%d" % pb, cp)])

                def ffn_gu(ex):
                    pb = ex % 2
                    widx = wix["i"]
                    issue_w(widx + NSLOT)
                    for fc in range(8):
                        bg = nb()
                        bu = nb()
                        for (w0, b) in ((widx, bg), (widx + 2, bu)):
                            for c in range(8):
                                wsel = (w0 + c // 4) % NSLOT
                                T(lambda e, b=b, wsel=wsel, c=c, fc=fc, pb=pb: e.matmul(ps[b][:, 0:NSL], wsl[wsel][:, c % 4, fc * 128:(fc + 1) * 128], xeT[pb][:, c, :],
                                                                                     start=(c == 0), stop=(c == 7)), ["wsl%d" % wsel, ("xeT%d" % pb, c // 2)], [pk(b)])
                        st_ = sgt[fc % 2]
                        A(lambda e, bg=bg, st_=st_: e.activation(st_, ps[bg][:, 0:NSL], AF.Silu), [pk(bg)], ["sgt%d" % (fc % 2)])
                        V(lambda e, bu=bu, st_=st_, fc=fc: e.tensor_tensor(hidT[:, fc, :], ps[bu][:, 0:NSL], st_, ALU.mult), [pk(bu), "sgt%d" % (fc % 2)], [("hidT", fc)])
                    wix["i"] += 4

                def ffn_down(ex):
                    pb = ex % 2
                    p3 = ex % 3
                    widx = wix["i"]
                    issue_w(widx + NSLOT)
                    for nd in range(2):
                        for q in range(2 * n_seq):
                            s, ct = q // 2, q % 2
                            b = nb()
                            for fc in range(8):
                                sd_ = (widx + fc // 4) % NSLOT
                                T(lambda e, b=b, fc=fc, q=q, sd_=sd_, nd=nd: e.matmul(ps[b][:, :], hidT[:, fc, q * 128:(q + 1) * 128], wsl[sd_][:, fc % 4, nd * 512:(nd + 1) * 512],
                                                                                   start=(fc == 0), stop=(fc == 7)),
                                  [("hidT", fc), "wsl%d" % sd_], [pk(b)])
                            A(lambda e, b=b, pb=pb, p3=p3, q=q, nd=nd, s=s, ct=ct: e.activation(yef[pb][q][:, nd * 512:(nd + 1) * 512], ps[b][:, :], AF.Copy, scale=gcol[p3][s][:, ct:ct + 1]),
                              [pk(b), "gcol%d%d" % (p3, s)], ["yef%d%d" % (pb, q)])
                    wix["i"] += 2
                    for q in range(2 * n_seq):
                        s, ct = q // 2, q % 2
                        tr.op(POOL, lambda e, ct=ct, pb=pb, p3=p3, s=s, q=q: e.indirect_dma_start(
                            out=acc_dram[s][ct], out_offset=bass.IndirectOffsetOnAxis(ap=idxi[p3][s][:, ct:ct + 1], axis=0),
                            in_=yef[pb][q], in_offset=None, bounds_check="BCREG", oob_is_err=True, compute_op=ALU.add),
                            reads=["yef%d%d" % (pb, q), "idxi%d%d" % (p3, s), "accd%d%d" % (s, ct)], writes=["accd%d%d" % (s, ct)], dma=s_acc[s][ct])

                prep_sel(0)
                prep_idx(0)
                gather(0)
                prep_sel(1)
                prep_idx(1)
                prep_tr(0)
                for ex in range(NE):
                    if ex + 1 < NE:
                        gather(ex + 1)
                    if ex + 2 < NE:
                        prep_sel(ex + 2)
                    ffn_gu(ex)
                    if ex + 2 < NE:
                        prep_idx(ex + 2)
                    ffn_down(ex)
                    if ex + 1 < NE:
                        prep_tr(ex + 1)
                tr.barrier()
                EC = Region(0)
                xch = [EC.a([128, 8, 512], F32) for _ in range(2)]
                accb = [[EC.a([128, 1024], F32) for _ in range(2)] for _ in range(4)]
                assert EC.off <= 16384
                kx = 0
                for s in range(n_seq):
                    accv = [acc_dram[s][ct].rearrange("(j p) d -> p j d", p=128) for ct in range(2)]
                    xsv = xs_dram[s].rearrange("(c p) t -> p c t", p=128)
                    dstv = (out_d[s] if l == L - 1 else xs_dram[s]).rearrange("(c p) t -> p c t", p=128)
                    for tc in range(4):
                        xc = xch[kx % 2]
                        xk = "xch%d" % (kx % 2)
                        tr.op(SP, lambda e, xc=xc, xsv=xsv, tc=tc: e.dma_start(out=xc, in_=xsv[:, :, tc * 512:(tc + 1) * 512]),
                              reads=[("xs%d" % s, tc)], writes=[xk], dma=s_xc[kx % 2])
                        for j4 in range(4):
                            j = tc * 4 + j4
                            ab = accb[j % 4]
                            for ct in range(2):
                                tr.op(SP if ct == 0 else ACT, lambda e, j=j, ab=ab, accv=accv, ct=ct: e.dma_start(out=ab[ct], in_=accv[ct][:, j, :]), reads=["accd%d%d" % (s, ct)],
                                      writes=["accb%d_%d" % (j % 4, ct)], dma=s_ab2[j % 4][ct])
                            V(lambda e, ab=ab: e.tensor_tensor(ab[0], ab[0], ab[1], ALU.add), ["accb%d_0" % (j % 4), "accb%d_1" % (j % 4)], ["accb%d_0" % (j % 4)])
                            for hf in range(2):
                                b = nb()
                                for c4 in range(4):
                                    c = hf * 4 + c4
                                    T(lambda e, b=b, c=c, c4=c4, ab=ab: e.matmul(ps[b][:, c4 * 128:(c4 + 1) * 128], ab[0][:, c * 128:(c + 1) * 128], identf, start=True, stop=True),
                                      ["accb%d_0" % (j % 4), "identf"], [pk(b)])
                                for c4 in range(4):
                                    c = hf * 4 + c4
                                    V(lambda e, b=b, c=c, c4=c4, j4=j4, xc=xc, s=s: e.scalar_tensor_tensor(xc[:, c, j4 * 128:(j4 + 1) * 128], ps[b][:, c4 * 128:(c4 + 1) * 128], mcs[s][:, 40 + c:41 + c],
                                                                                                      xc[:, c, j4 * 128:(j4 + 1) * 128], ALU.mult, ALU.add),
                                      [pk(b), "mc", xk], [xk])
                        tr.op(POOL, lambda e, xc=xc, dstv=dstv, tc=tc: e.dma_start(out=dstv[:, :, tc * 512:(tc + 1) * 512], in_=xc),
                              reads=[xk], writes=[("xs%d" % s, tc)], dma=s_out)
                        kx += 1
        try:
            main_body()
        except _Stop:
            pass
        tr.emit(nc)
    return nc


def _consts():
    c = np.zeros((128, NCST), np.float32)
    c[:, 0:128] = np.eye(128, dtype=np.float32)
    tp = np.arange(128)
    c[:, 128:256] = (tp[:, None] < tp[None, :]).astype(np.float32)
    c[:, 256:512] = np.arange(1, 257, dtype=np.float32)[None, :]
    c[:, 512] = tp + 1
    c[:, 513] = tp + 129
    inv_freq = (1.0 / (np.float32(10000.0) ** (np.arange(0, 64, 2, dtype=np.float32) / np.float32(64)))).astype(np.float32)
    c[0:64, 514] = np.concatenate([inv_freq, inv_freq])
    c[0:32, 515] = -1.0
    c[32:64, 515] = 1.0
    c[:, 516] = EPS
    c[:, 517] = tp
    c[:, 520:776] = (np.arange(256) // 16).astype(np.float32)[None, :]
    return c


def _col(v, nch):
    return np.ascontiguousarray(np.asarray(v, np.float32).reshape(nch, 128).T)


def _pack_layer_inputs(inp, L):
    vec = np.zeros((L, 128, NV), np.float32)
    for l in range(L):
        v = vec[l]
        v[:, 0:8] = _col(inp["norm1_g"][l], 8)
        v[:, 8:16] = _col(inp["norm2_g"][l], 8)
        v[:, 16:18] = _col(inp["q_latent_g"][l], 2)
        v[:, 18:19] = _col(inp["kv_latent_g"][l], 1)
        qg = np.asarray(inp["q_head_g"][l], np.float32)
        kg = np.asarray(inp["k_head_g"][l], np.float32)
        v[:, 19] = qg[0:128]
        v[0:64, 20] = qg[128:192]
        v[0:64, 21] = np.concatenate([qg[160:192], qg[128:160]])
        v[:, 22] = kg[0:128]
        v[0:64, 23] = kg[128:192]
        v[0:64, 24] = np.concatenate([kg[160:192], kg[128:160]])
        v[:, 25:29] = _col(inp["conv_b"][l], 4)
        v[:, 29:33] = _col(inp["conv_norm_g"][l], 4)
        v[:, 33:37] = _col(inp["conv_norm_b"][l], 4)
        cw = np.asarray(inp["conv_w"][l], np.float32)
        for cc in range(4):
            v[:, 37 + cc * 31:37 + (cc + 1) * 31] = cw[:, cc * 128:(cc + 1) * 128].T
    bada = np.stack([_col(inp["b_ada"][l], 48) for l in range(L)])
    wr = np.stack([np.ascontiguousarray(np.asarray(inp["w_router"][l], np.float32).reshape(8, 128, NE).transpose(1, 0, 2)).reshape(128, 8 * NE)
                   for l in range(L)])
    return vec, bada, wr


def make_in_maps(inp, n_cores, n_seq, L, batch_ids=None):
    f32 = lambda a: np.ascontiguousarray(np.asarray(a, np.float32))
    vec, bada, wr = _pack_layer_inputs(inp, L)
    cst = _consts()
    shared = {
        "cst": cst, "vec": vec, "bada": bada, "w_router": wr,
        "w_ada": f32(inp["w_ada"][:L]), "w_in": f32(inp["w_in"][:L]), "w_uq": f32(inp["w_uq"][:L]),
        "w_ukv": f32(inp["w_ukv"][:L]), "w_out": f32(inp["w_out"][:L]),
        "w_gate": f32(inp["w_gate"][:L]), "w_up": f32(inp["w_up"][:L]), "w_down": f32(inp["w_down"][:L]),
    }
    x = np.asarray(inp["x"], np.float32)
    c = np.asarray(inp["c"], np.float32)
    pos = np.asarray(inp["positions"], np.int32)
    maps = []
    for core in range(n_cores):
        ids = batch_ids[core] if batch_ids is not None else list(range(core * n_seq, (core + 1) * n_seq))
        xT = np.ascontiguousarray(np.stack([x[b].T for b in ids]))
        cT = np.zeros((128, 8 * n_seq), np.float32)
        for si, b in enumerate(ids):
            cT.reshape(128, 8, n_seq)[:, :, si] = c[b].reshape(8, 128).T
        posr = np.ascontiguousarray(np.stack([np.broadcast_to(pos[b][None, :], (64, S_LEN)) for b in ids])).astype(np.int32)
        m = dict(shared)
        m.update({"xT": xT, "cT": cT, "posr": posr})
        maps.append(m)
    return maps


_NC_CACHE = {}


def kernel(**inputs):
    n_cores, n_seq, L = 8, 2, 2
    key = (n_seq, L)
    if key not in _NC_CACHE:
        _NC_CACHE[key] = build_program(n_seq, L)
    nc = _NC_CACHE[key]
    maps = make_in_maps(inputs, n_cores, n_seq, L)
    res = run_bass_kernel_spmd(nc, maps, core_ids=list(range(n_cores)))
    out = np.empty((n_cores * n_seq, S_LEN, D), np.float32)
    for core in range(n_cores):
        oT = res.results[core]["outT"]
        for si in range(n_seq):
            out[core * n_seq + si] = oT[si].T
    return out
```

```python
import contextlib
import math
import numpy as np
import concourse.bass as bass
import concourse.mybir as mybir
from concourse.bass_utils import run_bass_kernel_spmd

F32 = mybir.dt.float32
BF16 = mybir.dt.bfloat16
I32 = mybir.dt.int32
ALU = mybir.AluOpType
AF = mybir.ActivationFunctionType

PE, ACT, DVE, POOL, SP = "pe", "act", "dve", "pool", "sp"
ENGS = [PE, ACT, DVE, POOL, SP]

S_LEN = 2048
D = 1024
NH = 4
NE = 16
CAP = 256
EPS = 1e-6
NV = 161
NCST = 520 + 256


class _Op:
    __slots__ = ("eng", "idx", "fn", "waits", "sig", "sigval", "dma", "clock", "dclock")


class DmaSlot:
    def __init__(self, name):
        self.name = name
        self.count = 0
        self.sem = None
        self.last_op = None


class _Rec:
    def __getattr__(self, name):
        return lambda *a, **k: (name, a, k)


_REC = _Rec()


class Tracker:
    def __init__(self):
        self.ops = {e: [] for e in ENGS}
        self.state = {}
        self.clock = {e: {} for e in ENGS}
        self.dclock = {e: {} for e in ENGS}
        self.pending = {e: [] for e in ENGS}
        self.slots = []

    def slot(self, name):
        s = DmaSlot(name)
        self.slots.append(s)
        return s

    def _collect(self, key, is_write, deps):
        buf, sub = key if isinstance(key, tuple) else (key, None)
        st = self.state.setdefault(buf, {})
        if sub is None:
            ents = list(st.values())
        else:
            ents = [st[k] for k in (sub, None) if k in st]
        for ent in ents:
            if ent[0] is not None:
                deps.append(ent[0])
            if is_write:
                deps.extend(ent[1].values())
                deps.extend(ent[2])

    def _update(self, key, is_write, tok):
        buf, sub = key if isinstance(key, tuple) else (key, None)
        st = self.state.setdefault(buf, {})
        if is_write:
            if sub is None:
                st.clear()
                st[None] = [tok, {}, []]
            else:
                st[sub] = [tok, {}, []]
        else:
            ent = st.setdefault(sub, [None, {}, []])
            if tok[0] == "op":
                ent[1][tok[1]] = tok
            else:
                ent[2].append(tok)
                if len(ent[2]) > 8:
                    ent[2] = ent[2][-8:]

    def op(self, eng, fn, reads=(), writes=(), dma=None):
        o = _Op()
        o.eng = eng
        o.idx = len(self.ops[eng])
        o.fn = fn(_REC)
        o.sig = False
        o.sigval = None
        o.dma = None
        deps = list(self.pending[eng])
        self.pending[eng] = []
        for k in reads:
            self._collect(k, False, deps)
        for k in writes:
            self._collect(k, True, deps)
        clock = self.clock[eng]
        dclock = self.dclock[eng]
        waits = []
        for d in deps:
            if d[0] == "op":
                _, e2, i2 = d
                if e2 == eng:
                    if eng in (PE, SP):
                        continue
                    if i2 < o.idx - 3:
                        continue
                    if clock.get(e2, -1) >= i2:
                        continue
                    src = self.ops[e2][i2]
                    src.sig = True
                    waits.append(("op", src))
                    clock[e2] = i2
                    continue
                if clock.get(e2, -1) >= i2:
                    continue
                src = self.ops[e2][i2]
                src.sig = True
                waits.append(("op", src))
                clock[e2] = i2
                for k, v in src.clock.items():
                    if k != eng and clock.get(k, -1) < v:
                        clock[k] = v
                for k, v in src.dclock.items():
                    if dclock.get(k, -1) < v:
                        dclock[k] = v
            else:
                _, slot, val, src = d
                val = slot.count
                src = slot.last_op
                if dclock.get(slot, -1) >= val:
                    continue
                waits.append(("dma", slot, val))
                dclock[slot] = val
                for k, v in src.clock.items():
                    if k != eng and clock.get(k, -1) < v:
                        clock[k] = v
                for k, v in src.dclock.items():
                    if dclock.get(k, -1) < v:
                        dclock[k] = v
        o.waits = waits
        o.clock = dict(clock)
        o.dclock = dict(dclock)
        if dma is not None:
            dma.count += 16
            o.dma = (dma, dma.count)
            dma.last_op = o
            tok = ("dma", dma, dma.count, o)
        else:
            tok = ("op", eng, o.idx)
        self.ops[eng].append(o)
        for k in reads:
            self._update(k, False, tok)
        for k in writes:
            self._update(k, True, tok)
        return o

    def barrier(self):
        toks = []
        for e in (PE, ACT, DVE, POOL):
            for o in reversed(self.ops[e]):
                if o.dma is None:
                    toks.append(("op", e, o.idx))
                    break
        for s in self.slots:
            if s.last_op is not None:
                toks.append(("dma", s, s.count, s.last_op))
        for e in ENGS:
            self.pending[e] = list(toks)

    def emit(self, nc):
        stack = contextlib.ExitStack()
        with stack:
            esem = {}
            for e in (PE, ACT, DVE, POOL):
                esem[e] = stack.enter_context(nc.semaphore("s_" + e))
            for i, s in enumerate(self.slots):
                if s.count > 0:
                    s.sem = stack.enter_context(nc.semaphore("d%d_%s" % (i, s.name)))
            for e in (PE, ACT, DVE, POOL):
                c = 0
                for o in self.ops[e]:
                    if o.sig:
                        c += 1
                        o.sigval = c
            block = stack.enter_context(nc.Block())

            def run(engobj, e):
                bcreg = None
                if e == POOL and any(o.fn[2].get("bounds_check") == "BCREG" for o in self.ops[e]):
                    bcreg = engobj.alloc_register("bcreg")
                    engobj.reg_mov(bcreg, S_LEN - 1)
                for o in self.ops[e]:
                    for w in o.waits:
                        if w[0] == "op":
                            engobj.wait_ge(esem[w[1].eng], w[1].sigval)
                        else:
                            engobj.wait_ge(w[1].sem, w[2])
                    name, a_, k_ = o.fn
                    if k_.get("bounds_check") == "BCREG":
                        k_ = dict(k_)
                        k_["bounds_check"] = bcreg
                    ins = getattr(engobj, name)(*a_, **k_)
                    if o.dma is not None:
                        ins.then_inc(o.dma[0].sem, 16)
                    elif o.sig:
                        ins.then_inc(esem[e], 1)

            @block.tensor
            def _(t):
                run(t, PE)

            @block.scalar
            def _(a):
                run(a, ACT)

            @block.vector
            def _(v):
                run(v, DVE)

            @block.gpsimd
            def _(g):
                run(g, POOL)

            @block.sync
            def _(s):
                run(s, SP)
                for sl in self.slots:
                    if sl.count > 0:
                        s.wait_ge(sl.sem, sl.count)


LAY = []


class _Stop(Exception):
    pass


def build_program(n_seq=2, n_layers=2, stop=None):
    import inspect
    del LAY[:]
    nc = bass.Bass("TRN2", target_bir_lowering=False)
    L = n_layers
    xT_d = nc.dram_tensor("xT", [n_seq, D, S_LEN], F32, kind="ExternalInput").ap()
    out_d = nc.dram_tensor("outT", [n_seq, D, S_LEN], F32, kind="ExternalOutput").ap()
    cT_d = nc.dram_tensor("cT", [128, 8 * n_seq], F32, kind="ExternalInput").ap()
    pos_d = nc.dram_tensor("posr", [n_seq, 64, S_LEN], I32, kind="ExternalInput").ap()
    cst_d = nc.dram_tensor("cst", [128, NCST], F32, kind="ExternalInput").ap()
    vec_d = nc.dram_tensor("vec", [L, 128, NV], F32, kind="ExternalInput").ap()
    bada_d = nc.dram_tensor("bada", [L, 128, 48], F32, kind="ExternalInput").ap()
    wada_d = nc.dram_tensor("w_ada", [L, D, 6 * D], F32, kind="ExternalInput").ap()
    win_d = nc.dram_tensor("w_in", [L, D, 1472], F32, kind="ExternalInput").ap()
    wuq_d = nc.dram_tensor("w_uq", [L, 256, 768], F32, kind="ExternalInput").ap()
    wukv_d = nc.dram_tensor("w_ukv", [L, 128, 1024], F32, kind="ExternalInput").ap()
    wout_d = nc.dram_tensor("w_out", [L, D, D], F32, kind="ExternalInput").ap()
    wr_d = nc.dram_tensor("w_router", [L, 128, 8 * NE], F32, kind="ExternalInput").ap()
    wg_d = nc.dram_tensor("w_gate", [L, NE, D, D], F32, kind="ExternalInput").ap()
    wu_d = nc.dram_tensor("w_up", [L, NE, D, D], F32, kind="ExternalInput").ap()
    wd_d = nc.dram_tensor("w_down", [L, NE, D, D], F32, kind="ExternalInput").ap()

    h2_dram = [nc.dram_tensor("h2s%d" % i, [S_LEN, D], BF16, kind="Internal").ap() for i in range(n_seq)]
    acc_dram = [[nc.dram_tensor("accs%d_%d" % (i, ct), [S_LEN, D], F32, kind="Internal").ap() for ct in range(2)] for i in range(n_seq)]
    xs_dram = nc.dram_tensor("xss", [n_seq, D, S_LEN], F32, kind="Internal").ap()
    tr = Tracker()
    TOTW = 53000
    dump_d = nc.dram_tensor("dump", [128, TOTW], F32, kind="ExternalOutput").ap() if stop is not None else None
    stack = contextlib.ExitStack()
    with stack:
        arena = stack.enter_context(nc.sbuf_tensor("arena", [128, TOTW], F32))
        ps = [stack.enter_context(nc.psum_tensor("ps%d" % i, [128, 512], F32)) for i in range(8)]

        def view(off, shape, dt):
            n = int(np.prod(shape[1:]))
            nb = n * (2 if dt == BF16 else 4)
            nw = (nb + 3) // 4
            assert off + nw <= TOTW, (off, nw)
            if stop is not None:
                LAY.append((inspect.stack()[2].lineno, off, tuple(shape), "bf16" if dt == BF16 else ("i32" if dt == I32 else "f32")))
            v = arena[:, off:off + nw]
            if dt != F32:
                v = v.bitcast(dt)
            if len(shape) == 3:
                v = v.rearrange("p (a b) -> p a b", a=shape[1])
            elif len(shape) == 4:
                v = v.rearrange("p (a b c) -> p a b c", a=shape[1], b=shape[2])
            return v, off + nw

        class Region:
            def __init__(self, start):
                self.off = start

            def a(self, shape, dt):
                v, self.off = view(self.off, shape, dt)
                return v

        def chk(name):
            if stop == name:
                tr.barrier()
                tr.op(SP, lambda e: e.dma_start(out=dump_d, in_=arena[:, :]), dma=s_out)
                raise _Stop()

        V = lambda fn, r=(), w=(): tr.op(DVE, fn, r, w)
        A = lambda fn, r=(), w=(): tr.op(ACT, fn, r, w)
        G = lambda fn, r=(), w=(): tr.op(POOL, fn, r, w)
        T = lambda fn, r=(), w=(): tr.op(PE, fn, r, w)
        bank_ctr = [0]

        def nb(pool=(0, 1, 2, 3, 4, 5, 6, 7)):
            b = pool[bank_ctr[0] % len(pool)]
            bank_ctr[0] += 1
            return b

        def pk(b):
            return "ps%d" % b

        P = Region(0)
        xT = P.a([128, 8, S_LEN], F32)
        identb = P.a([128, 128], BF16)
        onesb = P.a([128, 128], BF16)
        BIG = P.a([128, 1024], BF16)
        jrow = P.a([128, 256], F32)
        identf = P.a([128, 128], F32)
        iota1 = P.a([128, 256], F32)
        ccols = P.a([128, 8], F32)
        onesf = P.a([128, 16], F32)
        modc = P.a([128, L * 48 * n_seq], F32)
        mcs = [P.a([128, 48], F32) for _ in range(n_seq)]
        pos1tm_s = [P.a([128, 256], F32) for _ in range(n_seq)]
        G4_s = [P.a([128, 256, 4], BF16) for _ in range(n_seq)]
        vecs = P.a([128, NV], F32)
        cact = P.a([128, 8 * n_seq], F32)
        bada = P.a([128, L * 48], F32)
        PH0 = P.off
        epsc = ccols[:, 4:5]

        s_x = tr.slot("x")
        s_out = tr.slot("out")
        s_misc = tr.slot("misc")
        s_ada = [tr.slot("ada0"), tr.slot("ada1")]
        s_w = [tr.slot("w%d" % i) for i in range(12)]
        s_tw = [tr.slot("tw%d" % i) for i in range(4)]
        s_g = [tr.slot("g0"), tr.slot("g1")]
        s_acc = [[tr.slot("acc%d_%d" % (i, ct)) for ct in range(2)] for i in range(n_seq)]
        s_zero = tr.slot("zero")
        s_sp = tr.slot("sp")
        s_xc = [tr.slot("xc0"), tr.slot("xc1")]
        s_h2 = tr.slot("h2")
        s_ab = [tr.slot("ab%d" % i) for i in range(4)]
        s_ab2 = [[tr.slot("ab%d_%d" % (i, ct)) for ct in range(2)] for i in range(4)]

        def load_x(l, s):
            xsrc = xT_d[s] if l == 0 else xs_dram[s]
            for c in range(8):
                tr.op(SP, lambda e, c=c, xsrc=xsrc: e.dma_start(out=xT[:, c, :], in_=xsrc[c * 128:(c + 1) * 128, :]),
                      reads=["xs%d" % s], writes=[("xT", (c, t)) for t in range(4)], dma=s_x)

        load_x(0, 0)

        R = Region(PH0)
        cstf = R.a([128, NCST], F32)
        slab = [R.a([128, 8, 512], F32) for _ in range(2)]
        tr.op(SP, lambda e: e.dma_start(out=cstf, in_=cst_d), writes=["cstf"], dma=s_misc)
        tr.op(SP, lambda e: e.dma_start(out=cact, in_=cT_d), writes=["cact"], dma=s_misc)
        for l in range(L):
            tr.op(SP, lambda e, l=l: e.dma_start(out=bada[:, l * 48:(l + 1) * 48], in_=bada_d[l]), writes=["bada"], dma=s_misc)
        V(lambda e: e.tensor_copy(identf, cstf[:, 0:128]), ["cstf"], ["identf"])
        V(lambda e: e.tensor_copy(identb, cstf[:, 0:128]), ["cstf"], ["identb"])
        V(lambda e: e.memset(onesb, 1.0), [], ["onesb"])
        V(lambda e: e.memset(BIG[:, 0:384], 0.0), [], ["BIG"])
        V(lambda e: e.tensor_copy(BIG[:, 384:512], cstf[:, 128:256]), ["cstf"], ["BIG"])
        V(lambda e: e.memset(BIG[:, 512:1024], 1.0), [], ["BIG"])
        V(lambda e: e.tensor_copy(jrow, cstf[:, 520:776]), ["cstf"], ["jrow"])
        V(lambda e: e.tensor_copy(iota1, cstf[:, 256:512]), ["cstf"], ["iota1"])
        V(lambda e: e.tensor_copy(ccols, cstf[:, 512:520]), ["cstf"], ["ccols"])
        V(lambda e: e.memset(onesf, 1.0), [], ["onesf"])
        A(lambda e: e.activation(cact, cact, AF.Silu), ["cact"], ["cact"])
        def emit_mod(l, slab, sbs=range(12)):
            wv = wada_d[l].rearrange("(c p) n -> p c n", p=128)
            for sb in sbs:
                bi = sb % 2
                tr.op(SP, lambda e, bi=bi, wv=wv, sb=sb: e.dma_start(out=slab[bi], in_=wv[:, :, sb * 512:(sb + 1) * 512]),
                      writes=["slab%d" % bi], dma=s_ada[bi])
                b = nb()
                for j in range(4):
                    for kc in range(8):
                        T(lambda e, b=b, bi=bi, j=j, kc=kc: e.matmul(
                            ps[b][:, j * n_seq:(j + 1) * n_seq], slab[bi][:, kc, j * 128:(j + 1) * 128],
                            cact[:, kc * n_seq:(kc + 1) * n_seq], start=(kc == 0), stop=(kc == 7)),
                          ["slab%d" % bi, "cact"], [pk(b)])
                for s in range(n_seq):
                    base = l * 48 * n_seq
                    mview = modc[:, base:base + 48 * n_seq].rearrange("p (j s) -> p j s", s=n_seq)
                    pview = ps[b][:, 0:4 * n_seq].rearrange("p (j s) -> p j s", s=n_seq)
                    V(lambda e, mview=mview, pview=pview, s=s, sb=sb, l=l: e.tensor_tensor(
                        mview[:, sb * 4:(sb + 1) * 4, s], pview[:, :, s], bada[:, l * 48 + sb * 4:l * 48 + (sb + 1) * 4], ALU.add),
                      [pk(b), "bada"], ["modc"])

        emit_mod(0, slab)
        tr.barrier()

        def rms_a(tc, sq_b):
            xs = xT[:, :, tc * 512:(tc + 1) * 512]
            xk = [("xT", (c, tc)) for c in range(8)]
            A(lambda e: e.activation(sq_b, xs, AF.Square), xk, ["sq_b"])

        def rms_chunk(tc, acols, shcols, sq_b, hT_b, xn, rstd, tag, hkey="hT", do_a=True):
            if do_a:
                rms_a(tc, sq_b)
            b = nb()
            for c in range(8):
                T(lambda e, c=c, b=b: e.matmul(ps[b][:, :], onesb, sq_b[:, c, :], start=(c == 0), stop=(c == 7)),
                  ["sq_b", "onesb"], [pk(b)])
            A(lambda e, b=b: e.activation(rstd, ps[b][:, :], AF.Sqrt, bias=epsc, scale=1.0 / D), [pk(b), "ccols"], [tag + "rstd"])
            V(lambda e: e.reciprocal(rstd, rstd), [tag + "rstd"], [tag + "rstd"])
            for c in range(8):
                xi = xn[c % 2]
                V(lambda e, c=c, xi=xi: e.scalar_tensor_tensor(xi, xT[:, c, tc * 512:(tc + 1) * 512], acols[:, c:c + 1], rstd, ALU.mult, ALU.mult),
                  [("xT", (c, tc)), "mc", tag + "rstd"], ["xn%d" % (c % 2)])
                A(lambda e, c=c, xi=xi: e.activation(hT_b[:, c, :], xi, AF.Identity, bias=shcols[:, c:c + 1], scale=1.0),
                  ["xn%d" % (c % 2), "mc"], [(hkey, c)])

        def rstd_from(bank, out, width, tag, npart=128):
            A(lambda e: e.activation(out[0:npart], ps[bank][0:npart, :], AF.Sqrt, bias=epsc[0:npart], scale=1.0 / width), [pk(bank), "ccols"], [tag])
            V(lambda e: e.reciprocal(out[0:npart], out[0:npart]), [tag], [tag])

        RA = Region(PH0)
        cqn_b = RA.a([128, 2, S_LEN], BF16)
        catC = RA.a([128, 4, S_LEN], BF16)
        QT0 = RA.off
        ckvn_b = RA.a([128, S_LEN], BF16)
        kr_f = RA.a([128, S_LEN], F32)
        krs_f = RA.a([128, S_LEN], F32)
        YP0 = RA.off
        ypad = RA.a([128, 4, 2080], BF16)
        PB0 = RA.off
        RY = Region(YP0)
        cos2 = RY.a([128, S_LEN], F32)
        sin2 = RY.a([128, S_LEN], F32)
        assert RY.off <= PB0

        def main_body():
            chk("setup")
            for l in range(L):
                tr.barrier()
                tr.op(SP, lambda e, l=l: e.dma_start(out=vecs, in_=vec_d[l]), writes=["vecs"], dma=s_misc)
                for s in range(n_seq):
                    tr.barrier()
                    if s == 0 and l > 0:
                        load_x(l, 0)
                    mc = mcs[s]
                    pos1tm = pos1tm_s[s]
                    G4 = G4_s[s]
                    mbase = l * 48 * n_seq
                    mv = modc[:, mbase:mbase + 48 * n_seq].rearrange("p (j s) -> p j s", s=n_seq)
                    V(lambda e, mv=mv, s=s: e.scalar_tensor_tensor(mc[:, 0:8], mv[:, 8:16, s], 1.0, vecs[:, 0:8], ALU.add, ALU.mult), ["modc", "vecs"], ["mc"])
                    V(lambda e, mv=mv, s=s: e.tensor_copy(mc[:, 8:16], mv[:, 0:8, s]), ["modc"], ["mc"])
                    V(lambda e, mv=mv, s=s: e.tensor_copy(mc[:, 16:24], mv[:, 16:24, s]), ["modc"], ["mc"])
                    V(lambda e, mv=mv, s=s: e.scalar_tensor_tensor(mc[:, 24:32], mv[:, 32:40, s], 1.0, vecs[:, 8:16], ALU.add, ALU.mult), ["modc", "vecs"], ["mc"])
                    V(lambda e, mv=mv, s=s: e.tensor_copy(mc[:, 32:40], mv[:, 24:32, s]), ["modc"], ["mc"])
                    V(lambda e, mv=mv, s=s: e.tensor_copy(mc[:, 40:48], mv[:, 40:48, s]), ["modc"], ["mc"])

                    R = Region(PB0)
                    w_in_b = R.a([128, 8, 1472], BF16)
                    wkrs_b = R.a([128, 8, 64], BF16)
                    sq_b = R.a([128, 8, 512], BF16)
                    hTs = [R.a([128, 8, 512], BF16) for _ in range(2)]
                    xn = [R.a([128, 512], F32) for _ in range(2)]
                    rstd = R.a([128, 512], F32)
                    sig = [R.a([128, 512], F32) for _ in range(2)]
                    cq_f = R.a([128, 2, 512], F32)
                    sqc = R.a([128, 3, 512], BF16)
                    ckv_f = R.a([128, 512], F32)
                    rs2_ = R.a([128, 512], F32)
                    rs2 = [rs2_, rs2_]
                    wv = win_d[l].rearrange("(c p) n -> p c n", p=128)
                    for q in range(4):
                        tr.op(POOL, lambda e, q=q, wv=wv: e.dma_start(out=w_in_b[:, 2 * q:2 * q + 2, :], in_=wv[:, 2 * q:2 * q + 2, :]),
                              writes=[("w_in", q)], dma=s_tw[q])
                    G(lambda e: e.tensor_copy(wkrs_b[:, :, 0:32], w_in_b[:, :, 416:448]), ["w_in"], ["wkrs"])
                    G(lambda e: e.tensor_copy(wkrs_b[:, :, 32:64], w_in_b[:, :, 384:416]), ["w_in"], ["wkrs"])
                    G(lambda e: e.memset(ypad[:, :, 0:16], 0.0), [], ["ypad"])
                    G(lambda e: e.memset(ypad[:, :, 2064:2080], 0.0), [], ["ypad"])
                    rms_chunk(0, mc[:, 0:8], mc[:, 8:16], sq_b, hTs[0], xn, rstd, "t1", hkey="hT0")
                    for tc in range(4):
                        tsl = slice(tc * 512, (tc + 1) * 512)
                        hT_b = hTs[tc % 2]
                        hk = "hT%d" % (tc % 2)
                        if tc + 1 < 4:
                            rms_a(tc + 1, sq_b)

                        def proj(b, lhs_fn, mrows=128):
                            for c in range(8):
                                T(lambda e, c=c: e.matmul(ps[b][0:mrows, :], lhs_fn(c), hT_b[:, c, :], start=(c == 0), stop=(c == 7)),
                                  [(hk, c), "w_in", "wkrs"], [pk(b)])
                        for i in range(3):
                            b = nb()
                            proj(b, lambda c, i=i: w_in_b[:, c, i * 128:(i + 1) * 128])
                            dst = cq_f[:, i, :] if i < 2 else ckv_f
                            A(lambda e, b=b, dst=dst: e.activation(dst, ps[b][:, :], AF.Copy), [pk(b)], [("cqf", i)])
                            A(lambda e, b=b, i=i: e.activation(sqc[:, i, :], ps[b][:, :], AF.Square), [pk(b)], [("sqc", i)])
                        b = nb()
                        for i in range(2):
                            T(lambda e, i=i, b=b: e.matmul(ps[b][:, :], onesb, sqc[:, i, :], start=(i == 0), stop=(i == 1)), [("sqc", i)], [pk(b)])
                        rstd_from(b, rs2[0], 256.0, "rs2")
                        for i in range(2):
                            V(lambda e, i=i: e.scalar_tensor_tensor(cqn_b[:, i, tsl], cq_f[:, i, :], vecs[:, 16 + i:17 + i], rs2[0], ALU.mult, ALU.mult),
                              [("cqf", i), "vecs", "rs2"], [("cqn", tc)])
                        b = nb()
                        T(lambda e, b=b: e.matmul(ps[b][:, :], onesb, sqc[:, 2, :], start=True, stop=True), [("sqc", 2)], [pk(b)])
                        rstd_from(b, rs2[1], 128.0, "rs2")
                        V(lambda e: e.scalar_tensor_tensor(ckvn_b[:, tsl], ckv_f, vecs[:, 18:19], rs2[1], ALU.mult, ALU.mult),
                          [("cqf", 2), "vecs", "rs2"], [("ckvn", tc)])
                        if tc + 1 < 4:
                            rms_chunk(tc + 1, mc[:, 0:8], mc[:, 8:16], sq_b, hTs[(tc + 1) % 2], xn, rstd, "t1", hkey="hT%d" % ((tc + 1) % 2), do_a=False)
                        b = nb()
                        proj(b, lambda c: w_in_b[:, c, 384:448], 64)
                        A(lambda e, b=b: e.activation(kr_f[0:64, tsl], ps[b][0:64, :], AF.Copy), [pk(b)], [("krf", tc)])
                        b = nb()
                        proj(b, lambda c: wkrs_b[:, c, :], 64)
                        A(lambda e, b=b: e.activation(krs_f[0:64, tsl], ps[b][0:64, :], AF.Copy), [pk(b)], [("krsf", tc)])
                        for cc in range(4):
                            bg = nb()
                            proj(bg, lambda c, cc=cc: w_in_b[:, c, 960 + cc * 128:960 + (cc + 1) * 128])
                            ba = nb()
                            proj(ba, lambda c, cc=cc: w_in_b[:, c, 448 + cc * 128:448 + (cc + 1) * 128])
                            sg = sig[cc % 2]
                            A(lambda e, bg=bg, sg=sg: e.activation(sg, ps[bg][:, :], AF.Sigmoid), [pk(bg)], ["sig%d" % (cc % 2)])
                            V(lambda e, ba=ba, sg=sg, cc=cc: e.tensor_tensor(ypad[:, cc, 16 + tc * 512:16 + (tc + 1) * 512], ps[ba][:, :], sg, ALU.mult),
                              [pk(ba), "sig%d" % (cc % 2)], [("ypad", (cc, tc))])

                    chk("T1")
                    tr.barrier()
                    R = Region(PB0)
                    dg = R.a([128, 4, 31, 128], BF16)
                    yc = R.a([128, 4, 512], F32)
                    ycb = R.a([128, 4, 512], BF16)
                    sqy = R.a([128, 4, 512], BF16)
                    mean = R.a([128, 512], F32)
                    var = R.a([128, 512], F32)
                    msq = R.a([128, 512], F32)
                    tt = [R.a([128, 512], F32) for _ in range(2)]
                    for cc in range(4):
                        for j in range(31):
                            if j % 2 == 0:
                                V(lambda e, cc=cc, j=j: e.tensor_scalar(dg[:, cc, j, :], identf, vecs[:, 37 + cc * 31 + j:38 + cc * 31 + j], None, ALU.mult),
                                  ["identf", "vecs"], [("dg", cc)])
                            else:
                                A(lambda e, cc=cc, j=j: e.activation(dg[:, cc, j, :], identf, AF.Copy, scale=vecs[:, 37 + cc * 31 + j:38 + cc * 31 + j]),
                                  ["identf", "vecs"], [("dg", cc)])
                    for tc in range(4):
                        tsl = slice(tc * 512, (tc + 1) * 512)
                        for cc in range(4):
                            b = nb()
                            for j in range(31):
                                T(lambda e, b=b, cc=cc, j=j: e.matmul(ps[b][:, :], dg[:, cc, j, :], ypad[:, cc, tc * 512 + j + 1:tc * 512 + j + 513],
                                                                      start=(j == 0), stop=(j == 30)),
                                  [("dg", cc), "ypad"], [pk(b)])
                            A(lambda e, b=b, cc=cc: e.activation(yc[:, cc, :], ps[b][:, :], AF.Identity, bias=vecs[:, 25 + cc:26 + cc], scale=1.0),
                              [pk(b), "vecs"], [("yc", cc)])
                            A(lambda e, b=b, cc=cc: e.activation(sqy[:, cc, :], ps[b][:, :], AF.Square, bias=vecs[:, 25 + cc:26 + cc], scale=1.0),
                              [pk(b), "vecs"], [("sqy", cc)])
                            V(lambda e, cc=cc: e.tensor_copy(ycb[:, cc, :], yc[:, cc, :]), [("yc", cc)], [("ycb", cc)])
                        b1 = nb()
                        for cc in range(4):
                            T(lambda e, cc=cc, b1=b1: e.matmul(ps[b1][:, :], onesb, ycb[:, cc, :], start=(cc == 0), stop=(cc == 3)), [("ycb", cc)], [pk(b1)])
                        b2 = nb()
                        for cc in range(4):
                            T(lambda e, cc=cc, b2=b2: e.matmul(ps[b2][:, :], onesb, sqy[:, cc, :], start=(cc == 0), stop=(cc == 3)), [("sqy", cc)], [pk(b2)])
                        A(lambda e, b1=b1: e.activation(mean, ps[b1][:, :], AF.Copy, scale=1.0 / 512), [pk(b1)], ["mean"])
                        V(lambda e: e.tensor_tensor(msq, mean, mean, ALU.mult), ["mean"], ["msq"])
                        V(lambda e, b2=b2: e.scalar_tensor_tensor(var, ps[b2][:, :], 1.0 / 512, msq, ALU.mult, ALU.subtract), [pk(b2), "msq"], ["var"])
                        A(lambda e: e.activation(var, var, AF.Sqrt, bias=epsc, scale=1.0), ["var", "ccols"], ["var"])
                        V(lambda e: e.reciprocal(var, var), ["var"], ["var"])
                        for cc in range(4):
                            t = tt[cc % 2]
                            V(lambda e, cc=cc, t=t: e.tensor_tensor(t, yc[:, cc, :], mean, ALU.subtract), [("yc", cc), "mean"], ["tt%d" % (cc % 2)])
                            V(lambda e, t=t, cc=cc: e.tensor_tensor(t, t, var, ALU.mult), ["tt%d" % (cc % 2), "var"], ["tt%d" % (cc % 2)])
                            A(lambda e, cc=cc, t=t: e.activation(catC[:, cc, tsl], t, AF.Silu, bias=vecs[:, 33 + cc:34 + cc], scale=vecs[:, 29 + cc:30 + cc]),
                              ["tt%d" % (cc % 2), "vecs"], [("catC", tc)])

                    chk("T2")
                    tr.barrier()
                    R = Region(PB0)
                    w_uq_b = R.a([128, 2, 768], BF16)
                    w_uqs = R.a([128, 2, 256], BF16)
                    w_ukv_b = R.a([128, 1024], BF16)
                    w_out_b = R.a([128, 8, 1024], BF16)
                    KnT = R.a([128, 4, S_LEN], BF16)
                    KrT = R.a([128, S_LEN], BF16)
                    Vb = R.a([128, 16, 512], BF16)
                    scl = R.a([128, 64], F32)
                    sskr = R.a([128, 16], F32)
                    ssk = R.a([128, 16], F32)
                    KT0 = R.off
                    sqk = [R.a([128, 512], BF16) for _ in range(2)]
                    kt1 = R.a([128, 512], F32)
                    kt2 = R.a([128, 512], F32)
                    sqkr = R.a([128, 512], BF16)
                    tr.op(POOL, lambda e, l=l: e.dma_start(out=w_uq_b, in_=wuq_d[l].rearrange("(c p) n -> p c n", p=128)), writes=["w_uq"], dma=s_tw[0])
                    tr.op(POOL, lambda e, l=l: e.dma_start(out=w_ukv_b, in_=wukv_d[l]), writes=["w_ukv"], dma=s_tw[1])
                    wv = wout_d[l].rearrange("(c p) n -> p c n", p=128)
                    for q in range(2):
                        tr.op(POOL, lambda e, q=q, wv=wv: e.dma_start(out=w_out_b[:, 4 * q:4 * q + 4, :], in_=wv[:, 4 * q:4 * q + 4, :]),
                              writes=[("w_out", q)], dma=s_tw[2 + q])
                    for h in range(NH):
                        G(lambda e, h=h: e.tensor_copy(w_uqs[:, :, h * 64:h * 64 + 32], w_uq_b[:, :, h * 192 + 160:h * 192 + 192]), ["w_uq"], ["w_uqs"])
                        G(lambda e, h=h: e.tensor_copy(w_uqs[:, :, h * 64 + 32:h * 64 + 64], w_uq_b[:, :, h * 192 + 128:h * 192 + 160]), ["w_uq"], ["w_uqs"])
                    RT = Region(PB0 + (768 + 256 + 512 + 4096))
                    pos_i = RT.a([128, 1024], I32)
                    ang = RT.a([128, 1024], F32)
                    kf = RT.a([128, 1024], F32)
                    ki = RT.a([128, 1024], I32)
                    C1 = 6.28125
                    C2 = 2.0 * math.pi - 6.28125
                    for hf in range(2):
                        hs = slice(hf * 1024, (hf + 1) * 1024)
                        tr.op(SP, lambda e, s=s, hs=hs: e.dma_start(out=pos_i[0:64, :], in_=pos_d[s, :, hs]), writes=["pos_i"], dma=s_misc)
                        V(lambda e: e.tensor_copy(ang[0:64], pos_i[0:64]), ["pos_i"], ["ang"])
                        V(lambda e: e.tensor_scalar(ang[0:64], ang[0:64], ccols[0:64, 2:3], None, ALU.mult), ["ang", "ccols"], ["ang"])
                        V(lambda e: e.tensor_scalar(kf[0:64], ang[0:64], 1.0 / (2.0 * math.pi), None, ALU.mult), ["ang"], ["kf"])
                        V(lambda e: e.tensor_copy(ki[0:64], kf[0:64]), ["kf"], ["ki"])
                        V(lambda e: e.tensor_copy(kf[0:64], ki[0:64]), ["ki"], ["kf"])
                        V(lambda e: e.scalar_tensor_tensor(ang[0:64], kf[0:64], -C1, ang[0:64], ALU.mult, ALU.add), ["kf", "ang"], ["ang"])
                        V(lambda e: e.scalar_tensor_tensor(ang[0:64], kf[0:64], -C2, ang[0:64], ALU.mult, ALU.add), ["kf", "ang"], ["ang"])
                        V(lambda e: e.tensor_scalar(ang[0:64], ang[0:64], 3.1415925, -3.1415925, ALU.min, ALU.max), ["ang"], ["ang"])
                        A(lambda e, hs=hs: e.activation(sin2[0:64, hs], ang[0:64], AF.Sin, scale=ccols[0:64, 3:4]), ["ang", "ccols"], ["sin2"])
                        V(lambda e: e.scalar_tensor_tensor(kf[0:64], ang[0:64], -1.0, ang[0:64], ALU.mult, ALU.max), ["ang"], ["kf"])
                        V(lambda e: e.tensor_scalar(kf[0:64], kf[0:64], -1.0, math.pi / 2, ALU.mult, ALU.add), ["kf"], ["kf"])
                        A(lambda e, hs=hs: e.activation(cos2[0:64, hs], kf[0:64], AF.Sin), ["kf"], ["cos2"])
                    tr.barrier()
                    chk("tab")
                    wv3 = w_ukv_b.rearrange("p (h c) -> p h c", h=4)
                    for j in range(16):
                        b = nb()
                        T(lambda e, b=b, j=j: e.matmul(ps[b][:, :], ckvn_b[:, j * 128:(j + 1) * 128], wv3[:, :, 128:256], start=True, stop=True),
                          ["ckvn", "w_ukv"], [pk(b)])
                        if j % 2 == 0:
                            A(lambda e, b=b, j=j: e.activation(Vb[:, j, :], ps[b][:, :], AF.Copy), [pk(b)], [("Vb", j)])
                        else:
                            V(lambda e, b=b, j=j: e.tensor_copy(Vb[:, j, :], ps[b][:, :]), [pk(b)], [("Vb", j)])
                    bS = nb()
                    for tc in range(4):
                        tsl = slice(tc * 512, (tc + 1) * 512)
                        V(lambda e, tsl=tsl: e.scalar_tensor_tensor(kt1[0:64], kr_f[0:64, tsl], vecs[0:64, 23:24], cos2[0:64, tsl], ALU.mult, ALU.mult),
                          ["krf", "vecs", "cos2"], ["kt1"])
                        V(lambda e, tsl=tsl: e.scalar_tensor_tensor(kt2[0:64], krs_f[0:64, tsl], vecs[0:64, 24:25], sin2[0:64, tsl], ALU.mult, ALU.mult),
                          ["krsf", "vecs", "sin2"], ["kt2"])
                        V(lambda e, tsl=tsl: e.tensor_tensor(KrT[0:64, tsl], kt1[0:64], kt2[0:64], ALU.add), ["kt1", "kt2"], [("KrT", tc)])
                        A(lambda e, tsl=tsl: e.activation(sqkr[0:64], kr_f[0:64, tsl], AF.Square), ["krf"], ["sqkr"])
                        for j in range(4):
                            T(lambda e, tc=tc, j=j: e.matmul(ps[bS][:, tc * 4 + j:tc * 4 + j + 1], sqkr[0:64, j * 128:(j + 1) * 128], onesb[0:64, 0:1],
                                                           start=True, stop=True), ["sqkr", "onesb"], [pk(bS)])
                    V(lambda e: e.tensor_copy(sskr, ps[bS][:, 0:16]), [pk(bS)], ["sskr"])
                    for h in range(NH):
                        bS = nb()
                        for tc in range(4):
                            tsl = slice(tc * 512, (tc + 1) * 512)
                            b = nb()
                            T(lambda e, b=b, h=h, tsl=tsl: e.matmul(ps[b][:, :], w_ukv_b[:, h * 256:h * 256 + 128], ckvn_b[:, tsl], start=True, stop=True),
                              ["ckvn", "w_ukv"], [pk(b)])
                            A(lambda e, b=b, h=h, tsl=tsl: e.activation(KnT[:, h, tsl], ps[b][:, :], AF.Copy, scale=vecs[:, 22:23]), [pk(b), "vecs"], [("KnT", h)])
                            sq = sqk[tc % 2]
                            A(lambda e, b=b, sq=sq: e.activation(sq, ps[b][:, :], AF.Square), [pk(b)], ["sqk%d" % (tc % 2)])
                            for j in range(4):
                                T(lambda e, tc=tc, j=j, sq=sq, bS=bS: e.matmul(ps[bS][:, tc * 4 + j:tc * 4 + j + 1], sq[:, j * 128:(j + 1) * 128], onesb[:, 0:1],
                                                                               start=True, stop=True), ["sqk%d" % (tc % 2), "onesb"], [pk(bS)])
                        V(lambda e, bS=bS: e.tensor_tensor(ssk, ps[bS][:, 0:16], sskr, ALU.add), [pk(bS), "sskr"], ["ssk"])
                        A(lambda e: e.activation(ssk, ssk, AF.Sqrt, bias=epsc, scale=1.0 / 192), ["ssk", "ccols"], ["ssk"])
                        V(lambda e: e.reciprocal(ssk, ssk), ["ssk"], ["ssk"])
                        V(lambda e, h=h: e.tensor_scalar(scl[:, h * 16:(h + 1) * 16], ssk, 1.0 / math.sqrt(192.0), None, ALU.mult), ["ssk"], [("scl", h)])
                    tr.barrier()
                    chk("kprep")
                    RQ = Region(QT0)
                    catA = RQ.a([128, 4, 512], BF16)
                    QnT = [RQ.a([128, 512], BF16) for _ in range(2)]
                    QrT = [RQ.a([128, 512], BF16) for _ in range(2)]
                    PT = [RQ.a([128, 512], BF16) for _ in range(3)]
                    sqn = RQ.a([128, 512], BF16)
                    sqr = RQ.a([128, 512], BF16)
                    rD = RQ.a([128, 512], F32)
                    assert RQ.off <= YP0
                    RK = Region(KT0)
                    rq = RK.a([128, 512], F32)
                    qt1 = RK.a([128, 512], F32)
                    qt2 = RK.a([128, 512], F32)
                    assert RK.off <= TOTW
                    SB = (0, 1, 2)
                    QB = (3, 4, 5)
                    bO, bD = 6, 7
                    pt_state = {"n": 0}

                    def q_prep(qc, h, qb):
                        qsl = slice(qc * 512, (qc + 1) * 512)
                        bqn, bqr, bqs = QB
                        for k in range(2):
                            T(lambda e, k=k: e.matmul(ps[bqn][:, :], w_uq_b[:, k, h * 192:h * 192 + 128], cqn_b[:, k, qsl], start=(k == 0), stop=(k == 1)),
                              ["w_uq", "cqn"], [pk(bqn)])
                        for k in range(2):
                            T(lambda e, k=k: e.matmul(ps[bqr][0:64, :], w_uq_b[:, k, h * 192 + 128:h * 192 + 192], cqn_b[:, k, qsl], start=(k == 0), stop=(k == 1)),
                              ["w_uq", "cqn"], [pk(bqr)])
                        for k in range(2):
                            T(lambda e, k=k: e.matmul(ps[bqs][0:64, :], w_uqs[:, k, h * 64:(h + 1) * 64], cqn_b[:, k, qsl], start=(k == 0), stop=(k == 1)),
                              ["w_uqs", "cqn"], [pk(bqs)])
                        A(lambda e: e.activation(sqn, ps[bqn][:, :], AF.Square), [pk(bqn)], ["sqn"])
                        A(lambda e: e.activation(sqr[0:64], ps[bqr][0:64, :], AF.Square), [pk(bqr)], ["sqr"])
                        bss = nb(SB)
                        T(lambda e: e.matmul(ps[bss][:, :], onesb, sqn, start=True, stop=False), ["sqn", "onesb"], [pk(bss)])
                        T(lambda e: e.matmul(ps[bss][:, :], onesb[0:64, :], sqr[0:64], start=False, stop=True), ["sqr", "onesb"], [pk(bss)])
                        rstd_from(bss, rq, 192.0, "rq")
                        V(lambda e: e.scalar_tensor_tensor(QnT[qb], ps[bqn][:, :], vecs[:, 19:20], rq, ALU.mult, ALU.mult), [pk(bqn), "vecs", "rq"], ["QnT%d" % qb])
                        V(lambda e: e.scalar_tensor_tensor(qt1[0:64], ps[bqr][0:64, :], vecs[0:64, 20:21], cos2[0:64, qsl], ALU.mult, ALU.mult),
                          [pk(bqr), "vecs", "cos2"], ["qt1"])
                        V(lambda e: e.scalar_tensor_tensor(qt2[0:64], ps[bqs][0:64, :], vecs[0:64, 21:22], sin2[0:64, qsl], ALU.mult, ALU.mult),
                          [pk(bqs), "vecs", "sin2"], ["qt2"])
                        V(lambda e: e.tensor_tensor(qt1[0:64], qt1[0:64], qt2[0:64], ALU.add), ["qt1", "qt2"], ["qt1"])
                        V(lambda e: e.tensor_tensor(QrT[qb][0:64], qt1[0:64], rq[0:64], ALU.mult), ["qt1", "rq"], ["QrT%d" % qb])

                    def core(qc, h, qb, hook):
                        def S_mm(kt):
                            b = nb(SB)
                            ksl = slice(kt * 128, (kt + 1) * 128)
                            T(lambda e: e.matmul(ps[b][:, :], KnT[:, h, ksl], QnT[qb], start=True, stop=False), [("KnT", h), "QnT%d" % qb], [pk(b)])
                            T(lambda e: e.matmul(ps[b][:, :], KrT[0:64, ksl], QrT[qb][0:64], start=False, stop=True), ["KrT", "QrT%d" % qb], [pk(b)])
                            return b
                        sb_cur = S_mm(0)
                        for kt in range(16):
                            sb_next = S_mm(kt + 1) if kt < 15 else None
                            pi = pt_state["n"] % 3
                            pt_state["n"] += 1
                            p_t = PT[pi]
                            A(lambda e: e.activation(p_t, ps[sb_cur][:, :], AF.Exp, scale=scl[:, h * 16 + kt:h * 16 + kt + 1]),
                              [pk(sb_cur), ("scl", h)], ["PT%d" % pi])
                            T(lambda e: e.matmul(ps[bO][:, :], Vb[:, kt, h * 128:(h + 1) * 128], p_t, start=(kt == 0), stop=(kt == 15)),
                              ["PT%d" % pi, ("Vb", kt)], [pk(bO)])
                            T(lambda e: e.matmul(ps[bD][:, :], onesb, p_t, start=(kt == 0), stop=(kt == 15)),
                              ["PT%d" % pi, "onesb"], [pk(bD)])
                            sb_cur = sb_next
                            if kt == 3:
                                hook()
                        V(lambda e: e.reciprocal(rD, ps[bD][:, :]), [pk(bD)], ["rD"])
                        V(lambda e: e.tensor_tensor(catA[:, h, :], ps[bO][:, :], rD, ALU.mult), [pk(bO), "rD"], [("catA", h)])

                    pairs = [(qc, h) for qc in range(4) for h in range(NH)]
                    q_prep(0, 0, 0)
                    for i, (qc, h) in enumerate(pairs):
                        qsl = slice(qc * 512, (qc + 1) * 512)
                        if i + 1 < len(pairs):
                            nqc, nh = pairs[i + 1]
                            hook = (lambda nqc=nqc, nh=nh, i=i: q_prep(nqc, nh, (i + 1) % 2))
                        else:
                            hook = (lambda: None)
                        core(qc, h, i % 2, hook)
                        if h == NH - 1:
                            for m in range(8):
                                b = nb(SB)
                                for k in range(8):
                                    rhs = catA[:, k, :] if k < 4 else catC[:, k - 4, qsl]
                                    rk = ("catA", k) if k < 4 else ("catC", qc)
                                    T(lambda e, b=b, k=k, m=m, rhs=rhs: e.matmul(ps[b][:, :], w_out_b[:, k, m * 128:(m + 1) * 128], rhs, start=(k == 0), stop=(k == 7)),
                                      ["w_out", rk], [pk(b)])
                                V(lambda e, b=b, m=m: e.scalar_tensor_tensor(xT[:, m, qsl], ps[b][:, :], mc[:, 16 + m:17 + m], xT[:, m, qsl], ALU.mult, ALU.add),
                                  [pk(b), "mc", ("xT", (m, qc))], [("xT", (m, qc))])

                    chk("T3")
                    tr.barrier()
                    M = Region(PH0)
                    zer = M.a([128, 1024], F32)
                    h2st = [M.a([128, 1024], BF16) for _ in range(4)]
                    M1 = M
                    sq_b = M1.a([128, 8, 512], BF16)
                    h2Ts = [M1.a([128, 8, 512], BF16) for _ in range(2)]
                    xn = [M1.a([128, 512], F32) for _ in range(2)]
                    rstd2s = [M1.a([128, 512], F32) for _ in range(2)]
                    affT = M1.a([128, S_LEN], F32)
                    wr_f = M1.a([128, 8, NE], F32)
                    awr = M1.a([128, 8, NE], F32)
                    lg = M1.a([128, 512], F32)
                    rden = M1.a([128, 512], F32)
                    cstc = M1.a([128, 1], F32)
                    m8 = M1.a([128, 8], F32)
                    masktm_b = M1.a([128, 256], BF16)
                    masktm_f = M1.a([128, 256], F32)
                    afftm = M1.a([128, 256], F32)
                    hi_f = M1.a([128, 256], F32)
                    maskT = M1.a([128, S_LEN], BF16)
                    ET = M1.a([128, S_LEN], F32)
                    work = ET
                    mod_slab = [M1.a([128, 8, 512], F32) for _ in range(2)]

                    tr.op(SP, lambda e, l=l: e.dma_start(out=wr_f, in_=wr_d[l].rearrange("p (c n) -> p c n", c=8)), writes=["wr_f"], dma=s_misc)
                    V(lambda e: e.memset(zer, 0.0), [], ["zer"])
                    for c in range(8):
                        V(lambda e, c=c: e.tensor_scalar(awr[:, c, :], wr_f[:, c, :], mc[:, 24 + c:25 + c], None, ALU.mult), ["wr_f", "mc"], ["awr"])
                    b = nb()
                    for c in range(8):
                        T(lambda e, c=c, b=b: e.matmul(ps[b][0:16, 0:1], wr_f[:, c, :], mc[:, 32 + c:33 + c], start=(c == 0), stop=(c == 7)), ["wr_f", "mc"], [pk(b)])
                    V(lambda e, b=b: e.tensor_copy(cstc[0:16], ps[b][0:16, 0:1]), [pk(b)], ["cstc"])
                    h2v = h2_dram[s].rearrange("(j p) d -> p j d", p=128)
                    rms_chunk(0, mc[:, 24:32], mc[:, 32:40], sq_b, h2Ts[0], xn, rstd2s[0], "m1p0", hkey="hTm0")
                    for tc in range(4):
                        tsl = slice(tc * 512, (tc + 1) * 512)
                        h2T = h2Ts[tc % 2]
                        rstd2 = rstd2s[tc % 2]
                        hkm = "hTm%d" % (tc % 2)
                        if tc + 1 < 4:
                            rms_a(tc + 1, sq_b)
                        for j4 in range(4):
                            jt = tc * 4 + j4
                            hst = h2st[jt % 4]
                            for hf in range(2):
                                b = nb()
                                for c4 in range(4):
                                    c = hf * 4 + c4
                                    T(lambda e, b=b, c=c, c4=c4, j4=j4: e.matmul(ps[b][:, c4 * 128:(c4 + 1) * 128], h2T[:, c, j4 * 128:(j4 + 1) * 128], identb,
                                                                              start=True, stop=True), [(hkm, c), "identb"], [pk(b)])
                                dst = hst[:, hf * 512:(hf + 1) * 512]
                                if (j4 + hf) % 2 == 0:
                                    A(lambda e, b=b, dst=dst: e.activation(dst, ps[b][:, :], AF.Copy), [pk(b)], [("h2st", jt % 4)])
                                else:
                                    V(lambda e, b=b, dst=dst: e.tensor_copy(dst, ps[b][:, :]), [pk(b)], [("h2st", jt % 4)])
                            tr.op(SP, lambda e, jt=jt, hst=hst: e.dma_start(out=h2v[:, jt, :], in_=hst), reads=[("h2st", jt % 4)], writes=["h2d%d" % s], dma=s_h2)
                        b = nb()
                        for c in range(8):
                            T(lambda e, b=b, c=c, tsl=tsl: e.matmul(ps[b][0:16, :], awr[:, c, :], xT[:, c, tsl], start=(c == 0), stop=(c == 7)),
                              ["awr", ("xT", (c, tc))], [pk(b)])
                        if tc + 1 < 4:
                            rms_chunk(tc + 1, mc[:, 24:32], mc[:, 32:40], sq_b, h2Ts[(tc + 1) % 2], xn, rstd2s[(tc + 1) % 2], "m1p%d" % ((tc + 1) % 2),
                                      hkey="hTm%d" % ((tc + 1) % 2), do_a=False)
                        V(lambda e, b=b: e.tensor_tensor(lg[0:16], ps[b][0:16, :], rstd2[0:16], ALU.mult), [pk(b), "m1p%drstd" % (tc % 2)], ["lg"])
                        A(lambda e, tsl=tsl: e.activation(ET[0:16, tsl], lg[0:16], AF.Exp, bias=cstc[0:16], scale=1.0), ["lg", "cstc"], [("ET", tc)])
                        b = nb()
                        T(lambda e, b=b, tsl=tsl: e.matmul(ps[b][0:16, :], onesf[0:16, 0:16], ET[0:16, tsl], start=True, stop=True), [("ET", tc), "onesf"], [pk(b)])
                        V(lambda e, b=b: e.reciprocal(rden[0:16], ps[b][0:16, :]), [pk(b)], ["rden"])
                        V(lambda e, tsl=tsl: e.tensor_tensor(affT[0:16, tsl], ET[0:16, tsl], rden[0:16], ALU.mult), [("ET", tc), "rden"], [("affT", tc)])
                        if l + 1 < L:
                            per = (12 + n_seq - 1) // n_seq
                            mine = list(range(s * per, min(12, (s + 1) * per)))
                            q4 = (len(mine) + 3) // 4
                            emit_mod(l + 1, mod_slab, mine[tc * q4:(tc + 1) * q4])
                    for c in range(8):
                        tr.op(SP, lambda e, s=s, c=c: e.dma_start(out=xs_dram[s, c * 128:(c + 1) * 128, :], in_=xT[:, c, :]),
                              reads=[("xT", (c, t)) for t in range(4)], writes=["xs%d" % s], dma=s_sp)
                    if s + 1 < n_seq:
                        load_x(l, s + 1)
                    for ct in range(2):
                        accv = acc_dram[s][ct].rearrange("(j p) d -> p j d", p=128)
                        for j in range(16):
                            tr.op(SP, lambda e, j=j, accv=accv: e.dma_start(out=accv[:, j, :], in_=zer), reads=["zer"], writes=["accd%d%d" % (s, ct)], dma=s_zero)
                    ba = nb()
                    for j in range(16):
                        jsl = slice(j * 128, (j + 1) * 128)
                        T(lambda e, j=j, jsl=jsl: e.matmul(ps[ba][:, j * 16:(j + 1) * 16], affT[0:16, jsl], identf[0:16, 0:16], start=True, stop=True),
                          ["affT", "identf"], [pk(ba)])
                    V(lambda e: e.tensor_copy(afftm, ps[ba][:, 0:256]), [pk(ba)], ["afftm"])
                    lo16 = M1.a([128, 16], F32)
                    mid16 = M1.a([128, 16], F32)
                    cnt16 = M1.a([128, 16], F32)
                    ge16 = M1.a([128, 16], F32)
                    a3 = afftm.rearrange("p (j e) -> p j e", e=16)
                    m3 = masktm_b.rearrange("p (j e) -> p j e", e=16)
                    V(lambda e: e.memset(lo16, 0.0), [], ["lo16"])
                    NBIS = 30
                    for k in range(NBIS):
                        wk = 2.0 ** (-(k + 1))
                        V(lambda e, wk=wk: e.tensor_scalar(mid16, lo16, wk, None, ALU.add), ["lo16"], ["mid16"])
                        midb = mid16.rearrange("p (o e) -> p o e", o=1).to_broadcast([128, 16, 16])
                        V(lambda e, midb=midb: e.tensor_tensor(m3, a3, midb, ALU.is_ge), ["afftm", "mid16"], ["masktm_b"])
                        bc = nb()
                        T(lambda e, bc=bc: e.matmul(ps[bc][:, 0:256], onesb, masktm_b, start=True, stop=True), ["masktm_b", "onesb"], [pk(bc)])
                        pv = ps[bc][:, 0:256].rearrange("p (j e) -> p e j", e=16)
                        V(lambda e, pv=pv: e.tensor_reduce(out=cnt16, in_=pv, axis=mybir.AxisListType.X, op=ALU.add), [pk(bc)], ["cnt16"])
                        V(lambda e, wk=wk: e.tensor_scalar(ge16, cnt16, float(CAP) - 0.5, wk, ALU.is_ge, ALU.mult), ["cnt16"], ["ge16"])
                        V(lambda e: e.tensor_tensor(lo16, lo16, ge16, ALU.add), ["lo16", "ge16"], ["lo16"])
                    lob = lo16.rearrange("p (o e) -> p o e", o=1).to_broadcast([128, 16, 16])
                    mf3 = masktm_f.rearrange("p (j e) -> p j e", e=16)
                    V(lambda e: e.tensor_tensor(mf3, a3, lob, ALU.is_ge), ["afftm", "lo16"], ["masktm_f"])
                    V(lambda e: e.tensor_copy(masktm_b, masktm_f), ["masktm_f"], ["masktm_b"])
                    V(lambda e: e.tensor_copy(G4[:, :, 0], afftm), ["afftm"], ["G4%d" % s])
                    V(lambda e: e.tensor_copy(hi_f, G4[:, :, 0]), ["G4%d" % s], ["hi_f"])
                    V(lambda e: e.tensor_tensor(G4[:, :, 1], afftm, hi_f, ALU.subtract), ["afftm", "hi_f"], ["G4%d" % s])
                    V(lambda e: e.tensor_scalar(G4[:, :, 2], jrow, 0.0, ccols[:, 5:6], ALU.mult, ALU.add), ["jrow", "ccols"], ["G4%d" % s])
                    V(lambda e: e.tensor_copy(G4[:, :, 3], jrow), ["jrow"], ["G4%d" % s])
                    bp = nb()
                    for j in range(16):
                        for i in range(j):
                            T(lambda e, i=i, j=j: e.matmul(ps[bp][:, j * 16:(j + 1) * 16], onesb, masktm_b[:, i * 16:(i + 1) * 16], start=(i == 0), stop=False),
                              ["masktm_b", "onesb"], [pk(bp)])
                        T(lambda e, j=j: e.matmul(ps[bp][:, j * 16:(j + 1) * 16], BIG[:, 384:512], masktm_b[:, j * 16:(j + 1) * 16], start=(j == 0), stop=True),
                          ["masktm_b", "BIG"], [pk(bp)])
                    V(lambda e: e.scalar_tensor_tensor(pos1tm, ps[bp][:, 0:256], 1.0, masktm_f, ALU.add, ALU.mult), [pk(bp), "masktm_f"], ["pos1tm%d" % s])

                    chk("M1")

                tr.barrier()
                NSL = 256 * n_seq
                EA = Region(0)
                yef = [[EA.a([128, 1024], F32) for _ in range(2 * n_seq)] for _ in range(2)]
                xe_tm = [[EA.a([128, 2, 1024], BF16) for _ in range(n_seq)] for _ in range(2)]
                xeT = [EA.a([128, 8, NSL], BF16) for _ in range(2)]
                assert EA.off <= 16384, EA.off
                EB = Region(PH0)
                NSLOT = 12
                wsl = [EB.a([128, 4, 1024], BF16) for _ in range(NSLOT)]
                Sel = [EB.a([128, 16, 256], BF16) for _ in range(n_seq)]
                hidT = EB.a([128, 8, NSL], BF16)
                sgt = [EB.a([128, NSL], F32) for _ in range(2)]
                gcol = [[EB.a([128, 2], F32) for _ in range(n_seq)] for _ in range(3)]
                idxi = [[EB.a([128, 2], I32) for _ in range(n_seq)] for _ in range(3)]
                gtmp = EB.a([128, 8], F32)
                idxf = EB.a([128, 2], F32)

                wl = []
                for ex in range(NE):
                    wl += [(wg_d, ex, 0), (wg_d, ex, 1), (wu_d, ex, 0), (wu_d, ex, 1), (wd_d, ex, 0), (wd_d, ex, 1)]
                wstate = {"n": 0}

                def issue_w(upto):
                    while wstate["n"] < min(upto, len(wl)):
                        k = wstate["n"]
                        src, ex, hf = wl[k]
                        sl = k % NSLOT
                        sv = src[l, ex].rearrange("(c p) f -> p c f", p=128)[:, hf * 4:(hf + 1) * 4, :]
                        tr.op(POOL, lambda e, sl=sl, sv=sv: e.dma_start(out=wsl[sl], in_=sv), writes=["wsl%d" % sl], dma=s_w[sl])
                        wstate["n"] += 1

                issue_w(NSLOT - 1)
                wix = {"i": 0}

                def prep_sel(ex):
                    for s in range(n_seq):
                        for j in range(16):
                            V(lambda e, j=j, ex=ex, s=s: e.tensor_scalar(Sel[s][:, j, :], iota1, pos1tm_s[s][:, j * 16 + ex:j * 16 + ex + 1], None, ALU.is_equal),
                              ["iota1", "pos1tm%d" % s], [("Sel%d" % s, j)])

                def prep_idx(ex):
                    pb = ex % 3
                    for s in range(n_seq):
                        b = nb()
                        for ct in range(2):
                            for j in range(16):
                                T(lambda e, b=b, ct=ct, j=j, ex=ex, s=s: e.matmul(ps[b][:, ct * 4:ct * 4 + 4], Sel[s][:, j, ct * 128:(ct + 1) * 128], G4_s[s][:, j * 16 + ex, :],
                                                                                 start=(j == 0), stop=(j == 15)), [("Sel%d" % s, j), "G4%d" % s], [pk(b)])
                        V(lambda e, b=b: e.tensor_copy(gtmp, ps[b][:, 0:8]), [pk(b)], ["gtmp"])
                        g3 = gtmp.rearrange("p (a b) -> p a b", b=4)
                        V(lambda e, g3=g3, pb=pb, s=s: e.tensor_tensor(gcol[pb][s], g3[:, :, 0], g3[:, :, 1], ALU.add), ["gtmp"], ["gcol%d%d" % (pb, s)])
                        V(lambda e, g3=g3: e.scalar_tensor_tensor(idxf, g3[:, :, 3], 128.0, g3[:, :, 2], ALU.mult, ALU.add), ["gtmp"], ["idxf"])
                        V(lambda e, pb=pb, s=s: e.tensor_copy(idxi[pb][s], idxf), ["idxf"], ["idxi%d%d" % (pb, s)])

                def gather(ex):
                    pb, p3 = ex % 2, ex % 3
                    for s in range(n_seq):
                        for ct in range(2):
                            tr.op(POOL, lambda e, ct=ct, pb=pb, p3=p3, s=s: e.indirect_dma_start(
                                out=xe_tm[pb][s][:, ct, :], out_offset=None, in_=h2_dram[s],
                                in_offset=bass.IndirectOffsetOnAxis(ap=idxi[p3][s][:, ct:ct + 1], axis=0)),
                                reads=["h2d%d" % s, "idxi%d%d" % (p3, s)], writes=[("xe_tm%d%d" % (pb, s), ct)], dma=s_g[pb])

                def prep_tr(ex):
                    pb = ex % 2
                    for s in range(n_seq):
                        for cp in range(4):
                            b = nb()
                            for c in (2 * cp, 2 * cp + 1):
                                for ct in range(2):
                                    T(lambda e, b=b, c=c, ct=ct, pb=pb, s=s: e.matmul(ps[b][:, (c % 2) * 256 + ct * 128:(c % 2) * 256 + (ct + 1) * 128],
                                                                                   xe_tm[pb][s][:, ct, c * 128:(c + 1) * 128], identb, start=True, stop=True),
                                      [("xe_tm%d%d" % (pb, s), ct), "identb"], [pk(b)])
                            dst = xeT[pb][:, 2 * cp:2 * cp + 2, s * 256:(s + 1) * 256]
                            if cp % 2 == 0:
                                A(lambda e, b=b, dst=dst: e.activation(dst, ps[b][:, :].rearrange("p (a b) -> p a b", a=2), AF.Copy), [pk(b)], [("xeT%d" % pb, cp)])
                            else:
                                V(lambda e, b=b, dst=dst: e.tensor_copy(dst, ps[b][:, :].rearrange("p (a b) -> p a b", a=2)), [pk(b)], [("xeT%d" % pb, cp)])

                def ffn_gu(ex):
                    pb = ex % 2
                    widx = wix["i"]
                    issue_w(widx + NSLOT)
                    for fc in range(8):
                        bg = nb()
                        bu = nb()
                        for (w0, b) in ((widx, bg), (widx + 2, bu)):
                            for c in range(8):
                                wsel = (w0 + c // 4) % NSLOT
                                T(lambda e, b=b, wsel=wsel, c=c, fc=fc, pb=pb: e.matmul(ps[b][:, 0:NSL], wsl[wsel][:, c % 4, fc * 128:(fc + 1) * 128], xeT[pb][:, c, :],
                                                                                     start=(c == 0), stop=(c == 7)), ["wsl%d" % wsel, ("xeT%d" % pb, c // 2)], [pk(b)])
                        st_ = sgt[fc % 2]
                        A(lambda e, bg=bg, st_=st_: e.activation(st_, ps[bg][:, 0:NSL], AF.Silu), [pk(bg)], ["sgt%d" % (fc % 2)])
                        V(lambda e, bu=bu, st_=st_, fc=fc: e.tensor_tensor(hidT[:, fc, :], ps[bu][:, 0:NSL], st_, ALU.mult), [pk(bu), "sgt%d" % (fc % 2)], [("hidT", fc)])
                    wix["i"] += 4

                def ffn_down(ex):
                    pb = ex % 2
                    p3 = ex % 3
                    widx = wix["i"]
                    issue_w(widx + NSLOT)
                    for nd in range(2):
                        for q in range(2 * n_seq):
                            s, ct = q // 2, q % 2
                            b = nb()
                            for fc in range(8):
                                sd_ = (widx + fc // 4) % NSLOT
                                T(lambda e, b=b, fc=fc, q=q, sd_=sd_, nd=nd: e.matmul(ps[b][:, :], hidT[:, fc, q * 128:(q + 1) * 128], wsl[sd_][:, fc % 4, nd * 512:(nd + 1) * 512],
                                                                                   start=(fc == 0), stop=(fc == 7)),
                                  [("hidT", fc), "wsl%d" % sd_], [pk(b)])
                            A(lambda e, b=b, pb=pb, p3=p3, q=q, nd=nd, s=s, ct=ct: e.activation(yef[pb][q][:, nd * 512:(nd + 1) * 512], ps[b][:, :], AF.Copy, scale=gcol[p3][s][:, ct:ct + 1]),
                              [pk(b), "gcol%d%d" % (p3, s)], ["yef%d%d" % (pb, q)])
                    wix["i"] += 2
                    for q in range(2 * n_seq):
                        s, ct = q // 2, q % 2
                        tr.op(POOL, lambda e, ct=ct, pb=pb, p3=p3, s=s, q=q: e.indirect_dma_start(
                            out=acc_dram[s][ct], out_offset=bass.IndirectOffsetOnAxis(ap=idxi[p3][s][:, ct:ct + 1], axis=0),
                            in_=yef[pb][q], in_offset=None, bounds_check="BCREG", oob_is_err=True, compute_op=ALU.add),
                            reads=["yef%d%d" % (pb, q), "idxi%d%d" % (p3, s), "accd%d%d" % (s, ct)], writes=["accd%d%d" % (s, ct)], dma=s_acc[s][ct])

                prep_sel(0)
                prep_idx(0)
                gather(0)
                prep_sel(1)
                prep_idx(1)
                prep_tr(0)
                for ex in range(NE):
                    if ex + 1 < NE:
                        gather(ex + 1)
                    if ex + 2 < NE:
                        prep_sel(ex + 2)
                    ffn_gu(ex)
                    if ex + 2 < NE:
                        prep_idx(ex + 2)
                    ffn_down(ex)
                    if ex + 1 < NE:
                        prep_tr(ex + 1)
                tr.barrier()
                EC = Region(0)
                xch = [EC.a([128, 8, 512], F32) for _ in range(2)]
                accb = [[EC.a([128, 1024], F32) for _ in range(2)] for _ in range(4)]
                assert EC.off <= 16384
                kx = 0
                for s in range(n_seq):
                    accv = [acc_dram[s][ct].rearrange("(j p) d -> p j d", p=128) for ct in range(2)]
                    xsv = xs_dram[s].rearrange("(c p) t -> p c t", p=128)
                    dstv = (out_d[s] if l == L - 1 else xs_dram[s]).rearrange("(c p) t -> p c t", p=128)
                    for tc in range(4):
                        xc = xch[kx % 2]
                        xk = "xch%d" % (kx % 2)
                        tr.op(SP, lambda e, xc=xc, xsv=xsv, tc=tc: e.dma_start(out=xc, in_=xsv[:, :, tc * 512:(tc + 1) * 512]),
                              reads=[("xs%d" % s, tc)], writes=[xk], dma=s_xc[kx % 2])
                        for j4 in range(4):
                            j = tc * 4 + j4
                            ab = accb[j % 4]
                            for ct in range(2):
                                tr.op(SP if ct == 0 else ACT, lambda e, j=j, ab=ab, accv=accv, ct=ct: e.dma_start(out=ab[ct], in_=accv[ct][:, j, :]), reads=["accd%d%d" % (s, ct)],
                                      writes=["accb%d_%d" % (j % 4, ct)], dma=s_ab2[j % 4][ct])
                            V(lambda e, ab=ab: e.tensor_tensor(ab[0], ab[0], ab[1], ALU.add), ["accb%d_0" % (j % 4), "accb%d_1" % (j % 4)], ["accb%d_0" % (j % 4)])
                            for hf in range(2):
                                b = nb()
                                for c4 in range(4):
                                    c = hf * 4 + c4
                                    T(lambda e, b=b, c=c, c4=c4, ab=ab: e.matmul(ps[b][:, c4 * 128:(c4 + 1) * 128], ab[0][:, c * 128:(c + 1) * 128], identf, start=True, stop=True),
                                      ["accb%d_0" % (j % 4), "identf"], [pk(b)])
                                for c4 in range(4):
                                    c = hf * 4 + c4
                                    V(lambda e, b=b, c=c, c4=c4, j4=j4, xc=xc, s=s: e.scalar_tensor_tensor(xc[:, c, j4 * 128:(j4 + 1) * 128], ps[b][:, c4 * 128:(c4 + 1) * 128], mcs[s][:, 40 + c:41 + c],
                                                                                                      xc[:, c, j4 * 128:(j4 + 1) * 128], ALU.mult, ALU.add),
                                      [pk(b), "mc", xk], [xk])
                        tr.op(POOL, lambda e, xc=xc, dstv=dstv, tc=tc: e.dma_start(out=dstv[:, :, tc * 512:(tc + 1) * 512], in_=xc),
                              reads=[xk], writes=[("xs%d" % s, tc)], dma=s_out)
                        kx += 1
        try:
            main_body()
        except _Stop:
            pass
        tr.emit(nc)
    return nc


def _consts():
    c = np.zeros((128, NCST), np.float32)
    c[:, 0:128] = np.eye(128, dtype=np.float32)
    tp = np.arange(128)
    c[:, 128:256] = (tp[:, None] < tp[None, :]).astype(np.float32)
    c[:, 256:512] = np.arange(1, 257, dtype=np.float32)[None, :]
    c[:, 512] = tp + 1
    c[:, 513] = tp + 129
    inv_freq = (1.0 / (np.float32(10000.0) ** (np.arange(0, 64, 2, dtype=np.float32) / np.float32(64)))).astype(np.float32)
    c[0:64, 514] = np.concatenate([inv_freq, inv_freq])
    c[0:32, 515] = -1.0
    c[32:64, 515] = 1.0
    c[:, 516] = EPS
    c[:, 517] = tp
    c[:, 520:776] = (np.arange(256) // 16).astype(np.float32)[None, :]
    return c


def _col(v, nch):
    return np.ascontiguousarray(np.asarray(v, np.float32).reshape(nch, 128).T)


def _pack_layer_inputs(inp, L):
    vec = np.zeros((L, 128, NV), np.float32)
    for l in range(L):
        v = vec[l]
        v[:, 0:8] = _col(inp["norm1_g"][l], 8)
        v[:, 8:16] = _col(inp["norm2_g"][l], 8)
        v[:, 16:18] = _col(inp["q_latent_g"][l], 2)
        v[:, 18:19] = _col(inp["kv_latent_g"][l], 1)
        qg = np.asarray(inp["q_head_g"][l], np.float32)
        kg = np.asarray(inp["k_head_g"][l], np.float32)
        v[:, 19] = qg[0:128]
        v[0:64, 20] = qg[128:192]
        v[0:64, 21] = np.concatenate([qg[160:192], qg[128:160]])
        v[:, 22] = kg[0:128]
        v[0:64, 23] = kg[128:192]
        v[0:64, 24] = np.concatenate([kg[160:192], kg[128:160]])
        v[:, 25:29] = _col(inp["conv_b"][l], 4)
        v[:, 29:33] = _col(inp["conv_norm_g"][l], 4)
        v[:, 33:37] = _col(inp["conv_norm_b"][l], 4)
        cw = np.asarray(inp["conv_w"][l], np.float32)
        for cc in range(4):
            v[:, 37 + cc * 31:37 + (cc + 1) * 31] = cw[:, cc * 128:(cc + 1) * 128].T
    bada = np.stack([_col(inp["b_ada"][l], 48) for l in range(L)])
    wr = np.stack([np.ascontiguousarray(np.asarray(inp["w_router"][l], np.float32).reshape(8, 128, NE).transpose(1, 0, 2)).reshape(128, 8 * NE)
                   for l in range(L)])
    return vec, bada, wr


def make_in_maps(inp, n_cores, n_seq, L, batch_ids=None):
    f32 = lambda a: np.ascontiguousarray(np.asarray(a, np.float32))
    vec, bada, wr = _pack_layer_inputs(inp, L)
    cst = _consts()
    shared = {
        "cst": cst, "vec": vec, "bada": bada, "w_router": wr,
        "w_ada": f32(inp["w_ada"][:L]), "w_in": f32(inp["w_in"][:L]), "w_uq": f32(inp["w_uq"][:L]),
        "w_ukv": f32(inp["w_ukv"][:L]), "w_out": f32(inp["w_out"][:L]),
        "w_gate": f32(inp["w_gate"][:L]), "w_up": f32(inp["w_up"][:L]), "w_down": f32(inp["w_down"][:L]),
    }
    x = np.asarray(inp["x"], np.float32)
    c = np.asarray(inp["c"], np.float32)
    pos = np.asarray(inp["positions"], np.int32)
    maps = []
    for core in range(n_cores):
        ids = batch_ids[core] if batch_ids is not None else list(range(core * n_seq, (core + 1) * n_seq))
        xT = np.ascontiguousarray(np.stack([x[b].T for b in ids]))
        cT = np.zeros((128, 8 * n_seq), np.float32)
        for si, b in enumerate(ids):
            cT.reshape(128, 8, n_seq)[:, :, si] = c[b].reshape(8, 128).T
        posr = np.ascontiguousarray(np.stack([np.broadcast_to(pos[b][None, :], (64, S_LEN)) for b in ids])).astype(np.int32)
        m = dict(shared)
        m.update({"xT": xT, "cT": cT, "posr": posr})
        maps.append(m)
    return maps


_NC_CACHE = {}


def kernel(**inputs):
    n_cores, n_seq, L = 8, 2, 2
    key = (n_seq, L)
    if key not in _NC_CACHE:
        _NC_CACHE[key] = build_program(n_seq, L)
    nc = _NC_CACHE[key]
    maps = make_in_maps(inputs, n_cores, n_seq, L)
    res = run_bass_kernel_spmd(nc, maps, core_ids=list(range(n_cores)))
    out = np.empty((n_cores * n_seq, S_LEN, D), np.float32)
    for core in range(n_cores):
        oT = res.results[core]["outT"]
        for si in range(n_seq):
            out[core * n_seq + si] = oT[si].T
    return out
```

```python
import contextlib
import math
import numpy as np
import concourse.bass as bass
import concourse.mybir as mybir
from concourse.bass_utils import run_bass_kernel_spmd

F32 = mybir.dt.float32
BF16 = mybir.dt.bfloat16
I32 = mybir.dt.int32
ALU = mybir.AluOpType
AF = mybir.ActivationFunctionType

PE, ACT, DVE, POOL, SP = "pe", "act", "dve", "pool", "sp"
ENGS = [PE, ACT, DVE, POOL, SP]

S_LEN = 2048
D = 1024
NH = 4
NE = 16
CAP = 256
EPS = 1e-6
NV = 161
NCST = 520 + 256


class _Op:
    __slots__ = ("eng", "idx", "fn", "waits", "sig", "sigval", "dma", "clock", "dclock")


class DmaSlot:
    def __init__(self, name):
        self.name = name
        self.count = 0
        self.sem = None
        self.last_op = None


class _Rec:
    def __getattr__(self, name):
        return lambda *a, **k: (name, a, k)


_REC = _Rec()


class Tracker:
    def __init__(self):
        self.ops = {e: [] for e in ENGS}
        self.state = {}
        self.clock = {e: {} for e in ENGS}
        self.dclock = {e: {} for e in ENGS}
        self.pending = {e: [] for e in ENGS}
        self.slots = []

    def slot(self, name):
        s = DmaSlot(name)
        self.slots.append(s)
        return s

    def _collect(self, key, is_write, deps):
        buf, sub = key if isinstance(key, tuple) else (key, None)
        st = self.state.setdefault(buf, {})
        if sub is None:
            ents = list(st.values())
        else:
            ents = [st[k] for k in (sub, None) if k in st]
        for ent in ents:
            if ent[0] is not None:
                deps.append(ent[0])
            if is_write:
                deps.extend(ent[1].values())
                deps.extend(ent[2])

    def _update(self, key, is_write, tok):
        buf, sub = key if isinstance(key, tuple) else (key, None)
        st = self.state.setdefault(buf, {})
        if is_write:
            if sub is None:
                st.clear()
                st[None] = [tok, {}, []]
            else:
                st[sub] = [tok, {}, []]
        else:
            ent = st.setdefault(sub, [None, {}, []])
            if tok[0] == "op":
                ent[1][tok[1]] = tok
            else:
                ent[2].append(tok)
                if len(ent[2]) > 8:
                    ent[2] = ent[2][-8:]

    def op(self, eng, fn, reads=(), writes=(), dma=None):
        o = _Op()
        o.eng = eng
        o.idx = len(self.ops[eng])
        o.fn = fn(_REC)
        o.sig = False
        o.sigval = None
        o.dma = None
        deps = list(self.pending[eng])
        self.pending[eng] = []
        for k in reads:
            self._collect(k, False, deps)
        for k in writes:
            self._collect(k, True, deps)
        clock = self.clock[eng]
        dclock = self.dclock[eng]
        waits = []
        for d in deps:
            if d[0] == "op":
                _, e2, i2 = d
                if e2 == eng:
                    if eng in (PE, SP):
                        continue
                    if i2 < o.idx - 3:
                        continue
                    if clock.get(e2, -1) >= i2:
                        continue
                    src = self.ops[e2][i2]
                    src.sig = True
                    waits.append(("op", src))
                    clock[e2] = i2
                    continue
                if clock.get(e2, -1) >= i2:
                    continue
                src = self.ops[e2][i2]
                src.sig = True
                waits.append(("op", src))
                clock[e2] = i2
                for k, v in src.clock.items():
                    if k != eng and clock.get(k, -1) < v:
                        clock[k] = v
                for k, v in src.dclock.items():
                    if dclock.get(k, -1) < v:
                        dclock[k] = v
            else:
                _, slot, val, src = d
                val = slot.count
                src = slot.last_op
                if dclock.get(slot, -1) >= val:
                    continue
                waits.append(("dma", slot, val))
                dclock[slot] = val
                for k, v in src.clock.items():
                    if k != eng and clock.get(k, -1) < v:
                        clock[k] = v
                for k, v in src.dclock.items():
                    if dclock.get(k, -1) < v:
                        dclock[k] = v
        o.waits = waits
        o.clock = dict(clock)
        o.dclock = dict(dclock)
        if dma is not None:
            dma.count += 16
            o.dma = (dma, dma.count)
            dma.last_op = o
            tok = ("dma", dma, dma.count, o)
        else:
            tok = ("op", eng, o.idx)
        self.ops[eng].append(o)
        for k in reads:
            self._update(k, False, tok)
        for k in writes:
            self._update(k, True, tok)
        return o

    def barrier(self):
        toks = []
        for e in (PE, ACT, DVE, POOL):
            for o in reversed(self.ops[e]):
                if o.dma is None:
                    toks.append(("op", e, o.idx))
                    break
        for s in self.slots:
            if s.last_op is not None:
                toks.append(("dma", s, s.count, s.last_op))
        for e in ENGS:
            self.pending[e] = list(toks)

    def emit(self, nc):
        stack = contextlib.ExitStack()
        with stack:
            esem = {}
            for e in (PE, ACT, DVE, POOL):
                esem[e] = stack.enter_context(nc.semaphore("s_" + e))
            for i, s in enumerate(self.slots):
                if s.count > 0:
                    s.sem = stack.enter_context(nc.semaphore("d%d_%s" % (i, s.name)))
            for e in (PE, ACT, DVE, POOL):
                c = 0
                for o in self.ops[e]:
                    if o.sig:
                        c += 1
                        o.sigval = c
            block = stack.enter_context(nc.Block())

            def run(engobj, e):
                bcreg = None
                if e == POOL and any(o.fn[2].get("bounds_check") == "BCREG" for o in self.ops[e]):
                    bcreg = engobj.alloc_register("bcreg")
                    engobj.reg_mov(bcreg, S_LEN - 1)
                for o in self.ops[e]:
                    for w in o.waits:
                        if w[0] == "op":
                            engobj.wait_ge(esem[w[1].eng], w[1].sigval)
                        else:
                            engobj.wait_ge(w[1].sem, w[2])
                    name, a_, k_ = o.fn
                    if k_.get("bounds_check") == "BCREG":
                        k_ = dict(k_)
                        k_["bounds_check"] = bcreg
                    ins = getattr(engobj, name)(*a_, **k_)
                    if o.dma is not None:
                        ins.then_inc(o.dma[0].sem, 16)
                    elif o.sig:
                        ins.then_inc(esem[e], 1)

            @block.tensor
            def _(t):
                run(t, PE)

            @block.scalar
            def _(a):
                run(a, ACT)

            @block.vector
            def _(v):
                run(v, DVE)

            @block.gpsimd
            def _(g):
                run(g, POOL)

            @block.sync
            def _(s):
                run(s, SP)
                for sl in self.slots:
                    if sl.count > 0:
                        s.wait_ge(sl.sem, sl.count)


LAY = []


class _Stop(Exception):
    pass


def build_program(n_seq=2, n_layers=2, stop=None):
    import inspect
    del LAY[:]
    nc = bass.Bass("TRN2", target_bir_lowering=False)
    L = n_layers
    xT_d = nc.dram_tensor("xT", [n_seq, D, S_LEN], F32, kind="ExternalInput").ap()
    out_d = nc.dram_tensor("outT", [n_seq, D, S_LEN], F32, kind="ExternalOutput").ap()
    cT_d = nc.dram_tensor("cT", [128, 8 * n_seq], F32, kind="ExternalInput").ap()
    pos_d = nc.dram_tensor("posr", [n_seq, 64, S_LEN], I32, kind="ExternalInput").ap()
    cst_d = nc.dram_tensor("cst", [128, NCST], F32, kind="ExternalInput").ap()
    vec_d = nc.dram_tensor("vec", [L, 128, NV], F32, kind="ExternalInput").ap()
    bada_d = nc.dram_tensor("bada", [L, 128, 48], F32, kind="ExternalInput").ap()
    wada_d = nc.dram_tensor("w_ada", [L, D, 6 * D], F32, kind="ExternalInput").ap()
    win_d = nc.dram_tensor("w_in", [L, D, 1472], F32, kind="ExternalInput").ap()
    wuq_d = nc.dram_tensor("w_uq", [L, 256, 768], F32, kind="ExternalInput").ap()
    wukv_d = nc.dram_tensor("w_ukv", [L, 128, 1024], F32, kind="ExternalInput").ap()
    wout_d = nc.dram_tensor("w_out", [L, D, D], F32, kind="ExternalInput").ap()
    wr_d = nc.dram_tensor("w_router", [L, 128, 8 * NE], F32, kind="ExternalInput").ap()
    wg_d = nc.dram_tensor("w_gate", [L, NE, D, D], F32, kind="ExternalInput").ap()
    wu_d = nc.dram_tensor("w_up", [L, NE, D, D], F32, kind="ExternalInput").ap()
    wd_d = nc.dram_tensor("w_down", [L, NE, D, D], F32, kind="ExternalInput").ap()

    h2_dram = [nc.dram_tensor("h2s%d" % i, [S_LEN, D], BF16, kind="Internal").ap() for i in range(n_seq)]
    acc_dram = [[nc.dram_tensor("accs%d_%d" % (i, ct), [S_LEN, D], F32, kind="Internal").ap() for ct in range(2)] for i in range(n_seq)]
    xs_dram = nc.dram_tensor("xss", [n_seq, D, S_LEN], F32, kind="Internal").ap()
    tr = Tracker()
    TOTW = 53000
    dump_d = nc.dram_tensor("dump", [128, TOTW], F32, kind="ExternalOutput").ap() if stop is not None else None
    stack = contextlib.ExitStack()
    with stack:
        arena = stack.enter_context(nc.sbuf_tensor("arena", [128, TOTW], F32))
        ps = [stack.enter_context(nc.psum_tensor("ps%d" % i, [128, 512], F32)) for i in range(8)]

        def view(off, shape, dt):
            n = int(np.prod(shape[1:]))
            nb = n * (2 if dt == BF16 else 4)
            nw = (nb + 3) // 4
            assert off + nw <= TOTW, (off, nw)
            if stop is not None:
                LAY.append((inspect.stack()[2].lineno, off, tuple(shape), "bf16" if dt == BF16 else ("i32" if dt == I32 else "f32")))
            v = arena[:, off:off + nw]
            if dt != F32:
                v = v.bitcast(dt)
            if len(shape) == 3:
                v = v.rearrange("p (a b) -> p a b", a=shape[1])
            elif len(shape) == 4:
                v = v.rearrange("p (a b c) -> p a b c", a=shape[1], b=shape[2])
            return v, off + nw

        class Region:
            def __init__(self, start):
                self.off = start

            def a(self, shape, dt):
                v, self.off = view(self.off, shape, dt)
                return v

        def chk(name):
            if stop == name:
                tr.barrier()
                tr.op(SP, lambda e: e.dma_start(out=dump_d, in_=arena[:, :]), dma=s_out)
                raise _Stop()

        V = lambda fn, r=(), w=(): tr.op(DVE, fn, r, w)
        A = lambda fn, r=(), w=(): tr.op(ACT, fn, r, w)
        G = lambda fn, r=(), w=(): tr.op(POOL, fn, r, w)
        T = lambda fn, r=(), w=(): tr.op(PE, fn, r, w)
        bank_ctr = [0]

        def nb(pool=(0, 1, 2, 3, 4, 5, 6, 7)):
            b = pool[bank_ctr[0] % len(pool)]
            bank_ctr[0] += 1
            return b

        def pk(b):
            return "ps%d" % b

        P = Region(0)
        xT = P.a([128, 8, S_LEN], F32)
        identb = P.a([128, 128], BF16)
        onesb = P.a([128, 128], BF16)
        BIG = P.a([128, 1024], BF16)
        jrow = P.a([128, 256], F32)
        identf = P.a([128, 128], F32)
        iota1 = P.a([128, 256], F32)
        ccols = P.a([128, 8], F32)
        onesf = P.a([128, 16], F32)
        modc = P.a([128, L * 48 * n_seq], F32)
        mcs = [P.a([128, 48], F32) for _ in range(n_seq)]
        pos1tm_s = [P.a([128, 256], F32) for _ in range(n_seq)]
        G4_s = [P.a([128, 256, 4], BF16) for _ in range(n_seq)]
        vecs = P.a([128, NV], F32)
        cact = P.a([128, 8 * n_seq], F32)
        bada = P.a([128, L * 48], F32)
        PH0 = P.off
        epsc = ccols[:, 4:5]

        s_x = tr.slot("x")
        s_out = tr.slot("out")
        s_misc = tr.slot("misc")
        s_ada = [tr.slot("ada0"), tr.slot("ada1")]
        s_w = [tr.slot("w%d" % i) for i in range(12)]
        s_tw = [tr.slot("tw%d" % i) for i in range(4)]
        s_g = [tr.slot("g0"), tr.slot("g1")]
        s_acc = [[tr.slot("acc%d_%d" % (i, ct)) for ct in range(2)] for i in range(n_seq)]
        s_zero = tr.slot("zero")
        s_sp = tr.slot("sp")
        s_xc = [tr.slot("xc0"), tr.slot("xc1")]
        s_h2 = tr.slot("h2")
        s_ab = [tr.slot("ab%d" % i) for i in range(4)]
        s_ab2 = [[tr.slot("ab%d_%d" % (i, ct)) for ct in range(2)] for i in range(4)]

        def load_x(l, s):
            xsrc = xT_d[s] if l == 0 else xs_dram[s]
            for c in range(8):
                tr.op(SP, lambda e, c=c, xsrc=xsrc: e.dma_start(out=xT[:, c, :], in_=xsrc[c * 128:(c + 1) * 128, :]),
                      reads=["xs%d" % s], writes=[("xT", (c, t)) for t in range(4)], dma=s_x)

        load_x(0, 0)

        R = Region(PH0)
        cstf = R.a([128, NCST], F32)
        slab = [R.a([128, 8, 512], F32) for _ in range(2)]
        tr.op(SP, lambda e: e.dma_start(out=cstf, in_=cst_d), writes=["cstf"], dma=s_misc)
        tr.op(SP, lambda e: e.dma_start(out=cact, in_=cT_d), writes=["cact"], dma=s_misc)
        for l in range(L):
            tr.op(SP, lambda e, l=l: e.dma_start(out=bada[:, l * 48:(l + 1) * 48], in_=bada_d[l]), writes=["bada"], dma=s_misc)
        V(lambda e: e.tensor_copy(identf, cstf[:, 0:128]), ["cstf"], ["identf"])
        V(lambda e: e.tensor_copy(identb, cstf[:, 0:128]), ["cstf"], ["identb"])
        V(lambda e: e.memset(onesb, 1.0), [], ["onesb"])
        V(lambda e: e.memset(BIG[:, 0:384], 0.0), [], ["BIG"])
        V(lambda e: e.tensor_copy(BIG[:, 384:512], cstf[:, 128:256]), ["cstf"], ["BIG"])
        V(lambda e: e.memset(BIG[:, 512:1024], 1.0), [], ["BIG"])
        V(lambda e: e.tensor_copy(jrow, cstf[:, 520:776]), ["cstf"], ["jrow"])
        V(lambda e: e.tensor_copy(iota1, cstf[:, 256:512]), ["cstf"], ["iota1"])
        V(lambda e: e.tensor_copy(ccols, cstf[:, 512:520]), ["cstf"], ["ccols"])
        V(lambda e: e.memset(onesf, 1.0), [], ["onesf"])
        A(lambda e: e.activation(cact, cact, AF.Silu), ["cact"], ["cact"])
        def emit_mod(l, slab, sbs=range(12)):
            wv = wada_d[l].rearrange("(c p) n -> p c n", p=128)
            for sb in sbs:
                bi = sb % 2
                tr.op(SP, lambda e, bi=bi, wv=wv, sb=sb: e.dma_start(out=slab[bi], in_=wv[:, :, sb * 512:(sb + 1) * 512]),
                      writes=["slab%d" % bi], dma=s_ada[bi])
                b = nb()
                for j in range(4):
                    for kc in range(8):
                        T(lambda e, b=b, bi=bi, j=j, kc=kc: e.matmul(
                            ps[b][:, j * n_seq:(j + 1) * n_seq], slab[bi][:, kc, j * 128:(j + 1) * 128],
                            cact[:, kc * n_seq:(kc + 1) * n_seq], start=(kc == 0), stop=(kc == 7)),
                          ["slab%d" % bi, "cact"], [pk(b)])
                for s in range(n_seq):
                    base = l * 48 * n_seq
                    mview = modc[:, base:base + 48 * n_seq].rearrange("p (j s) -> p j s", s=n_seq)
                    pview = ps[b][:, 0:4 * n_seq].rearrange("p (j s) -> p j s", s=n_seq)
                    V(lambda e, mview=mview, pview=pview, s=s, sb=sb, l=l: e.tensor_tensor(
                        mview[:, sb * 4:(sb + 1) * 4, s], pview[:, :, s], bada[:, l * 48 + sb * 4:l * 48 + (sb + 1) * 4], ALU.add),
                      [pk(b), "bada"], ["modc"])

        emit_mod(0, slab)
        tr.barrier()

        def rms_a(tc, sq_b):
            xs = xT[:, :, tc * 512:(tc + 1) * 512]
            xk = [("xT", (c, tc)) for c in range(8)]
            A(lambda e: e.activation(sq_b, xs, AF.Square), xk, ["sq_b"])

        def rms_chunk(tc, acols, shcols, sq_b, hT_b, xn, rstd, tag, hkey="hT", do_a=True):
            if do_a:
                rms_a(tc, sq_b)
            b = nb()
            for c in range(8):
                T(lambda e, c=c, b=b: e.matmul(ps[b][:, :], onesb, sq_b[:, c, :], start=(c == 0), stop=(c == 7)),
                  ["sq_b", "onesb"], [pk(b)])
            A(lambda e, b=b: e.activation(rstd, ps[b][:, :], AF.Sqrt, bias=epsc, scale=1.0 / D), [pk(b), "ccols"], [tag + "rstd"])
            V(lambda e: e.reciprocal(rstd, rstd), [tag + "rstd"], [tag + "rstd"])
            for c in range(8):
                xi = xn[c % 2]
                V(lambda e, c=c, xi=xi: e.scalar_tensor_tensor(xi, xT[:, c, tc * 512:(tc + 1) * 512], acols[:, c:c + 1], rstd, ALU.mult, ALU.mult),
                  [("xT", (c, tc)), "mc", tag + "rstd"], ["xn%d" % (c % 2)])
                A(lambda e, c=c, xi=xi: e.activation(hT_b[:, c, :], xi, AF.Identity, bias=shcols[:, c:c + 1], scale=1.0),
                  ["xn%d" % (c % 2), "mc"], [(hkey, c)])

        def rstd_from(bank, out, width, tag, npart=128):
            A(lambda e: e.activation(out[0:npart], ps[bank][0:npart, :], AF.Sqrt, bias=epsc[0:npart], scale=1.0 / width), [pk(bank), "ccols"], [tag])
            V(lambda e: e.reciprocal(out[0:npart], out[0:npart]), [tag], [tag])

        RA = Region(PH0)
        cqn_b = RA.a([128, 2, S_LEN], BF16)
        catC = RA.a([128, 4, S_LEN], BF16)
        QT0 = RA.off
        ckvn_b = RA.a([128, S_LEN], BF16)
        kr_f = RA.a([128, S_LEN], F32)
        krs_f = RA.a([128, S_LEN], F32)
        YP0 = RA.off
        ypad = RA.a([128, 4, 2080], BF16)
        PB0 = RA.off
        RY = Region(YP0)
        cos2 = RY.a([128, S_LEN], F32)
        sin2 = RY.a([128, S_LEN], F32)
        assert RY.off <= PB0

        def main_body():
            chk("setup")
            for l in range(L):
                tr.barrier()
                tr.op(SP, lambda e, l=l: e.dma_start(out=vecs, in_=vec_d[l]), writes=["vecs"], dma=s_misc)
                for s in range(n_seq):
                    tr.barrier()
                    if s == 0 and l > 0:
                        load_x(l, 0)
                    mc = mcs[s]
                    pos1tm = pos1tm_s[s]
                    G4 = G4_s[s]
                    mbase = l * 48 * n_seq
                    mv = modc[:, mbase:mbase + 48 * n_seq].rearrange("p (j s) -> p j s", s=n_seq)
                    V(lambda e, mv=mv, s=s: e.scalar_tensor_tensor(mc[:, 0:8], mv[:, 8:16, s], 1.0, vecs[:, 0:8], ALU.add, ALU.mult), ["modc", "vecs"], ["mc"])
                    V(lambda e, mv=mv, s=s: e.tensor_copy(mc[:, 8:16], mv[:, 0:8, s]), ["modc"], ["mc"])
                    V(lambda e, mv=mv, s=s: e.tensor_copy(mc[:, 16:24], mv[:, 16:24, s]), ["modc"], ["mc"])
                    V(lambda e, mv=mv, s=s: e.scalar_tensor_tensor(mc[:, 24:32], mv[:, 32:40, s], 1.0, vecs[:, 8:16], ALU.add, ALU.mult), ["modc", "vecs"], ["mc"])
                    V(lambda e, mv=mv, s=s: e.tensor_copy(mc[:, 32:40], mv[:, 24:32, s]), ["modc"], ["mc"])
                    V(lambda e, mv=mv, s=s: e.tensor_copy(mc[:, 40:48], mv[:, 40:48, s]), ["modc"], ["mc"])

                    R = Region(PB0)
                    w_in_b = R.a([128, 8, 1472], BF16)
                    wkrs_b = R.a([128, 8, 64], BF16)
                    sq_b = R.a([128, 8, 512], BF16)
                    hTs = [R.a([128, 8, 512], BF16) for _ in range(2)]
                    xn = [R.a([128, 512], F32) for _ in range(2)]
                    rstd = R.a([128, 512], F32)
                    sig = [R.a([128, 512], F32) for _ in range(2)]
                    cq_f = R.a([128, 2, 512], F32)
                    sqc = R.a([128, 3, 512], BF16)
                    ckv_f = R.a([128, 512], F32)
                    rs2_ = R.a([128, 512], F32)
                    rs2 = [rs2_, rs2_]
                    wv = win_d[l].rearrange("(c p) n -> p c n", p=128)
                    for q in range(4):
                        tr.op(POOL, lambda e, q=q, wv=wv: e.dma_start(out=w_in_b[:, 2 * q:2 * q + 2, :], in_=wv[:, 2 * q:2 * q + 2, :]),
                              writes=[("w_in", q)], dma=s_tw[q])
                    G(lambda e: e.tensor_copy(wkrs_b[:, :, 0:32], w_in_b[:, :, 416:448]), ["w_in"], ["wkrs"])
                    G(lambda e: e.tensor_copy(wkrs_b[:, :, 32:64], w_in_b[:, :, 384:416]), ["w_in"], ["wkrs"])
                    G(lambda e: e.memset(ypad[:, :, 0:16], 0.0), [], ["ypad"])
                    G(lambda e: e.memset(ypad[:, :, 2064:2080], 0.0), [], ["ypad"])
                    rms_chunk(0, mc[:, 0:8], mc[:, 8:16], sq_b, hTs[0], xn, rstd, "t1", hkey="hT0")
                    for tc in range(4):
                        tsl = slice(tc * 512, (tc + 1) * 512)
                        hT_b = hTs[tc % 2]
                        hk = "hT%d" % (tc % 2)
                        if tc + 1 < 4:
                            rms_a(tc + 1, sq_b)

                        def proj(b, lhs_fn, mrows=128):
                            for c in range(8):
                                T(lambda e, c=c: e.matmul(ps[b][0:mrows, :], lhs_fn(c), hT_b[:, c, :], start=(c == 0), stop=(c == 7)),
                                  [(hk, c), "w_in", "wkrs"], [pk(b)])
                        for i in range(3):
                            b = nb()
                            proj(b, lambda c, i=i: w_in_b[:, c, i * 128:(i + 1) * 128])
                            dst = cq_f[:, i, :] if i < 2 else ckv_f
                            A(lambda e, b=b, dst=dst: e.activation(dst, ps[b][:, :], AF.Copy), [pk(b)], [("cqf", i)])
                            A(lambda e, b=b, i=i: e.activation(sqc[:, i, :], ps[b][:, :], AF.Square), [pk(b)], [("sqc", i)])
                        b = nb()
                        for i in range(2):
                            T(lambda e, i=i, b=b: e.matmul(ps[b][:, :], onesb, sqc[:, i, :], start=(i == 0), stop=(i == 1)), [("sqc", i)], [pk(b)])
                        rstd_from(b, rs2[0], 256.0, "rs2")
                        for i in range(2):
                            V(lambda e, i=i: e.scalar_tensor_tensor(cqn_b[:, i, tsl], cq_f[:, i, :], vecs[:, 16 + i:17 + i], rs2[0], ALU.mult, ALU.mult),
                              [("cqf", i), "vecs", "rs2"], [("cqn", tc)])
                        b = nb()
                        T(lambda e, b=b: e.matmul(ps[b][:, :], onesb, sqc[:, 2, :], start=True, stop=True), [("sqc", 2)], [pk(b)])
                        rstd_from(b, rs2[1], 128.0, "rs2")
                        V(lambda e: e.scalar_tensor_tensor(ckvn_b[:, tsl], ckv_f, vecs[:, 18:19], rs2[1], ALU.mult, ALU.mult),
                          [("cqf", 2), "vecs", "rs2"], [("ckvn", tc)])
                        if tc + 1 < 4:
                            rms_chunk(tc + 1, mc[:, 0:8], mc[:, 8:16], sq_b, hTs[(tc + 1) % 2], xn, rstd, "t1", hkey="hT%d" % ((tc + 1) % 2), do_a=False)
                        b = nb()
                        proj(b, lambda c: w_in_b[:, c, 384:448], 64)
                        A(lambda e, b=b: e.activation(kr_f[0:64, tsl], ps[b][0:64, :], AF.Copy), [pk(b)], [("krf", tc)])
                        b = nb()
                        proj(b, lambda c: wkrs_b[:, c, :], 64)
                        A(lambda e, b=b: e.activation(krs_f[0:64, tsl], ps[b][0:64, :], AF.Copy), [pk(b)], [("krsf", tc)])
                        for cc in range(4):
                            bg = nb()
                            proj(bg, lambda c, cc=cc: w_in_b[:, c, 960 + cc * 128:960 + (cc + 1) * 128])
                            ba = nb()
                            proj(ba, lambda c, cc=cc: w_in_b[:, c, 448 + cc * 128:448 + (cc + 1) * 128])
                            sg = sig[cc % 2]
                            A(lambda e, bg=bg, sg=sg: e.activation(sg, ps[bg][:, :], AF.Sigmoid), [pk(bg)], ["sig%d" % (cc % 2)])
                            V(lambda e, ba=ba, sg=sg, cc=cc: e.tensor_tensor(ypad[:, cc, 16 + tc * 512:16 + (tc + 1) * 512], ps[ba][:, :], sg, ALU.mult),
                              [pk(ba), "sig%d" % (cc % 2)], [("ypad", (cc, tc))])

                    chk("T1")
                    tr.barrier()
                    R = Region(PB0)
                    dg = R.a([128, 4, 31, 128], BF16)
                    yc = R.a([128, 4, 512], F32)
                    ycb = R.a([128, 4, 512], BF16)
                    sqy = R.a([128, 4, 512], BF16)
                    mean = R.a([128, 512], F32)
                    var = R.a([128, 512], F32)
                    msq = R.a([128, 512], F32)
                    tt = [R.a([128, 512], F32) for _ in range(2)]
                    for cc in range(4):
                        for j in range(31):
                            if j % 2 == 0:
                                V(lambda e, cc=cc, j=j: e.tensor_scalar(dg[:, cc, j, :], identf, vecs[:, 37 + cc * 31 + j:38 + cc * 31 + j], None, ALU.mult),
                                  ["identf", "vecs"], [("dg", cc)])
                            else:
                                A(lambda e, cc=cc, j=j: e.activation(dg[:, cc, j, :], identf, AF.Copy, scale=vecs[:, 37 + cc * 31 + j:38 + cc * 31 + j]),
                                  ["identf", "vecs"], [("dg", cc)])
                    for tc in range(4):
                        tsl = slice(tc * 512, (tc + 1) * 512)
                        for cc in range(4):
                            b = nb()
                            for j in range(31):
                                T(lambda e, b=b, cc=cc, j=j: e.matmul(ps[b][:, :], dg[:, cc, j, :], ypad[:, cc, tc * 512 + j + 1:tc * 512 + j + 513],
                                                                      start=(j == 0), stop=(j == 30)),
                                  [("dg", cc), "ypad"], [pk(b)])
                            A(lambda e, b=b, cc=cc: e.activation(yc[:, cc, :], ps[b][:, :], AF.Identity, bias=vecs[:, 25 + cc:26 + cc], scale=1.0),
                              [pk(b), "vecs"], [("yc", cc)])
                            A(lambda e, b=b, cc=cc: e.activation(sqy[:, cc, :], ps[b][:, :], AF.Square, bias=vecs[:, 25 + cc:26 + cc], scale=1.0),
                              [pk(b), "vecs"], [("sqy", cc)])
                            V(lambda e, cc=cc: e.tensor_copy(ycb[:, cc, :], yc[:, cc, :]), [("yc", cc)], [("ycb", cc)])
                        b1 = nb()
                        for cc in range(4):
                            T(lambda e, cc=cc, b1=b1: e.matmul(ps[b1][:, :], onesb, ycb[:, cc, :], start=(cc == 0), stop=(cc == 3)), [("ycb", cc)], [pk(b1)])
                        b2 = nb()
                        for cc in range(4):
                            T(lambda e, cc=cc, b2=b2: e.matmul(ps[b2][:, :], onesb, sqy[:, cc, :], start=(cc == 0), stop=(cc == 3)), [("sqy", cc)], [pk(b2)])
                        A(lambda e, b1=b1: e.activation(mean, ps[b1][:, :], AF.Copy, scale=1.0 / 512), [pk(b1)], ["mean"])
                        V(lambda e: e.tensor_tensor(msq, mean, mean, ALU.mult), ["mean"], ["msq"])
                        V(lambda e, b2=b2: e.scalar_tensor_tensor(var, ps[b2][:, :], 1.0 / 512, msq, ALU.mult, ALU.subtract), [pk(b2), "msq"], ["var"])
                        A(lambda e: e.activation(var, var, AF.Sqrt, bias=epsc, scale=1.0), ["var", "ccols"], ["var"])
                        V(lambda e: e.reciprocal(var, var), ["var"], ["var"])
                        for cc in range(4):
                            t = tt[cc % 2]
                            V(lambda e, cc=cc, t=t: e.tensor_tensor(t, yc[:, cc, :], mean, ALU.subtract), [("yc", cc), "mean"], ["tt%d" % (cc % 2)])
                            V(lambda e, t=t, cc=cc: e.tensor_tensor(t, t, var, ALU.mult), ["tt%d" % (cc % 2), "var"], ["tt%d" % (cc % 2)])
                            A(lambda e, cc=cc, t=t: e.activation(catC[:, cc, tsl], t, AF.Silu, bias=vecs[:, 33 + cc:34 + cc], scale=vecs[:, 29 + cc:30 + cc]),
                              ["tt%d" % (cc % 2), "vecs"], [("catC", tc)])

                    chk("T2")
                    tr.barrier()
                    R = Region(PB0)
                    w_uq_b = R.a([128, 2, 768], BF16)
                    w_uqs = R.a([128, 2, 256], BF16)
                    w_ukv_b = R.a([128, 1024], BF16)
                    w_out_b = R.a([128, 8, 1024], BF16)
                    KnT = R.a([128, 4, S_LEN], BF16)
                    KrT = R.a([128, S_LEN], BF16)
                    Vb = R.a([128, 16, 512], BF16)
                    scl = R.a([128, 64], F32)
                    sskr = R.a([128, 16], F32)
                    ssk = R.a([128, 16], F32)
                    KT0 = R.off
                    sqk = [R.a([128, 512], BF16) for _ in range(2)]
                    kt1 = R.a([128, 512], F32)
                    kt2 = R.a([128, 512], F32)
                    sqkr = R.a([128, 512], BF16)
                    tr.op(POOL, lambda e, l=l: e.dma_start(out=w_uq_b, in_=wuq_d[l].rearrange("(c p) n -> p c n", p=128)), writes=["w_uq"], dma=s_tw[0])
                    tr.op(POOL, lambda e, l=l: e.dma_start(out=w_ukv_b, in_=wukv_d[l]), writes=["w_ukv"], dma=s_tw[1])
                    wv = wout_d[l].rearrange("(c p) n -> p c n", p=128)
                    for q in range(2):
                        tr.op(POOL, lambda e, q=q, wv=wv: e.dma_start(out=w_out_b[:, 4 * q:4 * q + 4, :], in_=wv[:, 4 * q:4 * q + 4, :]),
                              writes=[("w_out", q)], dma=s_tw[2 + q])
                    for h in range(NH):
                        G(lambda e, h=h: e.tensor_copy(w_uqs[:, :, h * 64:h * 64 + 32], w_uq_b[:, :, h * 192 + 160:h * 192 + 192]), ["w_uq"], ["w_uqs"])
                        G(lambda e, h=h: e.tensor_copy(w_uqs[:, :, h * 64 + 32:h * 64 + 64], w_uq_b[:, :, h * 192 + 128:h * 192 + 160]), ["w_uq"], ["w_uqs"])
                    RT = Region(PB0 + (768 + 256 + 512 + 4096))
                    pos_i = RT.a([128, 1024], I32)
                    ang = RT.a([128, 1024], F32)
                    kf = RT.a([128, 1024], F32)
                    ki = RT.a([128, 1024], I32)
                    C1 = 6.28125
                    C2 = 2.0 * math.pi - 6.28125
                    for hf in range(2):
                        hs = slice(hf * 1024, (hf + 1) * 1024)
                        tr.op(SP, lambda e, s=s, hs=hs: e.dma_start(out=pos_i[0:64, :], in_=pos_d[s, :, hs]), writes=["pos_i"], dma=s_misc)
                        V(lambda e: e.tensor_copy(ang[0:64], pos_i[0:64]), ["pos_i"], ["ang"])
                        V(lambda e: e.tensor_scalar(ang[0:64], ang[0:64], ccols[0:64, 2:3], None, ALU.mult), ["ang", "ccols"], ["ang"])
                        V(lambda e: e.tensor_scalar(kf[0:64], ang[0:64], 1.0 / (2.0 * math.pi), None, ALU.mult), ["ang"], ["kf"])
                        V(lambda e: e.tensor_copy(ki[0:64], kf[0:64]), ["kf"], ["ki"])
                        V(lambda e: e.tensor_copy(kf[0:64], ki[0:64]), ["ki"], ["kf"])
                        V(lambda e: e.scalar_tensor_tensor(ang[0:64], kf[0:64], -C1, ang[0:64], ALU.mult, ALU.add), ["kf", "ang"], ["ang"])
                        V(lambda e: e.scalar_tensor_tensor(ang[0:64], kf[0:64], -C2, ang[0:64], ALU.mult, ALU.add), ["kf", "ang"], ["ang"])
                        V(lambda e: e.tensor_scalar(ang[0:64], ang[0:64], 3.1415925, -3.1415925, ALU.min, ALU.max), ["ang"], ["ang"])
                        A(lambda e, hs=hs: e.activation(sin2[0:64, hs], ang[0:64], AF.Sin, scale=ccols[0:64, 3:4]), ["ang", "ccols"], ["sin2"])
                        V(lambda e: e.scalar_tensor_tensor(kf[0:64], ang[0:64], -1.0, ang[0:64], ALU.mult, ALU.max), ["ang"], ["kf"])
                        V(lambda e: e.tensor_scalar(kf[0:64], kf[0:64], -1.0, math.pi / 2, ALU.mult, ALU.add), ["kf"], ["kf"])
                        A(lambda e, hs=hs: e.activation(cos2[0:64, hs], kf[0:64], AF.Sin), ["kf"], ["cos2"])
                    tr.barrier()
                    chk("tab")
                    wv3 = w_ukv_b.rearrange("p (h c) -> p h c", h=4)
                    for j in range(16):
                        b = nb()
                        T(lambda e, b=b, j=j: e.matmul(ps[b][:, :], ckvn_b[:, j * 128:(j + 1) * 128], wv3[:, :, 128:256], start=True, stop=True),
                          ["ckvn", "w_ukv"], [pk(b)])
                        if j % 2 == 0:
                            A(lambda e, b=b, j=j: e.activation(Vb[:, j, :], ps[b][:, :], AF.Copy), [pk(b)], [("Vb", j)])
                        else:
                            V(lambda e, b=b, j=j: e.tensor_copy(Vb[:, j, :], ps[b][:, :]), [pk(b)], [("Vb", j)])
                    bS = nb()
                    for tc in range(4):
                        tsl = slice(tc * 512, (tc + 1) * 512)
                        V(lambda e, tsl=tsl: e.scalar_tensor_tensor(kt1[0:64], kr_f[0:64, tsl], vecs[0:64, 23:24], cos2[0:64, tsl], ALU.mult, ALU.mult),
                          ["krf", "vecs", "cos2"], ["kt1"])
                        V(lambda e, tsl=tsl: e.scalar_tensor_tensor(kt2[0:64], krs_f[0:64, tsl], vecs[0:64, 24:25], sin2[0:64, tsl], ALU.mult, ALU.mult),
                          ["krsf", "vecs", "sin2"], ["kt2"])
                        V(lambda e, tsl=tsl: e.tensor_tensor(KrT[0:64, tsl], kt1[0:64], kt2[0:64], ALU.add), ["kt1", "kt2"], [("KrT", tc)])
                        A(lambda e, tsl=tsl: e.activation(sqkr[0:64], kr_f[0:64, tsl], AF.Square), ["krf"], ["sqkr"])
                        for j in range(4):
                            T(lambda e, tc=tc, j=j: e.matmul(ps[bS][:, tc * 4 + j:tc * 4 + j + 1], sqkr[0:64, j * 128:(j + 1) * 128], onesb[0:64, 0:1],
                                                           start=True, stop=True), ["sqkr", "onesb"], [pk(bS)])
                    V(lambda e: e.tensor_copy(sskr, ps[bS][:, 0:16]), [pk(bS)], ["sskr"])
                    for h in range(NH):
                        bS = nb()
                        for tc in range(4):
                            tsl = slice(tc * 512, (tc + 1) * 512)
                            b = nb()
                            T(lambda e, b=b, h=h, tsl=tsl: e.matmul(ps[b][:, :], w_ukv_b[:, h * 256:h * 256 + 128], ckvn_b[:, tsl], start=True, stop=True),
                              ["ckvn", "w_ukv"], [pk(b)])
                            A(lambda e, b=b, h=h, tsl=tsl: e.activation(KnT[:, h, tsl], ps[b][:, :], AF.Copy, scale=vecs[:, 22:23]), [pk(b), "vecs"], [("KnT", h)])
                            sq = sqk[tc % 2]
                            A(lambda e, b=b, sq=sq: e.activation(sq, ps[b][:, :], AF.Square), [pk(b)], ["sqk%d" % (tc % 2)])
                            for j in range(4):
                                T(lambda e, tc=tc, j=j, sq=sq, bS=bS: e.matmul(ps[bS][:, tc * 4 + j:tc * 4 + j + 1], sq[:, j * 128:(j + 1) * 128], onesb[:, 0:1],
                                                                               start=True, stop=True), ["sqk%d" % (tc % 2), "onesb"], [pk(bS)])
                        V(lambda e, bS=bS: e.tensor_tensor(ssk, ps[bS][:, 0:16], sskr, ALU.add), [pk(bS), "sskr"], ["ssk"])
                        A(lambda e: e.activation(ssk, ssk, AF.Sqrt, bias=epsc, scale=1.0 / 192), ["ssk", "ccols"], ["ssk"])
                        V(lambda e: e.reciprocal(ssk, ssk), ["ssk"], ["ssk"])
                        V(lambda e, h=h: e.tensor_scalar(scl[:, h * 16:(h + 1) * 16], ssk, 1.0 / math.sqrt(192.0), None, ALU.mult), ["ssk"], [("scl", h)])
                    tr.barrier()
                    chk("kprep")
                    RQ = Region(QT0)
                    catA = RQ.a([128, 4, 512], BF16)
                    QnT = [RQ.a([128, 512], BF16) for _ in range(2)]
                    QrT = [RQ.a([128, 512], BF16) for _ in range(2)]
                    PT = [RQ.a([128, 512], BF16) for _ in range(4)]
                    sqn = RQ.a([128, 512], BF16)
                    sqr = RQ.a([128, 512], BF16)
                    rD = RQ.a([128, 512], F32)
                    assert RQ.off <= YP0
                    RK = Region(KT0)
                    rq = RK.a([128, 512], F32)
                    qt1 = RK.a([128, 512], F32)
                    qt2 = RK.a([128, 512], F32)
                    assert RK.off <= TOTW
                    SB = (0, 1, 2)
                    QB = (3, 4, 5)
                    bO, bD = 6, 7
                    pt_state = {"n": 0}

                    def q_prep(qc, h, qb):
                        qsl = slice(qc * 512, (qc + 1) * 512)
                        bqn, bqr, bqs = QB
                        for k in range(2):
                            T(lambda e, k=k: e.matmul(ps[bqn][:, :], w_uq_b[:, k, h * 192:h * 192 + 128], cqn_b[:, k, qsl], start=(k == 0), stop=(k == 1)),
                              ["w_uq", "cqn"], [pk(bqn)])
                        for k in range(2):
                            T(lambda e, k=k: e.matmul(ps[bqr][0:64, :], w_uq_b[:, k, h * 192 + 128:h * 192 + 192], cqn_b[:, k, qsl], start=(k == 0), stop=(k == 1)),
                              ["w_uq", "cqn"], [pk(bqr)])
                        for k in range(2):
                            T(lambda e, k=k: e.matmul(ps[bqs][0:64, :], w_uqs[:, k, h * 64:(h + 1) * 64], cqn_b[:, k, qsl], start=(k == 0), stop=(k == 1)),
                              ["w_uqs", "cqn"], [pk(bqs)])
                        A(lambda e: e.activation(sqn, ps[bqn][:, :], AF.Square), [pk(bqn)], ["sqn"])
                        A(lambda e: e.activation(sqr[0:64], ps[bqr][0:64, :], AF.Square), [pk(bqr)], ["sqr"])
                        bss = nb(SB)
                        T(lambda e: e.matmul(ps[bss][:, :], onesb, sqn, start=True, stop=False), ["sqn", "onesb"], [pk(bss)])
                        T(lambda e: e.matmul(ps[bss][:, :], onesb[0:64, :], sqr[0:64], start=False, stop=True), ["sqr", "onesb"], [pk(bss)])
                        rstd_from(bss, rq, 192.0, "rq")
                        V(lambda e: e.scalar_tensor_tensor(QnT[qb], ps[bqn][:, :], vecs[:, 19:20], rq, ALU.mult, ALU.mult), [pk(bqn), "vecs", "rq"], ["QnT%d" % qb])
                        V(lambda e: e.scalar_tensor_tensor(qt1[0:64], ps[bqr][0:64, :], vecs[0:64, 20:21], cos2[0:64, qsl], ALU.mult, ALU.mult),
                          [pk(bqr), "vecs", "cos2"], ["qt1"])
                        V(lambda e: e.scalar_tensor_tensor(qt2[0:64], ps[bqs][0:64, :], vecs[0:64, 21:22], sin2[0:64, qsl], ALU.mult, ALU.mult),
                          [pk(bqs), "vecs", "sin2"], ["qt2"])
                        V(lambda e: e.tensor_tensor(qt1[0:64], qt1[0:64], qt2[0:64], ALU.add), ["qt1", "qt2"], ["qt1"])
                        V(lambda e: e.tensor_tensor(QrT[qb][0:64], qt1[0:64], rq[0:64], ALU.mult), ["qt1", "rq"], ["QrT%d" % qb])

                    def core(qc, h, qb, hook):
                        def S_mm(kt):
                            b = nb(SB)
                            ksl = slice(kt * 128, (kt + 1) * 128)
                            T(lambda e: e.matmul(ps[b][:, :], KnT[:, h, ksl], QnT[qb], start=True, stop=False), [("KnT", h), "QnT%d" % qb], [pk(b)])
                            T(lambda e: e.matmul(ps[b][:, :], KrT[0:64, ksl], QrT[qb][0:64], start=False, stop=True), ["KrT", "QrT%d" % qb], [pk(b)])
                            return b
                        sb_cur = S_mm(0)
                        for kt in range(16):
                            sb_next = S_mm(kt + 1) if kt < 15 else None
                            pi = pt_state["n"] % 4
                            pt_state["n"] += 1
                            p_t = PT[pi]
                            A(lambda e: e.activation(p_t, ps[sb_cur][:, :], AF.Exp, scale=scl[:, h * 16 + kt:h * 16 + kt + 1]),
                              [pk(sb_cur), ("scl", h)], ["PT%d" % pi])
                            T(lambda e: e.matmul(ps[bO][:, :], Vb[:, kt, h * 128:(h + 1) * 128], p_t, start=(kt == 0), stop=(kt == 15)),
                              ["PT%d" % pi, ("Vb", kt)], [pk(bO)])
                            T(lambda e: e.matmul(ps[bD][:, :], onesb, p_t, start=(kt == 0), stop=(kt == 15)),
                              ["PT%d" % pi, "onesb"], [pk(bD)])
                            sb_cur = sb_next
                            if kt == 3:
                                hook()
                        V(lambda e: e.reciprocal(rD, ps[bD][:, :]), [pk(bD)], ["rD"])
                        V(lambda e: e.tensor_tensor(catA[:, h, :], ps[bO][:, :], rD, ALU.mult), [pk(bO), "rD"], [("catA", h)])

                    pairs = [(qc, h) for qc in range(4) for h in range(NH)]
                    q_prep(0, 0, 0)
                    for i, (qc, h) in enumerate(pairs):
                        qsl = slice(qc * 512, (qc + 1) * 512)
                        if i + 1 < len(pairs):
                            nqc, nh = pairs[i + 1]
                            hook = (lambda nqc=nqc, nh=nh, i=i: q_prep(nqc, nh, (i + 1) % 2))
                        else:
                            hook = (lambda: None)
                        core(qc, h, i % 2, hook)
                        if h == NH - 1:
                            for m in range(8):
                                b = nb(SB)
                                for k in range(8):
                                    rhs = catA[:, k, :] if k < 4 else catC[:, k - 4, qsl]
                                    rk = ("catA", k) if k < 4 else ("catC", qc)
                                    T(lambda e, b=b, k=k, m=m, rhs=rhs: e.matmul(ps[b][:, :], w_out_b[:, k, m * 128:(m + 1) * 128], rhs, start=(k == 0), stop=(k == 7)),
                                      ["w_out", rk], [pk(b)])
                                V(lambda e, b=b, m=m: e.scalar_tensor_tensor(xT[:, m, qsl], ps[b][:, :], mc[:, 16 + m:17 + m], xT[:, m, qsl], ALU.mult, ALU.add),
                                  [pk(b), "mc", ("xT", (m, qc))], [("xT", (m, qc))])

                    chk("T3")
                    tr.barrier()
                    M = Region(PH0)
                    zer = M.a([128, 1024], F32)
                    h2st = [M.a([128, 1024], BF16) for _ in range(4)]
                    M1 = M
                    sq_b = M1.a([128, 8, 512], BF16)
                    h2Ts = [M1.a([128, 8, 512], BF16) for _ in range(2)]
                    xn = [M1.a([128, 512], F32) for _ in range(2)]
                    rstd2s = [M1.a([128, 512], F32) for _ in range(2)]
                    affT = M1.a([128, S_LEN], F32)
                    wr_f = M1.a([128, 8, NE], F32)
                    awr = M1.a([128, 8, NE], F32)
                    lg = M1.a([128, 512], F32)
                    rden = M1.a([128, 512], F32)
                    cstc = M1.a([128, 1], F32)
                    m8 = M1.a([128, 8], F32)
                    masktm_b = M1.a([128, 256], BF16)
                    masktm_f = M1.a([128, 256], F32)
                    afftm = M1.a([128, 256], F32)
                    hi_f = M1.a([128, 256], F32)
                    maskT = M1.a([128, S_LEN], BF16)
                    ET = M1.a([128, S_LEN], F32)
                    work = ET
                    mod_slab = [M1.a([128, 8, 512], F32) for _ in range(2)]

                    tr.op(SP, lambda e, l=l: e.dma_start(out=wr_f, in_=wr_d[l].rearrange("p (c n) -> p c n", c=8)), writes=["wr_f"], dma=s_misc)
                    V(lambda e: e.memset(zer, 0.0), [], ["zer"])
                    for c in range(8):
                        V(lambda e, c=c: e.tensor_scalar(awr[:, c, :], wr_f[:, c, :], mc[:, 24 + c:25 + c], None, ALU.mult), ["wr_f", "mc"], ["awr"])
                    b = nb()
                    for c in range(8):
                        T(lambda e, c=c, b=b: e.matmul(ps[b][0:16, 0:1], wr_f[:, c, :], mc[:, 32 + c:33 + c], start=(c == 0), stop=(c == 7)), ["wr_f", "mc"], [pk(b)])
                    V(lambda e, b=b: e.tensor_copy(cstc[0:16], ps[b][0:16, 0:1]), [pk(b)], ["cstc"])
                    h2v = h2_dram[s].rearrange("(j p) d -> p j d", p=128)
                    rms_chunk(0, mc[:, 24:32], mc[:, 32:40], sq_b, h2Ts[0], xn, rstd2s[0], "m1p0", hkey="hTm0")
                    for tc in range(4):
                        tsl = slice(tc * 512, (tc + 1) * 512)
                        h2T = h2Ts[tc % 2]
                        rstd2 = rstd2s[tc % 2]
                        hkm = "hTm%d" % (tc % 2)
                        if tc + 1 < 4:
                            rms_a(tc + 1, sq_b)
                        for j4 in range(4):
                            jt = tc * 4 + j4
                            hst = h2st[jt % 4]
                            for hf in range(2):
                                b = nb()
                                for c4 in range(4):
                                    c = hf * 4 + c4
                                    T(lambda e, b=b, c=c, c4=c4, j4=j4: e.matmul(ps[b][:, c4 * 128:(c4 + 1) * 128], h2T[:, c, j4 * 128:(j4 + 1) * 128], identb,
                                                                              start=True, stop=True), [(hkm, c), "identb"], [pk(b)])
                                dst = hst[:, hf * 512:(hf + 1) * 512]
                                if (j4 + hf) % 2 == 0:
                                    A(lambda e, b=b, dst=dst: e.activation(dst, ps[b][:, :], AF.Copy), [pk(b)], [("h2st", jt % 4)])
                                else:
                                    V(lambda e, b=b, dst=dst: e.tensor_copy(dst, ps[b][:, :]), [pk(b)], [("h2st", jt % 4)])
                            tr.op(SP, lambda e, jt=jt, hst=hst: e.dma_start(out=h2v[:, jt, :], in_=hst), reads=[("h2st", jt % 4)], writes=["h2d%d" % s], dma=s_h2)
                        b = nb()
                        for c in range(8):
                            T(lambda e, b=b, c=c, tsl=tsl: e.matmul(ps[b][0:16, :], awr[:, c, :], xT[:, c, tsl], start=(c == 0), stop=(c == 7)),
                              ["awr", ("xT", (c, tc))], [pk(b)])
                        if tc + 1 < 4:
                            rms_chunk(tc + 1, mc[:, 24:32], mc[:, 32:40], sq_b, h2Ts[(tc + 1) % 2], xn, rstd2s[(tc + 1) % 2], "m1p%d" % ((tc + 1) % 2),
                                      hkey="hTm%d" % ((tc + 1) % 2), do_a=False)
                        V(lambda e, b=b: e.tensor_tensor(lg[0:16], ps[b][0:16, :], rstd2[0:16], ALU.mult), [pk(b), "m1p%drstd" % (tc % 2)], ["lg"])
                        A(lambda e, tsl=tsl: e.activation(ET[0:16, tsl], lg[0:16], AF.Exp, bias=cstc[0:16], scale=1.0), ["lg", "cstc"], [("ET", tc)])
                        b = nb()
                        T(lambda e, b=b, tsl=tsl: e.matmul(ps[b][0:16, :], onesf[0:16, 0:16], ET[0:16, tsl], start=True, stop=True), [("ET", tc), "onesf"], [pk(b)])
                        V(lambda e, b=b: e.reciprocal(rden[0:16], ps[b][0:16, :]), [pk(b)], ["rden"])
                        V(lambda e, tsl=tsl: e.tensor_tensor(affT[0:16, tsl], ET[0:16, tsl], rden[0:16], ALU.mult), [("ET", tc), "rden"], [("affT", tc)])
                        if l + 1 < L:
                            per = (12 + n_seq - 1) // n_seq
                            mine = list(range(s * per, min(12, (s + 1) * per)))
                            q4 = (len(mine) + 3) // 4
                            emit_mod(l + 1, mod_slab, mine[tc * q4:(tc + 1) * q4])
                    for c in range(8):
                        tr.op(SP, lambda e, s=s, c=c: e.dma_start(out=xs_dram[s, c * 128:(c + 1) * 128, :], in_=xT[:, c, :]),
                              reads=[("xT", (c, t)) for t in range(4)], writes=["xs%d" % s], dma=s_sp)
                    if s + 1 < n_seq:
                        load_x(l, s + 1)
                    for ct in range(2):
                        accv = acc_dram[s][ct].rearrange("(j p) d -> p j d", p=128)
                        for j in range(16):
                            tr.op(SP, lambda e, j=j, accv=accv: e.dma_start(out=accv[:, j, :], in_=zer), reads=["zer"], writes=["accd%d%d" % (s, ct)], dma=s_zero)
                    ba = nb()
                    for j in range(16):
                        jsl = slice(j * 128, (j + 1) * 128)
                        T(lambda e, j=j, jsl=jsl: e.matmul(ps[ba][:, j * 16:(j + 1) * 16], affT[0:16, jsl], identf[0:16, 0:16], start=True, stop=True),
                          ["affT", "identf"], [pk(ba)])
                    V(lambda e: e.tensor_copy(afftm, ps[ba][:, 0:256]), [pk(ba)], ["afftm"])
                    lo16 = M1.a([128, 16], F32)
                    mid16 = M1.a([128, 16], F32)
                    cnt16 = M1.a([128, 16], F32)
                    ge16 = M1.a([128, 16], F32)
                    a3 = afftm.rearrange("p (j e) -> p j e", e=16)
                    m3 = masktm_b.rearrange("p (j e) -> p j e", e=16)
                    V(lambda e: e.memset(lo16, 0.0), [], ["lo16"])
                    NBIS = 30
                    for k in range(NBIS):
                        wk = 2.0 ** (-(k + 1))
                        V(lambda e, wk=wk: e.tensor_scalar(mid16, lo16, wk, None, ALU.add), ["lo16"], ["mid16"])
                        midb = mid16.rearrange("p (o e) -> p o e", o=1).to_broadcast([128, 16, 16])
                        V(lambda e, midb=midb: e.tensor_tensor(m3, a3, midb, ALU.is_ge), ["afftm", "mid16"], ["masktm_b"])
                        bc = nb()
                        T(lambda e, bc=bc: e.matmul(ps[bc][:, 0:256], onesb, masktm_b, start=True, stop=True), ["masktm_b", "onesb"], [pk(bc)])
                        pv = ps[bc][:, 0:256].rearrange("p (j e) -> p e j", e=16)
                        V(lambda e, pv=pv: e.tensor_reduce(out=cnt16, in_=pv, axis=mybir.AxisListType.X, op=ALU.add), [pk(bc)], ["cnt16"])
                        V(lambda e, wk=wk: e.tensor_scalar(ge16, cnt16, float(CAP) - 0.5, wk, ALU.is_ge, ALU.mult), ["cnt16"], ["ge16"])
                        V(lambda e: e.tensor_tensor(lo16, lo16, ge16, ALU.add), ["lo16", "ge16"], ["lo16"])
                    lob = lo16.rearrange("p (o e) -> p o e", o=1).to_broadcast([128, 16, 16])
                    mf3 = masktm_f.rearrange("p (j e) -> p j e", e=16)
                    V(lambda e: e.tensor_tensor(mf3, a3, lob, ALU.is_ge), ["afftm", "lo16"], ["masktm_f"])
                    V(lambda e: e.tensor_copy(masktm_b, masktm_f), ["masktm_f"], ["masktm_b"])
                    V(lambda e: e.tensor_copy(G4[:, :, 0], afftm), ["afftm"], ["G4%d" % s])
                    V(lambda e: e.tensor_copy(hi_f, G4[:, :, 0]), ["G4%d" % s], ["hi_f"])
                    V(lambda e: e.tensor_tensor(G4[:, :, 1], afftm, hi_f, ALU.subtract), ["afftm", "hi_f"], ["G4%d" % s])
                    V(lambda e: e.tensor_scalar(G4[:, :, 2], jrow, 0.0, ccols[:, 5:6], ALU.mult, ALU.add), ["jrow", "ccols"], ["G4%d" % s])
                    V(lambda e: e.tensor_copy(G4[:, :, 3], jrow), ["jrow"], ["G4%d" % s])
                    bp = nb()
                    for j in range(16):
                        for i in range(j):
                            T(lambda e, i=i, j=j: e.matmul(ps[bp][:, j * 16:(j + 1) * 16], onesb, masktm_b[:, i * 16:(i + 1) * 16], start=(i == 0), stop=False),
                              ["masktm_b", "onesb"], [pk(bp)])
                        T(lambda e, j=j: e.matmul(ps[bp][:, j * 16:(j + 1) * 16], BIG[:, 384:512], masktm_b[:, j * 16:(j + 1) * 16], start=(j == 0), stop=True),
                          ["masktm_b", "BIG"], [pk(bp)])
                    V(lambda e: e.scalar_tensor_tensor(pos1tm, ps[bp][:, 0:256], 1.0, masktm_f, ALU.add, ALU.mult), [pk(bp), "masktm_f"], ["pos1tm%d" % s])

                    chk("M1")

                tr.barrier()
                NSL = 256 * n_seq
                EA = Region(0)
                yef = [[EA.a([128, 1024], F32) for _ in range(2 * n_seq)] for _ in range(2)]
                xe_tm = [[EA.a([128, 2, 1024], BF16) for _ in range(n_seq)] for _ in range(2)]
                xeT = [EA.a([128, 8, NSL], BF16) for _ in range(2)]
                assert EA.off <= 16384, EA.off
                EB = Region(PH0)
                NSLOT = 12
                wsl = [EB.a([128, 4, 1024], BF16) for _ in range(NSLOT)]
                Sel = [EB.a([128, 16, 256], BF16) for _ in range(n_seq)]
                hidT = EB.a([128, 8, NSL], BF16)
                sgt = [EB.a([128, NSL], F32) for _ in range(2)]
                gcol = [[EB.a([128, 2], F32) for _ in range(n_seq)] for _ in range(3)]
                idxi = [[EB.a([128, 2], I32) for _ in range(n_seq)] for _ in range(3)]
                gtmp = EB.a([128, 8], F32)
                idxf = EB.a([128, 2], F32)

                wl = []
                for ex in range(NE):
                    wl += [(wg_d, ex, 0), (wg_d, ex, 1), (wu_d, ex, 0), (wu_d, ex, 1), (wd_d, ex, 0), (wd_d, ex, 1)]
                wstate = {"n": 0}

                def issue_w(upto):
                    while wstate["n"] < min(upto, len(wl)):
                        k = wstate["n"]
                        src, ex, hf = wl[k]
                        sl = k % NSLOT
                        sv = src[l, ex].rearrange("(c p) f -> p c f", p=128)[:, hf * 4:(hf + 1) * 4, :]
                        tr.op(POOL, lambda e, sl=sl, sv=sv: e.dma_start(out=wsl[sl], in_=sv), writes=["wsl%d" % sl], dma=s_w[sl])
                        wstate["n"] += 1

                issue_w(NSLOT - 1)
                wix = {"i": 0}

                def prep_sel(ex):
                    for s in range(n_seq):
                        for j in range(16):
                            V(lambda e, j=j, ex=ex, s=s: e.tensor_scalar(Sel[s][:, j, :], iota1, pos1tm_s[s][:, j * 16 + ex:j * 16 + ex + 1], None, ALU.is_equal),
                              ["iota1", "pos1tm%d" % s], [("Sel%d" % s, j)])

                def prep_idx(ex):
                    pb = ex % 3
                    for s in range(n_seq):
                        b = nb()
                        for ct in range(2):
                            for j in range(16):
                                T(lambda e, b=b, ct=ct, j=j, ex=ex, s=s: e.matmul(ps[b][:, ct * 4:ct * 4 + 4], Sel[s][:, j, ct * 128:(ct + 1) * 128], G4_s[s][:, j * 16 + ex, :],
                                                                                 start=(j == 0), stop=(j == 15)), [("Sel%d" % s, j), "G4%d" % s], [pk(b)])
                        V(lambda e, b=b: e.tensor_copy(gtmp, ps[b][:, 0:8]), [pk(b)], ["gtmp"])
                        g3 = gtmp.rearrange("p (a b) -> p a b", b=4)
                        V(lambda e, g3=g3, pb=pb, s=s: e.tensor_tensor(gcol[pb][s], g3[:, :, 0], g3[:, :, 1], ALU.add), ["gtmp"], ["gcol%d%d" % (pb, s)])
                        V(lambda e, g3=g3: e.scalar_tensor_tensor(idxf, g3[:, :, 3], 128.0, g3[:, :, 2], ALU.mult, ALU.add), ["gtmp"], ["idxf"])
                        V(lambda e, pb=pb, s=s: e.tensor_copy(idxi[pb][s], idxf), ["idxf"], ["idxi%d%d" % (pb, s)])

                def gather(ex):
                    pb, p3 = ex % 2, ex % 3
                    for s in range(n_seq):
                        for ct in range(2):
                            tr.op(POOL, lambda e, ct=ct, pb=pb, p3=p3, s=s: e.indirect_dma_start(
                                out=xe_tm[pb][s][:, ct, :], out_offset=None, in_=h2_dram[s],
                                in_offset=bass.IndirectOffsetOnAxis(ap=idxi[p3][s][:, ct:ct + 1], axis=0)),
                                reads=["h2d%d" % s, "idxi%d%d" % (p3, s)], writes=[("xe_tm%d%d" % (pb, s), ct)], dma=s_g[pb])

                def prep_tr(ex):
                    pb = ex % 2
                    for s in range(n_seq):
                        for cp in range(4):
                            b = nb()
                            for c in (2 * cp, 2 * cp + 1):
                                for ct in range(2):
                                    T(lambda e, b=b, c=c, ct=ct, pb=pb, s=s: e.matmul(ps[b][:, (c % 2) * 256 + ct * 128:(c % 2) * 256 + (ct + 1) * 128],
                                                                                   xe_tm[pb][s][:, ct, c * 128:(c + 1) * 128], identb, start=True, stop=True),
                                      [("xe_tm%d%d" % (pb, s), ct), "identb"], [pk(b)])
                            dst = xeT[pb][:, 2 * cp:2 * cp + 2, s * 256:(s + 1) * 256]
                            if cp % 2 == 0:
                                A(lambda e, b=b, dst=dst: e.activation(dst, ps[b][:, :].rearrange("p (a b) -> p a b", a=2), AF.Copy), [pk(b)], [("xeT%d" % pb, cp)])
                            else:
                                V(lambda e, b=b, dst=dst: e.tensor_copy(dst, ps[b][:, :].rearrange("p (a b) -> p a b", a=2)), [pk(b)], [("xeT%d" % pb, cp)])

                def ffn_gu(ex):
                    pb = ex % 2
                    widx = wix["i"]
                    issue_w(widx + NSLOT)
                    for fc in range(8):
                        bg = nb()
                        bu = nb()
                        for (w0, b) in ((widx, bg), (widx + 2, bu)):
                            for c in range(8):
                                wsel = (w0 + c // 4) % NSLOT
                                T(lambda e, b=b, wsel=wsel, c=c, fc=fc, pb=pb: e.matmul(ps[b][:, 0:NSL], wsl[wsel][:, c % 4, fc * 128:(fc + 1) * 128], xeT[pb][:, c, :],
                                                                                     start=(c == 0), stop=(c == 7)), ["wsl%d" % wsel, ("xeT%d" % pb, c // 2)], [pk(b)])
                        st_ = sgt[fc % 2]
                        A(lambda e, bg=bg, st_=st_: e.activation(st_, ps[bg][:, 0:NSL], AF.Silu), [pk(bg)], ["sgt%d" % (fc % 2)])
                        V(lambda e, bu=bu, st_=st_, fc=fc: e.tensor_tensor(hidT[:, fc, :], ps[bu][:, 0:NSL], st_, ALU.mult), [pk(bu), "sgt%d" % (fc % 2)], [("hidT", fc)])
                    wix["i"] += 4

                def ffn_down(ex):
                    pb = ex % 2
                    p3 = ex % 3
                    widx = wix["i"]
                    issue_w(widx + NSLOT)
                    for nd in range(2):
                        for q in range(2 * n_seq):
                            s, ct = q // 2, q % 2
                            b = nb()
                            for fc in range(8):
                                sd_ = (widx + fc // 4) % NSLOT
                                T(lambda e, b=b, fc=fc, q=q, sd_=sd_, nd=nd: e.matmul(ps[b][:, :], hidT[:, fc, q * 128:(q + 1) * 128], wsl[sd_][:, fc % 4, nd * 512:(nd + 1) * 512],
                                                                                   start=(fc == 0), stop=(fc == 7)),
                                  [("hidT", fc), "wsl%d" % sd_], [pk(b)])
                            A(lambda e, b=b, pb=pb, p3=p3, q=q, nd=nd, s=s, ct=ct: e.activation(yef[pb][q][:, nd * 512:(nd + 1) * 512], ps[b][:, :], AF.Copy, scale=gcol[p3][s][:, ct:ct + 1]),
                              [pk(b), "gcol%d%d" % (p3, s)], ["yef%d%d" % (pb, q)])
                    wix["i"] += 2
                    for q in range(2 * n_seq):
                        s, ct = q // 2, q % 2
                        tr.op(POOL, lambda e, ct=ct, pb=pb, p3=p3, s=s, q=q: e.indirect_dma_start(
                            out=acc_dram[s][ct], out_offset=bass.IndirectOffsetOnAxis(ap=idxi[p3][s][:, ct:ct + 1], axis=0),
                            in_=yef[pb][q], in_offset=None, bounds_check="BCREG", oob_is_err=True, compute_op=ALU.add),
                            reads=["yef%d%d" % (pb, q), "idxi%d%d" % (p3, s), "accd%d%d" % (s, ct)], writes=["accd%d%d" % (s, ct)], dma=s_acc[s][ct])

                prep_sel(0)
                prep_idx(0)
                gather(0)
                prep_sel(1)
                prep_idx(1)
                prep_tr(0)
                for ex in range(NE):
                    if ex + 1 < NE:
                        gather(ex + 1)
                    if ex + 2 < NE:
                        prep_sel(ex + 2)
                    ffn_gu(ex)
                    if ex + 2 < NE:
                        prep_idx(ex + 2)
                    ffn_down(ex)
                    if ex + 1 < NE:
                        prep_tr(ex + 1)
                tr.barrier()
                EC = Region(0)
                xch = [EC.a([128, 8, 512], F32) for _ in range(2)]
                accb = [[EC.a([128, 1024], F32) for _ in range(2)] for _ in range(4)]
                assert EC.off <= 16384
                kx = 0
                for s in range(n_seq):
                    accv = [acc_dram[s][ct].rearrange("(j p) d -> p j d", p=128) for ct in range(2)]
                    xsv = xs_dram[s].rearrange("(c p) t -> p c t", p=128)
                    dstv = (out_d[s] if l == L - 1 else xs_dram[s]).rearrange("(c p) t -> p c t", p=128)
                    for tc in range(4):
                        xc = xch[kx % 2]
                        xk = "xch%d" % (kx % 2)
                        tr.op(SP, lambda e, xc=xc, xsv=xsv, tc=tc: e.dma_start(out=xc, in_=xsv[:, :, tc * 512:(tc + 1) * 512]),
                              reads=[("xs%d" % s, tc)], writes=[xk], dma=s_xc[kx % 2])
                        for j4 in range(4):
                            j = tc * 4 + j4
                            ab = accb[j % 4]
                            for ct in range(2):
                                tr.op(SP if ct == 0 else ACT, lambda e, j=j, ab=ab, accv=accv, ct=ct: e.dma_start(out=ab[ct], in_=accv[ct][:, j, :]), reads=["accd%d%d" % (s, ct)],
                                      writes=["accb%d_%d" % (j % 4, ct)], dma=s_ab2[j % 4][ct])
                            V(lambda e, ab=ab: e.tensor_tensor(ab[0], ab[0], ab[1], ALU.add), ["accb%d_0" % (j % 4), "accb%d_1" % (j % 4)], ["accb%d_0" % (j % 4)])
                            for hf in range(2):
                                b = nb()
                                for c4 in range(4):
                                    c = hf * 4 + c4
                                    T(lambda e, b=b, c=c, c4=c4, ab=ab: e.matmul(ps[b][:, c4 * 128:(c4 + 1) * 128], ab[0][:, c * 128:(c + 1) * 128], identf, start=True, stop=True),
                                      ["accb%d_0" % (j % 4), "identf"], [pk(b)])
                                for c4 in range(4):
                                    c = hf * 4 + c4
                                    V(lambda e, b=b, c=c, c4=c4, j4=j4, xc=xc, s=s: e.scalar_tensor_tensor(xc[:, c, j4 * 128:(j4 + 1) * 128], ps[b][:, c4 * 128:(c4 + 1) * 128], mcs[s][:, 40 + c:41 + c],
                                                                                                      xc[:, c, j4 * 128:(j4 + 1) * 128], ALU.mult, ALU.add),
                                      [pk(b), "mc", xk], [xk])
                        tr.op(POOL, lambda e, xc=xc, dstv=dstv, tc=tc: e.dma_start(out=dstv[:, :, tc * 512:(tc + 1) * 512], in_=xc),
                              reads=[xk], writes=[("xs%d" % s, tc)], dma=s_out)
                        kx += 1
        try:
            main_body()
        except _Stop:
            pass
        tr.emit(nc)
    return nc


def _consts():
    c = np.zeros((128, NCST), np.float32)
    c[:, 0:128] = np.eye(128, dtype=np.float32)
    tp = np.arange(128)
    c[:, 128:256] = (tp[:, None] < tp[None, :]).astype(np.float32)
    c[:, 256:512] = np.arange(1, 257, dtype=np.float32)[None, :]
    c[:, 512] = tp + 1
    c[:, 513] = tp + 129
    inv_freq = (1.0 / (np.float32(10000.0) ** (np.arange(0, 64, 2, dtype=np.float32) / np.float32(64)))).astype(np.float32)
    c[0:64, 514] = np.concatenate([inv_freq, inv_freq])
    c[0:32, 515] = -1.0
    c[32:64, 515] = 1.0
    c[:, 516] = EPS
    c[:, 517] = tp
    c[:, 520:776] = (np.arange(256) // 16).astype(np.float32)[None, :]
    return c


def _col(v, nch):
    return np.ascontiguousarray(np.asarray(v, np.float32).reshape(nch, 128).T)


def _pack_layer_inputs(inp, L):
    vec = np.zeros((L, 128, NV), np.float32)
    for l in range(L):
        v = vec[l]
        v[:, 0:8] = _col(inp["norm1_g"][l], 8)
        v[:, 8:16] = _col(inp["norm2_g"][l], 8)
        v[:, 16:18] = _col(inp["q_latent_g"][l], 2)
        v[:, 18:19] = _col(inp["kv_latent_g"][l], 1)
        qg = np.asarray(inp["q_head_g"][l], np.float32)
        kg = np.asarray(inp["k_head_g"][l], np.float32)
        v[:, 19] = qg[0:128]
        v[0:64, 20] = qg[128:192]
        v[0:64, 21] = np.concatenate([qg[160:192], qg[128:160]])
        v[:, 22] = kg[0:128]
        v[0:64, 23] = kg[128:192]
        v[0:64, 24] = np.concatenate([kg[160:192], kg[128:160]])
        v[:, 25:29] = _col(inp["conv_b"][l], 4)
        v[:, 29:33] = _col(inp["conv_norm_g"][l], 4)
        v[:, 33:37] = _col(inp["conv_norm_b"][l], 4)
        cw = np.asarray(inp["conv_w"][l], np.float32)
        for cc in range(4):
            v[:, 37 + cc * 31:37 + (cc + 1) * 31] = cw[:, cc * 128:(cc + 1) * 128].T
    bada = np.stack([_col(inp["b_ada"][l], 48) for l in range(L)])
    wr = np.stack([np.ascontiguousarray(np.asarray(inp["w_router"][l], np.float32).reshape(8, 128, NE).transpose(1, 0, 2)).reshape(128, 8 * NE)
                   for l in range(L)])
    return vec, bada, wr


def make_in_maps(inp, n_cores, n_seq, L, batch_ids=None):
    f32 = lambda a: np.ascontiguousarray(np.asarray(a, np.float32))
    vec, bada, wr = _pack_layer_inputs(inp, L)
    cst = _consts()
    shared = {
        "cst": cst, "vec": vec, "bada": bada, "w_router": wr,
        "w_ada": f32(inp["w_ada"][:L]), "w_in": f32(inp["w_in"][:L]), "w_uq": f32(inp["w_uq"][:L]),
        "w_ukv": f32(inp["w_ukv"][:L]), "w_out": f32(inp["w_out"][:L]),
        "w_gate": f32(inp["w_gate"][:L]), "w_up": f32(inp["w_up"][:L]), "w_down": f32(inp["w_down"][:L]),
    }
    x = np.asarray(inp["x"], np.float32)
    c = np.asarray(inp["c"], np.float32)
    pos = np.asarray(inp["positions"], np.int32)
    maps = []
    for core in range(n_cores):
        ids = batch_ids[core] if batch_ids is not None else list(range(core * n_seq, (core + 1) * n_seq))
        xT = np.ascontiguousarray(np.stack([x[b].T for b in ids]))
        cT = np.zeros((128, 8 * n_seq), np.float32)
        for si, b in enumerate(ids):
            cT.reshape(128, 8, n_seq)[:, :, si] = c[b].reshape(8, 128).T
        posr = np.ascontiguousarray(np.stack([np.broadcast_to(pos[b][None, :], (64, S_LEN)) for b in ids])).astype(np.int32)
        m = dict(shared)
        m.update({"xT": xT, "cT": cT, "posr": posr})
        maps.append(m)
    return maps


_NC_CACHE = {}


def kernel(**inputs):
    n_cores, n_seq, L = 8, 2, 2
    key = (n_seq, L)
    if key not in _NC_CACHE:
        _NC_CACHE[key] = build_program(n_seq, L)
    nc = _NC_CACHE[key]
    maps = make_in_maps(inputs, n_cores, n_seq, L)
    res = run_bass_kernel_spmd(nc, maps, core_ids=list(range(n_cores)))
    out = np.empty((n_cores * n_seq, S_LEN, D), np.float32)
    for core in range(n_cores):
        oT = res.results[core]["outT"]
        for si in range(n_seq):
            out[core * n_seq + si] = oT[si].T
    return out
```

```python
import contextlib
import math
import numpy as np
import concourse.bass as bass
import concourse.mybir as mybir
from concourse.bass_utils import run_bass_kernel_spmd

F32 = mybir.dt.float32
BF16 = mybir.dt.bfloat16
I32 = mybir.dt.int32
ALU = mybir.AluOpType
AF = mybir.ActivationFunctionType

PE, ACT, DVE, POOL, SP = "pe", "act", "dve", "pool", "sp"
ENGS = [PE, ACT, DVE, POOL, SP]

S_LEN = 2048
D = 1024
NH = 4
NE = 16
CAP = 256
EPS = 1e-6
NV = 161
NCST = 520 + 256


class _Op:
    __slots__ = ("eng", "idx", "fn", "waits", "sig", "sigval", "dma", "clock", "dclock")


class DmaSlot:
    def __init__(self, name):
        self.name = name
        self.count = 0
        self.sem = None
        self.last_op = None


class _Rec:
    def __getattr__(self, name):
        return lambda *a, **k: (name, a, k)


_REC = _Rec()


class Tracker:
    def __init__(self):
        self.ops = {e: [] for e in ENGS}
        self.state = {}
        self.clock = {e: {} for e in ENGS}
        self.dclock = {e: {} for e in ENGS}
        self.pending = {e: [] for e in ENGS}
        self.slots = []

    def slot(self, name):
        s = DmaSlot(name)
        self.slots.append(s)
        return s

    def _collect(self, key, is_write, deps):
        buf, sub = key if isinstance(key, tuple) else (key, None)
        st = self.state.setdefault(buf, {})
        if sub is None:
            ents = list(st.values())
        else:
            ents = [st[k] for k in (sub, None) if k in st]
        for ent in ents:
            if ent[0] is not None:
                deps.append(ent[0])
            if is_write:
                deps.extend(ent[1].values())
                deps.extend(ent[2])

    def _update(self, key, is_write, tok):
        buf, sub = key if isinstance(key, tuple) else (key, None)
        st = self.state.setdefault(buf, {})
        if is_write:
            if sub is None:
                st.clear()
                st[None] = [tok, {}, []]
            else:
                st[sub] = [tok, {}, []]
        else:
            ent = st.setdefault(sub, [None, {}, []])
            if tok[0] == "op":
                ent[1][tok[1]] = tok
            else:
                ent[2].append(tok)
                if len(ent[2]) > 8:
                    ent[2] = ent[2][-8:]

    def op(self, eng, fn, reads=(), writes=(), dma=None):
        o = _Op()
        o.eng = eng
        o.idx = len(self.ops[eng])
        o.fn = fn(_REC)
        o.sig = False
        o.sigval = None
        o.dma = None
        deps = list(self.pending[eng])
        self.pending[eng] = []
        for k in reads:
            self._collect(k, False, deps)
        for k in writes:
            self._collect(k, True, deps)
        clock = self.clock[eng]
        dclock = self.dclock[eng]
        waits = []
        for d in deps:
            if d[0] == "op":
                _, e2, i2 = d
                if e2 == eng:
                    if eng in (PE, SP):
                        continue
                    if i2 < o.idx - 3:
                        continue
                    if clock.get(e2, -1) >= i2:
                        continue
                    src = self.ops[e2][i2]
                    src.sig = True
                    waits.append(("op", src))
                    clock[e2] = i2
                    continue
                if clock.get(e2, -1) >= i2:
                    continue
                src = self.ops[e2][i2]
                src.sig = True
                waits.append(("op", src))
                clock[e2] = i2
                for k, v in src.clock.items():
                    if k != eng and clock.get(k, -1) < v:
                        clock[k] = v
                for k, v in src.dclock.items():
                    if dclock.get(k, -1) < v:
                        dclock[k] = v
            else:
                _, slot, val, src = d
                val = slot.count
                src = slot.last_op
                if dclock.get(slot, -1) >= val:
                    continue
                waits.append(("dma", slot, val))
                dclock[slot] = val
                for k, v in src.clock.items():
                    if k != eng and clock.get(k, -1) < v:
                        clock[k] = v
                for k, v in src.dclock.items():
                    if dclock.get(k, -1) < v:
                        dclock[k] = v
        o.waits = waits
        o.clock = dict(clock)
        o.dclock = dict(dclock)
        if dma is not None:
            dma.count += 16
            o.dma = (dma, dma.count)
            dma.last_op = o
            tok = ("dma", dma, dma.count, o)
        else:
            tok = ("op", eng, o.idx)
        self.ops[eng].append(o)
        for k in reads:
            self._update(k, False, tok)
        for k in writes:
            self._update(k, True, tok)
        return o

    def barrier(self):
        toks = []
        for e in (PE, ACT, DVE, POOL):
            for o in reversed(self.ops[e]):
                if o.dma is None:
                    toks.append(("op", e, o.idx))
                    break
        for s in self.slots:
            if s.last_op is not None:
                toks.append(("dma", s, s.count, s.last_op))
        for e in ENGS:
            self.pending[e] = list(toks)

    def emit(self, nc):
        stack = contextlib.ExitStack()
        with stack:
            esem = {}
            for e in (PE, ACT, DVE, POOL):
                esem[e] = stack.enter_context(nc.semaphore("s_" + e))
            for i, s in enumerate(self.slots):
                if s.count > 0:
                    s.sem = stack.enter_context(nc.semaphore("d%d_%s" % (i, s.name)))
            for e in (PE, ACT, DVE, POOL):
                c = 0
                for o in self.ops[e]:
                    if o.sig:
                        c += 1
                        o.sigval = c
            block = stack.enter_context(nc.Block())

            def run(engobj, e):
                bcreg = None
                if e == POOL and any(o.fn[2].get("bounds_check") == "BCREG" for o in self.ops[e]):
                    bcreg = engobj.alloc_register("bcreg")
                    engobj.reg_mov(bcreg, S_LEN - 1)
                for o in self.ops[e]:
                    for w in o.waits:
                        if w[0] == "op":
                            engobj.wait_ge(esem[w[1].eng], w[1].sigval)
                        else:
                            engobj.wait_ge(w[1].sem, w[2])
                    name, a_, k_ = o.fn
                    if k_.get("bounds_check") == "BCREG":
                        k_ = dict(k_)
                        k_["bounds_check"] = bcreg
                    ins = getattr(engobj, name)(*a_, **k_)
                    if o.dma is not None:
                        ins.then_inc(o.dma[0].sem, 16)
                    elif o.sig:
                        ins.then_inc(esem[e], 1)

            @block.tensor
            def _(t):
                run(t, PE)

            @block.scalar
            def _(a):
                run(a, ACT)

            @block.vector
            def _(v):
                run(v, DVE)

            @block.gpsimd
            def _(g):
                run(g, POOL)

            @block.sync
            def _(s):
                run(s, SP)
                for sl in self.slots:
                    if sl.count > 0:
                        s.wait_ge(sl.sem, sl.count)


LAY = []


class _Stop(Exception):
    pass


def build_program(n_seq=2, n_layers=2, stop=None):
    import inspect
    del LAY[:]
    nc = bass.Bass("TRN2", target_bir_lowering=False)
    L = n_layers
    xT_d = nc.dram_tensor("xT", [n_seq, D, S_LEN], F32, kind="ExternalInput").ap()
    out_d = nc.dram_tensor("outT", [n_seq, D, S_LEN], F32, kind="ExternalOutput").ap()
    cT_d = nc.dram_tensor("cT", [128, 8 * n_seq], F32, kind="ExternalInput").ap()
    pos_d = nc.dram_tensor("posr", [n_seq, 64, S_LEN], I32, kind="ExternalInput").ap()
    cst_d = nc.dram_tensor("cst", [128, NCST], F32, kind="ExternalInput").ap()
    vec_d = nc.dram_tensor("vec", [L, 128, NV], F32, kind="ExternalInput").ap()
    bada_d = nc.dram_tensor("bada", [L, 128, 48], F32, kind="ExternalInput").ap()
    wada_d = nc.dram_tensor("w_ada", [L, D, 6 * D], F32, kind="ExternalInput").ap()
    win_d = nc.dram_tensor("w_in", [L, D, 1472], F32, kind="ExternalInput").ap()
    wuq_d = nc.dram_tensor("w_uq", [L, 256, 768], F32, kind="ExternalInput").ap()
    wukv_d = nc.dram_tensor("w_ukv", [L, 128, 1024], F32, kind="ExternalInput").ap()
    wout_d = nc.dram_tensor("w_out", [L, D, D], F32, kind="ExternalInput").ap()
    wr_d = nc.dram_tensor("w_router", [L, 128, 8 * NE], F32, kind="ExternalInput").ap()
    wg_d = nc.dram_tensor("w_gate", [L, NE, D, D], F32, kind="ExternalInput").ap()
    wu_d = nc.dram_tensor("w_up", [L, NE, D, D], F32, kind="ExternalInput").ap()
    wd_d = nc.dram_tensor("w_down", [L, NE, D, D], F32, kind="ExternalInput").ap()

    h2_dram = [nc.dram_tensor("h2s%d" % i, [S_LEN, D], BF16, kind="Internal").ap() for i in range(n_seq)]
    acc_dram = [[nc.dram_tensor("accs%d_%d" % (i, ct), [S_LEN, D], F32, kind="Internal").ap() for ct in range(2)] for i in range(n_seq)]
    xs_dram = nc.dram_tensor("xss", [n_seq, D, S_LEN], F32, kind="Internal").ap()
    tr = Tracker()
    TOTW = 53000
    dump_d = nc.dram_tensor("dump", [128, TOTW], F32, kind="ExternalOutput").ap() if stop is not None else None
    stack = contextlib.ExitStack()
    with stack:
        arena = stack.enter_context(nc.sbuf_tensor("arena", [128, TOTW], F32))
        ps = [stack.enter_context(nc.psum_tensor("ps%d" % i, [128, 512], F32)) for i in range(8)]

        def view(off, shape, dt):
            n = int(np.prod(shape[1:]))
            nb = n * (2 if dt == BF16 else 4)
            nw = (nb + 3) // 4
            assert off + nw <= TOTW, (off, nw)
            if stop is not None:
                LAY.append((inspect.stack()[2].lineno, off, tuple(shape), "bf16" if dt == BF16 else ("i32" if dt == I32 else "f32")))
            v = arena[:, off:off + nw]
            if dt != F32:
                v = v.bitcast(dt)
            if len(shape) == 3:
                v = v.rearrange("p (a b) -> p a b", a=shape[1])
            elif len(shape) == 4:
                v = v.rearrange("p (a b c) -> p a b c", a=shape[1], b=shape[2])
            return v, off + nw

        class Region:
            def __init__(self, start):
                self.off = start

            def a(self, shape, dt):
                v, self.off = view(self.off, shape, dt)
                return v

        def chk(name):
            if stop == name:
                tr.barrier()
                tr.op(SP, lambda e: e.dma_start(out=dump_d, in_=arena[:, :]), dma=s_out)
                raise _Stop()

        V = lambda fn, r=(), w=(): tr.op(DVE, fn, r, w)
        A = lambda fn, r=(), w=(): tr.op(ACT, fn, r, w)
        G = lambda fn, r=(), w=(): tr.op(POOL, fn, r, w)
        T = lambda fn, r=(), w=(): tr.op(PE, fn, r, w)
        bank_ctr = [0]

        def nb(pool=(0, 1, 2, 3, 4, 5, 6, 7)):
            b = pool[bank_ctr[0] % len(pool)]
            bank_ctr[0] += 1
            return b

        def pk(b):
            return "ps%d" % b

        P = Region(0)
        xT = P.a([128, 8, S_LEN], F32)
        identb = P.a([128, 128], BF16)
        onesb = P.a([128, 128], BF16)
        BIG = P.a([128, 1024], BF16)
        jrow = P.a([128, 256], F32)
        identf = P.a([128, 128], F32)
        iota1 = P.a([128, 256], F32)
        ccols = P.a([128, 8], F32)
        onesf = P.a([128, 16], F32)
        modc = P.a([128, L * 48 * n_seq], F32)
        mcs = [P.a([128, 48], F32) for _ in range(n_seq)]
        pos1tm_s = [P.a([128, 256], F32) for _ in range(n_seq)]
        G4_s = [P.a([128, 256, 4], BF16) for _ in range(n_seq)]
        vecs = P.a([128, NV], F32)
        cact = P.a([128, 8 * n_seq], F32)
        bada = P.a([128, L * 48], F32)
        PH0 = P.off
        epsc = ccols[:, 4:5]

        s_x = tr.slot("x")
        s_out = tr.slot("out")
        s_misc = tr.slot("misc")
        s_ada = [tr.slot("ada0"), tr.slot("ada1")]
        s_w = [tr.slot("w%d" % i) for i in range(12)]
        s_tw = [tr.slot("tw%d" % i) for i in range(4)]
        s_g = [tr.slot("g0"), tr.slot("g1")]
        s_acc = [[tr.slot("acc%d_%d" % (i, ct)) for ct in range(2)] for i in range(n_seq)]
        s_zero = tr.slot("zero")
        s_sp = tr.slot("sp")
        s_xc = [tr.slot("xc0"), tr.slot("xc1")]
        s_h2 = tr.slot("h2")
        s_ab = [tr.slot("ab%d" % i) for i in range(4)]
        s_ab2 = [[tr.slot("ab%d_%d" % (i, ct)) for ct in range(2)] for i in range(4)]

        def load_x(l, s):
            xsrc = xT_d[s] if l == 0 else xs_dram[s]
            for c in range(8):
                tr.op(SP, lambda e, c=c, xsrc=xsrc: e.dma_start(out=xT[:, c, :], in_=xsrc[c * 128:(c + 1) * 128, :]),
                      reads=["xs%d" % s], writes=[("xT", (c, t)) for t in range(4)], dma=s_x)

        load_x(0, 0)

        R = Region(PH0)
        cstf = R.a([128, NCST], F32)
        slab = [R.a([128, 8, 512], F32) for _ in range(2)]
        tr.op(SP, lambda e: e.dma_start(out=cstf, in_=cst_d), writes=["cstf"], dma=s_misc)
        tr.op(SP, lambda e: e.dma_start(out=cact, in_=cT_d), writes=["cact"], dma=s_misc)
        for l in range(L):
            tr.op(SP, lambda e, l=l: e.dma_start(out=bada[:, l * 48:(l + 1) * 48], in_=bada_d[l]), writes=["bada"], dma=s_misc)
        V(lambda e: e.tensor_copy(identf, cstf[:, 0:128]), ["cstf"], ["identf"])
        V(lambda e: e.tensor_copy(identb, cstf[:, 0:128]), ["cstf"], ["identb"])
        V(lambda e: e.memset(onesb, 1.0), [], ["onesb"])
        V(lambda e: e.memset(BIG[:, 0:384], 0.0), [], ["BIG"])
        V(lambda e: e.tensor_copy(BIG[:, 384:512], cstf[:, 128:256]), ["cstf"], ["BIG"])
        V(lambda e: e.memset(BIG[:, 512:1024], 1.0), [], ["BIG"])
        V(lambda e: e.tensor_copy(jrow, cstf[:, 520:776]), ["cstf"], ["jrow"])
        V(lambda e: e.tensor_copy(iota1, cstf[:, 256:512]), ["cstf"], ["iota1"])
        V(lambda e: e.tensor_copy(ccols, cstf[:, 512:520]), ["cstf"], ["ccols"])
        V(lambda e: e.memset(onesf, 1.0), [], ["onesf"])
        A(lambda e: e.activation(cact, cact, AF.Silu), ["cact"], ["cact"])
        def emit_mod(l, slab, sbs=range(12)):
            wv = wada_d[l].rearrange("(c p) n -> p c n", p=128)
            for sb in sbs:
                bi = sb % 2
                tr.op(SP, lambda e, bi=bi, wv=wv, sb=sb: e.dma_start(out=slab[bi], in_=wv[:, :, sb * 512:(sb + 1) * 512]),
                      writes=["slab%d" % bi], dma=s_ada[bi])
                b = nb()
                for j in range(4):
                    for kc in range(8):
                        T(lambda e, b=b, bi=bi, j=j, kc=kc: e.matmul(
                            ps[b][:, j * n_seq:(j + 1) * n_seq], slab[bi][:, kc, j * 128:(j + 1) * 128],
                            cact[:, kc * n_seq:(kc + 1) * n_seq], start=(kc == 0), stop=(kc == 7)),
                          ["slab%d" % bi, "cact"], [pk(b)])
                for s in range(n_seq):
                    base = l * 48 * n_seq
                    mview = modc[:, base:base + 48 * n_seq].rearrange("p (j s) -> p j s", s=n_seq)
                    pview = ps[b][:, 0:4 * n_seq].rearrange("p (j s) -> p j s", s=n_seq)
                    V(lambda e, mview=mview, pview=pview, s=s, sb=sb, l=l: e.tensor_tensor(
                        mview[:, sb * 4:(sb + 1) * 4, s], pview[:, :, s], bada[:, l * 48 + sb * 4:l * 48 + (sb + 1) * 4], ALU.add),
                      [pk(b), "bada"], ["modc"])

        emit_mod(0, slab)
        tr.barrier()

        def rms_a(tc, sq_b):
            xs = xT[:, :, tc * 512:(tc + 1) * 512]
            xk = [("xT", (c, tc)) for c in range(8)]
            A(lambda e: e.activation(sq_b, xs, AF.Square), xk, ["sq_b"])

        def rms_chunk(tc, acols, shcols, sq_b, hT_b, xn, rstd, tag, hkey="hT", do_a=True):
            if do_a:
                rms_a(tc, sq_b)
            b = nb()
            for c in range(8):
                T(lambda e, c=c, b=b: e.matmul(ps[b][:, :], onesb, sq_b[:, c, :], start=(c == 0), stop=(c == 7)),
                  ["sq_b", "onesb"], [pk(b)])
            A(lambda e, b=b: e.activation(rstd, ps[b][:, :], AF.Sqrt, bias=epsc, scale=1.0 / D), [pk(b), "ccols"], [tag + "rstd"])
            V(lambda e: e.reciprocal(rstd, rstd), [tag + "rstd"], [tag + "rstd"])
            for c in range(8):
                xi = xn[c % 2]
                V(lambda e, c=c, xi=xi: e.scalar_tensor_tensor(xi, xT[:, c, tc * 512:(tc + 1) * 512], acols[:, c:c + 1], rstd, ALU.mult, ALU.mult),
                  [("xT", (c, tc)), "mc", tag + "rstd"], ["xn%d" % (c % 2)])
                A(lambda e, c=c, xi=xi: e.activation(hT_b[:, c, :], xi, AF.Identity, bias=shcols[:, c:c + 1], scale=1.0),
                  ["xn%d" % (c % 2), "mc"], [(hkey, c)])

        def rstd_from(bank, out, width, tag, npart=128):
            A(lambda e: e.activation(out[0:npart], ps[bank][0:npart, :], AF.Sqrt, bias=epsc[0:npart], scale=1.0 / width), [pk(bank), "ccols"], [tag])
            V(lambda e: e.reciprocal(out[0:npart], out[0:npart]), [tag], [tag])

        RA = Region(PH0)
        cqn_b = RA.a([128, 2, S_LEN], BF16)
        catC = RA.a([128, 4, S_LEN], BF16)
        QT0 = RA.off
        ckvn_b = RA.a([128, S_LEN], BF16)
        kr_f = RA.a([128, S_LEN], F32)
        krs_f = RA.a([128, S_LEN], F32)
        YP0 = RA.off
        ypad = RA.a([128, 4, 2080], BF16)
        PB0 = RA.off
        RY = Region(YP0)
        cos2 = RY.a([128, S_LEN], F32)
        sin2 = RY.a([128, S_LEN], F32)
        assert RY.off <= PB0

        def main_body():
            chk("setup")
            for l in range(L):
                tr.barrier()
                tr.op(SP, lambda e, l=l: e.dma_start(out=vecs, in_=vec_d[l]), writes=["vecs"], dma=s_misc)
                for s in range(n_seq):
                    tr.barrier()
                    if s == 0 and l > 0:
                        load_x(l, 0)
                    mc = mcs[s]
                    pos1tm = pos1tm_s[s]
                    G4 = G4_s[s]
                    mbase = l * 48 * n_seq
                    mv = modc[:, mbase:mbase + 48 * n_seq].rearrange("p (j s) -> p j s", s=n_seq)
                    V(lambda e, mv=mv, s=s: e.scalar_tensor_tensor(mc[:, 0:8], mv[:, 8:16, s], 1.0, vecs[:, 0:8], ALU.add, ALU.mult), ["modc", "vecs"], ["mc"])
                    V(lambda e, mv=mv, s=s: e.tensor_copy(mc[:, 8:16], mv[:, 0:8, s]), ["modc"], ["mc"])
                    V(lambda e, mv=mv, s=s: e.tensor_copy(mc[:, 16:24], mv[:, 16:24, s]), ["modc"], ["mc"])
                    V(lambda e, mv=mv, s=s: e.scalar_tensor_tensor(mc[:, 24:32], mv[:, 32:40, s], 1.0, vecs[:, 8:16], ALU.add, ALU.mult), ["modc", "vecs"], ["mc"])
                    V(lambda e, mv=mv, s=s: e.tensor_copy(mc[:, 32:40], mv[:, 24:32, s]), ["modc"], ["mc"])
                    V(lambda e, mv=mv, s=s: e.tensor_copy(mc[:, 40:48], mv[:, 40:48, s]), ["modc"], ["mc"])

                    R = Region(PB0)
                    w_in_b = R.a([128, 8, 1472], BF16)
                    wkrs_b = R.a([128, 8, 64], BF16)
                    sq_b = R.a([128, 8, 512], BF16)
                    hTs = [R.a([128, 8, 512], BF16) for _ in range(2)]
                    xn = [R.a([128, 512], F32) for _ in range(2)]
                    rstd = R.a([128, 512], F32)
                    sig = [R.a([128, 512], F32) for _ in range(2)]
                    cq_f = R.a([128, 2, 512], F32)
                    sqc = R.a([128, 3, 512], BF16)
                    ckv_f = R.a([128, 512], F32)
                    rs2_ = R.a([128, 512], F32)
                    rs2 = [rs2_, rs2_]
                    wv = win_d[l].rearrange("(c p) n -> p c n", p=128)
                    for q in range(4):
                        tr.op(POOL, lambda e, q=q, wv=wv: e.dma_start(out=w_in_b[:, 2 * q:2 * q + 2, :], in_=wv[:, 2 * q:2 * q + 2, :]),
                              writes=[("w_in", q)], dma=s_tw[q])
                    G(lambda e: e.tensor_copy(wkrs_b[:, :, 0:32], w_in_b[:, :, 416:448]), ["w_in"], ["wkrs"])
                    G(lambda e: e.tensor_copy(wkrs_b[:, :, 32:64], w_in_b[:, :, 384:416]), ["w_in"], ["wkrs"])
                    G(lambda e: e.memset(ypad[:, :, 0:16], 0.0), [], ["ypad"])
                    G(lambda e: e.memset(ypad[:, :, 2064:2080], 0.0), [], ["ypad"])
                    rms_chunk(0, mc[:, 0:8], mc[:, 8:16], sq_b, hTs[0], xn, rstd, "t1", hkey="hT0")
                    for tc in range(4):
                        tsl = slice(tc * 512, (tc + 1) * 512)
                        hT_b = hTs[tc % 2]
                        hk = "hT%d" % (tc % 2)
                        if tc + 1 < 4:
                            rms_a(tc + 1, sq_b)

                        def proj(b, lhs_fn, mrows=128):
                            for c in range(8):
                                T(lambda e, c=c: e.matmul(ps[b][0:mrows, :], lhs_fn(c), hT_b[:, c, :], start=(c == 0), stop=(c == 7)),
                                  [(hk, c), "w_in", "wkrs"], [pk(b)])
                        for i in range(3):
                            b = nb()
                            proj(b, lambda c, i=i: w_in_b[:, c, i * 128:(i + 1) * 128])
                            dst = cq_f[:, i, :] if i < 2 else ckv_f
                            A(lambda e, b=b, dst=dst: e.activation(dst, ps[b][:, :], AF.Copy), [pk(b)], [("cqf", i)])
                            A(lambda e, b=b, i=i: e.activation(sqc[:, i, :], ps[b][:, :], AF.Square), [pk(b)], [("sqc", i)])
                        b = nb()
                        for i in range(2):
                            T(lambda e, i=i, b=b: e.matmul(ps[b][:, :], onesb, sqc[:, i, :], start=(i == 0), stop=(i == 1)), [("sqc", i)], [pk(b)])
                        rstd_from(b, rs2[0], 256.0, "rs2")
                        for i in range(2):
                            V(lambda e, i=i: e.scalar_tensor_tensor(cqn_b[:, i, tsl], cq_f[:, i, :], vecs[:, 16 + i:17 + i], rs2[0], ALU.mult, ALU.mult),
                              [("cqf", i), "vecs", "rs2"], [("cqn", tc)])
                        b = nb()
                        T(lambda e, b=b: e.matmul(ps[b][:, :], onesb, sqc[:, 2, :], start=True, stop=True), [("sqc", 2)], [pk(b)])
                        rstd_from(b, rs2[1], 128.0, "rs2")
                        V(lambda e: e.scalar_tensor_tensor(ckvn_b[:, tsl], ckv_f, vecs[:, 18:19], rs2[1], ALU.mult, ALU.mult),
                          [("cqf", 2), "vecs", "rs2"], [("ckvn", tc)])
                        if tc + 1 < 4:
                            rms_chunk(tc + 1, mc[:, 0:8], mc[:, 8:16], sq_b, hTs[(tc + 1) % 2], xn, rstd, "t1", hkey="hT%d" % ((tc + 1) % 2), do_a=False)
                        b = nb()
                        proj(b, lambda c: w_in_b[:, c, 384:448], 64)
                        A(lambda e, b=b: e.activation(kr_f[0:64, tsl], ps[b][0:64, :], AF.Copy), [pk(b)], [("krf", tc)])
                        b = nb()
                        proj(b, lambda c: wkrs_b[:, c, :], 64)
                        A(lambda e, b=b: e.activation(krs_f[0:64, tsl], ps[b][0:64, :], AF.Copy), [pk(b)], [("krsf", tc)])
                        for cc in range(4):
                            bg = nb()
                            proj(bg, lambda c, cc=cc: w_in_b[:, c, 960 + cc * 128:960 + (cc + 1) * 128])
                            ba = nb()
                            proj(ba, lambda c, cc=cc: w_in_b[:, c, 448 + cc * 128:448 + (cc + 1) * 128])
                            sg = sig[cc % 2]
                            A(lambda e, bg=bg, sg=sg: e.activation(sg, ps[bg][:, :], AF.Sigmoid), [pk(bg)], ["sig%d" % (cc % 2)])
                            V(lambda e, ba=ba, sg=sg, cc=cc: e.tensor_tensor(ypad[:, cc, 16 + tc * 512:16 + (tc + 1) * 512], ps[ba][:, :], sg, ALU.mult),
                              [pk(ba), "sig%d" % (cc % 2)], [("ypad", (cc, tc))])

                    chk("T1")
                    tr.barrier()
                    R = Region(PB0)
                    dg = R.a([128, 4, 31, 128], BF16)
                    yc = R.a([128, 4, 512], F32)
                    ycb = R.a([128, 4, 512], BF16)
                    sqy = R.a([128, 4, 512], BF16)
                    mean = R.a([128, 512], F32)
                    var = R.a([128, 512], F32)
                    msq = R.a([128, 512], F32)
                    tt = [R.a([128, 512], F32) for _ in range(2)]
                    for cc in range(4):
                        for j in range(31):
                            if j % 2 == 0:
                                V(lambda e, cc=cc, j=j: e.tensor_scalar(dg[:, cc, j, :], identf, vecs[:, 37 + cc * 31 + j:38 + cc * 31 + j], None, ALU.mult),
                                  ["identf", "vecs"], [("dg", cc)])
                            else:
                                A(lambda e, cc=cc, j=j: e.activation(dg[:, cc, j, :], identf, AF.Copy, scale=vecs[:, 37 + cc * 31 + j:38 + cc * 31 + j]),
                                  ["identf", "vecs"], [("dg", cc)])
                    for tc in range(4):
                        tsl = slice(tc * 512, (tc + 1) * 512)
                        for cc in range(4):
                            b = nb()
                            for j in range(31):
                                T(lambda e, b=b, cc=cc, j=j: e.matmul(ps[b][:, :], dg[:, cc, j, :], ypad[:, cc, tc * 512 + j + 1:tc * 512 + j + 513],
                                                                      start=(j == 0), stop=(j == 30)),
                                  [("dg", cc), "ypad"], [pk(b)])
                            A(lambda e, b=b, cc=cc: e.activation(yc[:, cc, :], ps[b][:, :], AF.Identity, bias=vecs[:, 25 + cc:26 + cc], scale=1.0),
                              [pk(b), "vecs"], [("yc", cc)])
                            A(lambda e, b=b, cc=cc: e.activation(sqy[:, cc, :], ps[b][:, :], AF.Square, bias=vecs[:, 25 + cc:26 + cc], scale=1.0),
                              [pk(b), "vecs"], [("sqy", cc)])
                            V(lambda e, cc=cc: e.tensor_copy(ycb[:, cc, :], yc[:, cc, :]), [("yc", cc)], [("ycb", cc)])
                        b1 = nb()
                        for cc in range(4):
                            T(lambda e, cc=cc, b1=b1: e.matmul(ps[b1][:, :], onesb, ycb[:, cc, :], start=(cc == 0), stop=(cc == 3)), [("ycb", cc)], [pk(b1)])
                        b2 = nb()
                        for cc in range(4):
                            T(lambda e, cc=cc, b2=b2: e.matmul(ps[b2][:, :], onesb, sqy[:, cc, :], start=(cc == 0), stop=(cc == 3)), [("sqy", cc)], [pk(b2)])
                        A(lambda e, b1=b1: e.activation(mean, ps[b1][:, :], AF.Copy, scale=1.0 / 512), [pk(b1)], ["mean"])
                        V(lambda e: e.tensor_tensor(msq, mean, mean, ALU.mult), ["mean"], ["msq"])
                        V(lambda e, b2=b2: e.scalar_tensor_tensor(var, ps[b2][:, :], 1.0 / 512, msq, ALU.mult, ALU.subtract), [pk(b2), "msq"], ["var"])
                        A(lambda e: e.activation(var, var, AF.Sqrt, bias=epsc, scale=1.0), ["var", "ccols"], ["var"])
                        V(lambda e: e.reciprocal(var, var), ["var"], ["var"])
                        for cc in range(4):
                            t = tt[cc % 2]
                            V(lambda e, cc=cc, t=t: e.tensor_tensor(t, yc[:, cc, :], mean, ALU.subtract), [("yc", cc), "mean"], ["tt%d" % (cc % 2)])
                            V(lambda e, t=t, cc=cc: e.tensor_tensor(t, t, var, ALU.mult), ["tt%d" % (cc % 2), "var"], ["tt%d" % (cc % 2)])
                            A(lambda e, cc=cc, t=t: e.activation(catC[:, cc, tsl], t, AF.Silu, bias=vecs[:, 33 + cc:34 + cc], scale=vecs[:, 29 + cc:30 + cc]),
                              ["tt%d" % (cc % 2), "vecs"], [("catC", tc)])

                    chk("T2")
                    tr.barrier()
                    R = Region(PB0)
                    w_uq_b = R.a([128, 2, 768], BF16)
                    w_uqs = R.a([128, 2, 256], BF16)
                    w_ukv_b = R.a([128, 1024], BF16)
                    w_out_b = R.a([128, 8, 1024], BF16)
                    KnT = R.a([128, 4, S_LEN], BF16)
                    KrT = R.a([128, S_LEN], BF16)
                    Vb = R.a([128, 16, 512], BF16)
                    scl = R.a([128, 64], F32)
                    sskr = R.a([128, 16], F32)
                    ssk = R.a([128, 16], F32)
                    KT0 = R.off
                    sqk = [R.a([128, 512], BF16) for _ in range(2)]
                    kt1 = R.a([128, 512], F32)
                    kt2 = R.a([128, 512], F32)
                    sqkr = R.a([128, 512], BF16)
                    tr.op(POOL, lambda e, l=l: e.dma_start(out=w_uq_b, in_=wuq_d[l].rearrange("(c p) n -> p c n", p=128)), writes=["w_uq"], dma=s_tw[0])
                    tr.op(POOL, lambda e, l=l: e.dma_start(out=w_ukv_b, in_=wukv_d[l]), writes=["w_ukv"], dma=s_tw[1])
                    wv = wout_d[l].rearrange("(c p) n -> p c n", p=128)
                    for q in range(2):
                        tr.op(POOL, lambda e, q=q, wv=wv: e.dma_start(out=w_out_b[:, 4 * q:4 * q + 4, :], in_=wv[:, 4 * q:4 * q + 4, :]),
                              writes=[("w_out", q)], dma=s_tw[2 + q])
                    for h in range(NH):
                        G(lambda e, h=h: e.tensor_copy(w_uqs[:, :, h * 64:h * 64 + 32], w_uq_b[:, :, h * 192 + 160:h * 192 + 192]), ["w_uq"], ["w_uqs"])
                        G(lambda e, h=h: e.tensor_copy(w_uqs[:, :, h * 64 + 32:h * 64 + 64], w_uq_b[:, :, h * 192 + 128:h * 192 + 160]), ["w_uq"], ["w_uqs"])
                    RT = Region(PB0 + (768 + 256 + 512 + 4096))
                    pos_i = RT.a([128, 1024], I32)
                    ang = RT.a([128, 1024], F32)
                    kf = RT.a([128, 1024], F32)
                    ki = RT.a([128, 1024], I32)
                    C1 = 6.28125
                    C2 = 2.0 * math.pi - 6.28125
                    for hf in range(2):
                        hs = slice(hf * 1024, (hf + 1) * 1024)
                        tr.op(SP, lambda e, s=s, hs=hs: e.dma_start(out=pos_i[0:64, :], in_=pos_d[s, :, hs]), writes=["pos_i"], dma=s_misc)
                        V(lambda e: e.tensor_copy(ang[0:64], pos_i[0:64]), ["pos_i"], ["ang"])
                        V(lambda e: e.tensor_scalar(ang[0:64], ang[0:64], ccols[0:64, 2:3], None, ALU.mult), ["ang", "ccols"], ["ang"])
                        V(lambda e: e.tensor_scalar(kf[0:64], ang[0:64], 1.0 / (2.0 * math.pi), None, ALU.mult), ["ang"], ["kf"])
                        V(lambda e: e.tensor_copy(ki[0:64], kf[0:64]), ["kf"], ["ki"])
                        V(lambda e: e.tensor_copy(kf[0:64], ki[0:64]), ["ki"], ["kf"])
                        V(lambda e: e.scalar_tensor_tensor(ang[0:64], kf[0:64], -C1, ang[0:64], ALU.mult, ALU.add), ["kf", "ang"], ["ang"])
                        V(lambda e: e.scalar_tensor_tensor(ang[0:64], kf[0:64], -C2, ang[0:64], ALU.mult, ALU.add), ["kf", "ang"], ["ang"])
                        V(lambda e: e.tensor_scalar(ang[0:64], ang[0:64], 3.1415925, -3.1415925, ALU.min, ALU.max), ["ang"], ["ang"])
                        A(lambda e, hs=hs: e.activation(sin2[0:64, hs], ang[0:64], AF.Sin, scale=ccols[0:64, 3:4]), ["ang", "ccols"], ["sin2"])
                        V(lambda e: e.scalar_tensor_tensor(kf[0:64], ang[0:64], -1.0, ang[0:64], ALU.mult, ALU.max), ["ang"], ["kf"])
                        V(lambda e: e.tensor_scalar(kf[0:64], kf[0:64], -1.0, math.pi / 2, ALU.mult, ALU.add), ["kf"], ["kf"])
                        A(lambda e, hs=hs: e.activation(cos2[0:64, hs], kf[0:64], AF.Sin), ["kf"], ["cos2"])
                    tr.barrier()
                    chk("tab")
                    wv3 = w_ukv_b.rearrange("p (h c) -> p h c", h=4)
                    for j in range(16):
                        b = nb()
                        T(lambda e, b=b, j=j: e.matmul(ps[b][:, :], ckvn_b[:, j * 128:(j + 1) * 128], wv3[:, :, 128:256], start=True, stop=True),
                          ["ckvn", "w_ukv"], [pk(b)])
                        if j % 2 == 0:
                            A(lambda e, b=b, j=j: e.activation(Vb[:, j, :], ps[b][:, :], AF.Copy), [pk(b)], [("Vb", j)])
                        else:
                            V(lambda e, b=b, j=j: e.tensor_copy(Vb[:, j, :], ps[b][:, :]), [pk(b)], [("Vb", j)])
                    V(lambda e: e.memset(KrT[64:128, :], 0.0), [], ["KrT"])
                    bS = nb()
                    for tc in range(4):
                        tsl = slice(tc * 512, (tc + 1) * 512)
                        V(lambda e, tsl=tsl: e.scalar_tensor_tensor(kt1[0:64], kr_f[0:64, tsl], vecs[0:64, 23:24], cos2[0:64, tsl], ALU.mult, ALU.mult),
                          ["krf", "vecs", "cos2"], ["kt1"])
                        V(lambda e, tsl=tsl: e.scalar_tensor_tensor(kt2[0:64], krs_f[0:64, tsl], vecs[0:64, 24:25], sin2[0:64, tsl], ALU.mult, ALU.mult),
                          ["krsf", "vecs", "sin2"], ["kt2"])
                        V(lambda e, tsl=tsl: e.tensor_tensor(KrT[0:64, tsl], kt1[0:64], kt2[0:64], ALU.add), ["kt1", "kt2"], [("KrT", tc)])
                        A(lambda e, tsl=tsl: e.activation(sqkr[0:64], kr_f[0:64, tsl], AF.Square), ["krf"], ["sqkr"])
                        for j in range(4):
                            T(lambda e, tc=tc, j=j: e.matmul(ps[bS][:, tc * 4 + j:tc * 4 + j + 1], sqkr[0:64, j * 128:(j + 1) * 128], onesb[0:64, 0:1],
                                                           start=True, stop=True), ["sqkr", "onesb"], [pk(bS)])
                    V(lambda e: e.tensor_copy(sskr, ps[bS][:, 0:16]), [pk(bS)], ["sskr"])
                    for h in range(NH):
                        bS = nb()
                        for tc in range(4):
                            tsl = slice(tc * 512, (tc + 1) * 512)
                            b = nb()
                            T(lambda e, b=b, h=h, tsl=tsl: e.matmul(ps[b][:, :], w_ukv_b[:, h * 256:h * 256 + 128], ckvn_b[:, tsl], start=True, stop=True),
                              ["ckvn", "w_ukv"], [pk(b)])
                            A(lambda e, b=b, h=h, tsl=tsl: e.activation(KnT[:, h, tsl], ps[b][:, :], AF.Copy, scale=vecs[:, 22:23]), [pk(b), "vecs"], [("KnT", h)])
                            sq = sqk[tc % 2]
                            A(lambda e, b=b, sq=sq: e.activation(sq, ps[b][:, :], AF.Square), [pk(b)], ["sqk%d" % (tc % 2)])
                            for j in range(4):
                                T(lambda e, tc=tc, j=j, sq=sq, bS=bS: e.matmul(ps[bS][:, tc * 4 + j:tc * 4 + j + 1], sq[:, j * 128:(j + 1) * 128], onesb[:, 0:1],
                                                                               start=True, stop=True), ["sqk%d" % (tc % 2), "onesb"], [pk(bS)])
                        V(lambda e, bS=bS: e.tensor_tensor(ssk, ps[bS][:, 0:16], sskr, ALU.add), [pk(bS), "sskr"], ["ssk"])
                        A(lambda e: e.activation(ssk, ssk, AF.Sqrt, bias=epsc, scale=1.0 / 192), ["ssk", "ccols"], ["ssk"])
                        V(lambda e: e.reciprocal(ssk, ssk), ["ssk"], ["ssk"])
                        V(lambda e, h=h: e.tensor_scalar(scl[:, h * 16:(h + 1) * 16], ssk, 1.0 / math.sqrt(192.0), None, ALU.mult), ["ssk"], [("scl", h)])
                    tr.barrier()
                    chk("kprep")
                    RQ = Region(QT0)
                    catA = RQ.a([128, 4, 512], BF16)
                    QnT = [RQ.a([128, 512], BF16) for _ in range(2)]
                    QrT = [RQ.a([128, 512], BF16) for _ in range(2)]
                    PT = [RQ.a([128, 512], BF16) for _ in range(4)]
                    sqn = RQ.a([128, 512], BF16)
                    sqr = RQ.a([128, 512], BF16)
                    rD = RQ.a([128, 512], F32)
                    assert RQ.off <= YP0
                    RK = Region(KT0)
                    rq = RK.a([128, 512], F32)
                    qt1 = RK.a([128, 512], F32)
                    qt2 = RK.a([128, 512], F32)
                    assert RK.off <= TOTW
                    for qq in range(2):
                        V(lambda e, qq=qq: e.memset(QrT[qq][64:128, :], 0.0), [], ["QrT%d" % qq])
                    SB = (0, 1, 2)
                    QB = (3, 4, 5)
                    bO, bD = 6, 7
                    pt_state = {"n": 0}

                    def q_prep(qc, h, qb):
                        qsl = slice(qc * 512, (qc + 1) * 512)
                        bqn, bqr, bqs = QB
                        for k in range(2):
                            T(lambda e, k=k: e.matmul(ps[bqn][:, :], w_uq_b[:, k, h * 192:h * 192 + 128], cqn_b[:, k, qsl], start=(k == 0), stop=(k == 1)),
                              ["w_uq", "cqn"], [pk(bqn)])
                        for k in range(2):
                            T(lambda e, k=k: e.matmul(ps[bqr][0:64, :], w_uq_b[:, k, h * 192 + 128:h * 192 + 192], cqn_b[:, k, qsl], start=(k == 0), stop=(k == 1)),
                              ["w_uq", "cqn"], [pk(bqr)])
                        for k in range(2):
                            T(lambda e, k=k: e.matmul(ps[bqs][0:64, :], w_uqs[:, k, h * 64:(h + 1) * 64], cqn_b[:, k, qsl], start=(k == 0), stop=(k == 1)),
                              ["w_uqs", "cqn"], [pk(bqs)])
                        A(lambda e: e.activation(sqn, ps[bqn][:, :], AF.Square), [pk(bqn)], ["sqn"])
                        A(lambda e: e.activation(sqr[0:64], ps[bqr][0:64, :], AF.Square), [pk(bqr)], ["sqr"])
                        bss = nb(SB)
                        T(lambda e: e.matmul(ps[bss][:, :], onesb, sqn, start=True, stop=False), ["sqn", "onesb"], [pk(bss)])
                        T(lambda e: e.matmul(ps[bss][:, :], onesb[0:64, :], sqr[0:64], start=False, stop=True), ["sqr", "onesb"], [pk(bss)])
                        rstd_from(bss, rq, 192.0, "rq")
                        V(lambda e: e.scalar_tensor_tensor(QnT[qb], ps[bqn][:, :], vecs[:, 19:20], rq, ALU.mult, ALU.mult), [pk(bqn), "vecs", "rq"], ["QnT%d" % qb])
                        V(lambda e: e.scalar_tensor_tensor(qt1[0:64], ps[bqr][0:64, :], vecs[0:64, 20:21], cos2[0:64, qsl], ALU.mult, ALU.mult),
                          [pk(bqr), "vecs", "cos2"], ["qt1"])
                        V(lambda e: e.scalar_tensor_tensor(qt2[0:64], ps[bqs][0:64, :], vecs[0:64, 21:22], sin2[0:64, qsl], ALU.mult, ALU.mult),
                          [pk(bqs), "vecs", "sin2"], ["qt2"])
                        V(lambda e: e.tensor_tensor(qt1[0:64], qt1[0:64], qt2[0:64], ALU.add), ["qt1", "qt2"], ["qt1"])
                        V(lambda e: e.tensor_tensor(QrT[qb][0:64], qt1[0:64], rq[0:64], ALU.mult), ["qt1", "rq"], ["QrT%d" % qb])

                    def core(qc, h, qb, hook):
                        def S_mm(kt):
                            b = nb(SB)
                            ksl = slice(kt * 128, (kt + 1) * 128)
                            T(lambda e: e.matmul(ps[b][:, :], KnT[:, h, ksl], QnT[qb], start=True, stop=False), [("KnT", h), "QnT%d" % qb], [pk(b)])
                            T(lambda e: e.matmul(ps[b][:, :], KrT[:, ksl], QrT[qb], start=False, stop=True), ["KrT", "QrT%d" % qb], [pk(b)])
                            return b
                        sb_cur = S_mm(0)
                        for kt in range(16):
                            sb_next = S_mm(kt + 1) if kt < 15 else None
                            pi = pt_state["n"] % 4
                            pt_state["n"] += 1
                            p_t = PT[pi]
                            A(lambda e: e.activation(p_t, ps[sb_cur][:, :], AF.Exp, scale=scl[:, h * 16 + kt:h * 16 + kt + 1]),
                              [pk(sb_cur), ("scl", h)], ["PT%d" % pi])
                            T(lambda e: e.matmul(ps[bO][:, :], Vb[:, kt, h * 128:(h + 1) * 128], p_t, start=(kt == 0), stop=(kt == 15)),
                              ["PT%d" % pi, ("Vb", kt)], [pk(bO)])
                            T(lambda e: e.matmul(ps[bD][:, :], onesb, p_t, start=(kt == 0), stop=(kt == 15)),
                              ["PT%d" % pi, "onesb"], [pk(bD)])
                            sb_cur = sb_next
                            if kt == 3:
                                hook()
                        V(lambda e: e.reciprocal(rD, ps[bD][:, :]), [pk(bD)], ["rD"])
                        V(lambda e: e.tensor_tensor(catA[:, h, :], ps[bO][:, :], rD, ALU.mult), [pk(bO), "rD"], [("catA", h)])

                    pairs = [(qc, h) for qc in range(4) for h in range(NH)]
                    q_prep(0, 0, 0)
                    for i, (qc, h) in enumerate(pairs):
                        qsl = slice(qc * 512, (qc + 1) * 512)
                        if i + 1 < len(pairs):
                            nqc, nh = pairs[i + 1]
                            hook = (lambda nqc=nqc, nh=nh, i=i: q_prep(nqc, nh, (i + 1) % 2))
                        else:
                            hook = (lambda: None)
                        core(qc, h, i % 2, hook)
                        if h == NH - 1:
                            for m in range(8):
                                b = nb(SB)
                                for k in range(8):
                                    rhs = catA[:, k, :] if k < 4 else catC[:, k - 4, qsl]
                                    rk = ("catA", k) if k < 4 else ("catC", qc)
                                    T(lambda e, b=b, k=k, m=m, rhs=rhs: e.matmul(ps[b][:, :], w_out_b[:, k, m * 128:(m + 1) * 128], rhs, start=(k == 0), stop=(k == 7)),
                                      ["w_out", rk], [pk(b)])
                                V(lambda e, b=b, m=m: e.scalar_tensor_tensor(xT[:, m, qsl], ps[b][:, :], mc[:, 16 + m:17 + m], xT[:, m, qsl], ALU.mult, ALU.add),
                                  [pk(b), "mc", ("xT", (m, qc))], [("xT", (m, qc))])

                    chk("T3")
                    tr.barrier()
                    M = Region(PH0)
                    zer = M.a([128, 1024], F32)
                    h2st = [M.a([128, 1024], BF16) for _ in range(4)]
                    M1 = M
                    sq_b = M1.a([128, 8, 512], BF16)
                    h2Ts = [M1.a([128, 8, 512], BF16) for _ in range(2)]
                    xn = [M1.a([128, 512], F32) for _ in range(2)]
                    rstd2s = [M1.a([128, 512], F32) for _ in range(2)]
                    affT = M1.a([128, S_LEN], F32)
                    wr_f = M1.a([128, 8, NE], F32)
                    awr = M1.a([128, 8, NE], F32)
                    lg = M1.a([128, 512], F32)
                    rden = M1.a([128, 512], F32)
                    cstc = M1.a([128, 1], F32)
                    m8 = M1.a([128, 8], F32)
                    masktm_b = M1.a([128, 256], BF16)
                    masktm_f = M1.a([128, 256], F32)
                    afftm = M1.a([128, 256], F32)
                    hi_f = M1.a([128, 256], F32)
                    maskT = M1.a([128, S_LEN], BF16)
                    ET = M1.a([128, S_LEN], F32)
                    work = ET
                    mod_slab = [M1.a([128, 8, 512], F32) for _ in range(2)]

                    tr.op(SP, lambda e, l=l: e.dma_start(out=wr_f, in_=wr_d[l].rearrange("p (c n) -> p c n", c=8)), writes=["wr_f"], dma=s_misc)
                    V(lambda e: e.memset(zer, 0.0), [], ["zer"])
                    for c in range(8):
                        V(lambda e, c=c: e.tensor_scalar(awr[:, c, :], wr_f[:, c, :], mc[:, 24 + c:25 + c], None, ALU.mult), ["wr_f", "mc"], ["awr"])
                    b = nb()
                    for c in range(8):
                        T(lambda e, c=c, b=b: e.matmul(ps[b][0:16, 0:1], wr_f[:, c, :], mc[:, 32 + c:33 + c], start=(c == 0), stop=(c == 7)), ["wr_f", "mc"], [pk(b)])
                    V(lambda e, b=b: e.tensor_copy(cstc[0:16], ps[b][0:16, 0:1]), [pk(b)], ["cstc"])
                    h2v = h2_dram[s].rearrange("(j p) d -> p j d", p=128)
                    rms_chunk(0, mc[:, 24:32], mc[:, 32:40], sq_b, h2Ts[0], xn, rstd2s[0], "m1p0", hkey="hTm0")
                    for tc in range(4):
                        tsl = slice(tc * 512, (tc + 1) * 512)
                        h2T = h2Ts[tc % 2]
                        rstd2 = rstd2s[tc % 2]
                        hkm = "hTm%d" % (tc % 2)
                        if tc + 1 < 4:
                            rms_a(tc + 1, sq_b)
                        for j4 in range(4):
                            jt = tc * 4 + j4
                            hst = h2st[jt % 4]
                            for hf in range(2):
                                b = nb()
                                for c4 in range(4):
                                    c = hf * 4 + c4
                                    T(lambda e, b=b, c=c, c4=c4, j4=j4: e.matmul(ps[b][:, c4 * 128:(c4 + 1) * 128], h2T[:, c, j4 * 128:(j4 + 1) * 128], identb,
                                                                              start=True, stop=True), [(hkm, c), "identb"], [pk(b)])
                                dst = hst[:, hf * 512:(hf + 1) * 512]
                                if (j4 + hf) % 2 == 0:
                                    A(lambda e, b=b, dst=dst: e.activation(dst, ps[b][:, :], AF.Copy), [pk(b)], [("h2st", jt % 4)])
                                else:
                                    V(lambda e, b=b, dst=dst: e.tensor_copy(dst, ps[b][:, :]), [pk(b)], [("h2st", jt % 4)])
                            tr.op(SP, lambda e, jt=jt, hst=hst: e.dma_start(out=h2v[:, jt, :], in_=hst), reads=[("h2st", jt % 4)], writes=["h2d%d" % s], dma=s_h2)
                        b = nb()
                        for c in range(8):
                            T(lambda e, b=b, c=c, tsl=tsl: e.matmul(ps[b][0:16, :], awr[:, c, :], xT[:, c, tsl], start=(c == 0), stop=(c == 7)),
                              ["awr", ("xT", (c, tc))], [pk(b)])
                        if tc + 1 < 4:
                            rms_chunk(tc + 1, mc[:, 24:32], mc[:, 32:40], sq_b, h2Ts[(tc + 1) % 2], xn, rstd2s[(tc + 1) % 2], "m1p%d" % ((tc + 1) % 2),
                                      hkey="hTm%d" % ((tc + 1) % 2), do_a=False)
                        V(lambda e, b=b: e.tensor_tensor(lg[0:16], ps[b][0:16, :], rstd2[0:16], ALU.mult), [pk(b), "m1p%drstd" % (tc % 2)], ["lg"])
                        A(lambda e, tsl=tsl: e.activation(ET[0:16, tsl], lg[0:16], AF.Exp, bias=cstc[0:16], scale=1.0), ["lg", "cstc"], [("ET", tc)])
                        b = nb()
                        T(lambda e, b=b, tsl=tsl: e.matmul(ps[b][0:16, :], onesf[0:16, 0:16], ET[0:16, tsl], start=True, stop=True), [("ET", tc), "onesf"], [pk(b)])
                        V(lambda e, b=b: e.reciprocal(rden[0:16], ps[b][0:16, :]), [pk(b)], ["rden"])
                        V(lambda e, tsl=tsl: e.tensor_tensor(affT[0:16, tsl], ET[0:16, tsl], rden[0:16], ALU.mult), [("ET", tc), "rden"], [("affT", tc)])
                        if l + 1 < L:
                            per = (12 + n_seq - 1) // n_seq
                            mine = list(range(s * per, min(12, (s + 1) * per)))
                            q4 = (len(mine) + 3) // 4
                            emit_mod(l + 1, mod_slab, mine[tc * q4:(tc + 1) * q4])
                    for c in range(8):
                        tr.op(SP, lambda e, s=s, c=c: e.dma_start(out=xs_dram[s, c * 128:(c + 1) * 128, :], in_=xT[:, c, :]),
                              reads=[("xT", (c, t)) for t in range(4)], writes=["xs%d" % s], dma=s_sp)
                    if s + 1 < n_seq:
                        load_x(l, s + 1)
                    for ct in range(2):
                        accv = acc_dram[s][ct].rearrange("(j p) d -> p j d", p=128)
                        for j in range(16):
                            tr.op(SP, lambda e, j=j, accv=accv: e.dma_start(out=accv[:, j, :], in_=zer), reads=["zer"], writes=["accd%d%d" % (s, ct)], dma=s_zero)
                    ba = nb()
                    for j in range(16):
                        jsl = slice(j * 128, (j + 1) * 128)
                        T(lambda e, j=j, jsl=jsl: e.matmul(ps[ba][:, j * 16:(j + 1) * 16], affT[0:16, jsl], identf[0:16, 0:16], start=True, stop=True),
                          ["affT", "identf"], [pk(ba)])
                    V(lambda e: e.tensor_copy(afftm, ps[ba][:, 0:256]), [pk(ba)], ["afftm"])
                    lo16 = M1.a([128, 16], F32)
                    mid16 = M1.a([128, 16], F32)
                    cnt16 = M1.a([128, 16], F32)
                    ge16 = M1.a([128, 16], F32)
                    a3 = afftm.rearrange("p (j e) -> p j e", e=16)
                    m3 = masktm_b.rearrange("p (j e) -> p j e", e=16)
                    V(lambda e: e.memset(lo16, 0.0), [], ["lo16"])
                    NBIS = 30
                    for k in range(NBIS):
                        wk = 2.0 ** (-(k + 1))
                        V(lambda e, wk=wk: e.tensor_scalar(mid16, lo16, wk, None, ALU.add), ["lo16"], ["mid16"])
                        midb = mid16.rearrange("p (o e) -> p o e", o=1).to_broadcast([128, 16, 16])
                        V(lambda e, midb=midb: e.tensor_tensor(m3, a3, midb, ALU.is_ge), ["afftm", "mid16"], ["masktm_b"])
                        bc = nb()
                        T(lambda e, bc=bc: e.matmul(ps[bc][:, 0:256], onesb, masktm_b, start=True, stop=True), ["masktm_b", "onesb"], [pk(bc)])
                        pv = ps[bc][:, 0:256].rearrange("p (j e) -> p e j", e=16)
                        V(lambda e, pv=pv: e.tensor_reduce(out=cnt16, in_=pv, axis=mybir.AxisListType.X, op=ALU.add), [pk(bc)], ["cnt16"])
                        V(lambda e, wk=wk: e.tensor_scalar(ge16, cnt16, float(CAP) - 0.5, wk, ALU.is_ge, ALU.mult), ["cnt16"], ["ge16"])
                        V(lambda e: e.tensor_tensor(lo16, lo16, ge16, ALU.add), ["lo16", "ge16"], ["lo16"])
                    lob = lo16.rearrange("p (o e) -> p o e", o=1).to_broadcast([128, 16, 16])
                    mf3 = masktm_f.rearrange("p (j e) -> p j e", e=16)
                    V(lambda e: e.tensor_tensor(mf3, a3, lob, ALU.is_ge), ["afftm", "lo16"], ["masktm_f"])
                    V(lambda e: e.tensor_copy(masktm_b, masktm_f), ["masktm_f"], ["masktm_b"])
                    V(lambda e: e.tensor_copy(G4[:, :, 0], afftm), ["afftm"], ["G4%d" % s])
                    V(lambda e: e.tensor_copy(hi_f, G4[:, :, 0]), ["G4%d" % s], ["hi_f"])
                    V(lambda e: e.tensor_tensor(G4[:, :, 1], afftm, hi_f, ALU.subtract), ["afftm", "hi_f"], ["G4%d" % s])
                    V(lambda e: e.tensor_scalar(G4[:, :, 2], jrow, 0.0, ccols[:, 5:6], ALU.mult, ALU.add), ["jrow", "ccols"], ["G4%d" % s])
                    V(lambda e: e.tensor_copy(G4[:, :, 3], jrow), ["jrow"], ["G4%d" % s])
                    bp = nb()
                    for j in range(16):
                        for i in range(j):
                            T(lambda e, i=i, j=j: e.matmul(ps[bp][:, j * 16:(j + 1) * 16], onesb, masktm_b[:, i * 16:(i + 1) * 16], start=(i == 0), stop=False),
                              ["masktm_b", "onesb"], [pk(bp)])
                        T(lambda e, j=j: e.matmul(ps[bp][:, j * 16:(j + 1) * 16], BIG[:, 384:512], masktm_b[:, j * 16:(j + 1) * 16], start=(j == 0), stop=True),
                          ["masktm_b", "BIG"], [pk(bp)])
                    V(lambda e: e.scalar_tensor_tensor(pos1tm, ps[bp][:, 0:256], 1.0, masktm_f, ALU.add, ALU.mult), [pk(bp), "masktm_f"], ["pos1tm%d" % s])

                    chk("M1")

                tr.barrier()
                NSL = 256 * n_seq
                EA = Region(0)
                yef = [[EA.a([128, 1024], F32) for _ in range(2 * n_seq)] for _ in range(2)]
                xe_tm = [[EA.a([128, 2, 1024], BF16) for _ in range(n_seq)] for _ in range(2)]
                xeT = [EA.a([128, 8, NSL], BF16) for _ in range(2)]
                assert EA.off <= 16384, EA.off
                EB = Region(PH0)
                NSLOT = 12
                wsl = [EB.a([128, 4, 1024], BF16) for _ in range(NSLOT)]
                Sel = [EB.a([128, 16, 256], BF16) for _ in range(n_seq)]
                hidT = EB.a([128, 8, NSL], BF16)
                sgt = [EB.a([128, NSL], F32) for _ in range(2)]
                gcol = [[EB.a([128, 2], F32) for _ in range(n_seq)] for _ in range(3)]
                idxi = [[EB.a([128, 2], I32) for _ in range(n_seq)] for _ in range(3)]
                gtmp = EB.a([128, 8], F32)
                idxf = EB.a([128, 2], F32)

                wl = []
                for ex in range(NE):
                    wl += [(wg_d, ex, 0), (wg_d, ex, 1), (wu_d, ex, 0), (wu_d, ex, 1), (wd_d, ex, 0), (wd_d, ex, 1)]
                wstate = {"n": 0}

                def issue_w(upto):
                    while wstate["n"] < min(upto, len(wl)):
                        k = wstate["n"]
                        src, ex, hf = wl[k]
                        sl = k % NSLOT
                        sv = src[l, ex].rearrange("(c p) f -> p c f", p=128)[:, hf * 4:(hf + 1) * 4, :]
                        tr.op(POOL, lambda e, sl=sl, sv=sv: e.dma_start(out=wsl[sl], in_=sv), writes=["wsl%d" % sl], dma=s_w[sl])
                        wstate["n"] += 1

                issue_w(NSLOT - 1)
                wix = {"i": 0}

                def prep_sel(ex):
                    for s in range(n_seq):
                        for j in range(16):
                            V(lambda e, j=j, ex=ex, s=s: e.tensor_scalar(Sel[s][:, j, :], iota1, pos1tm_s[s][:, j * 16 + ex:j * 16 + ex + 1], None, ALU.is_equal),
                              ["iota1", "pos1tm%d" % s], [("Sel%d" % s, j)])

                def prep_idx(ex):
                    pb = ex % 3
                    for s in range(n_seq):
                        b = nb()
                        for ct in range(2):
                            for j in range(16):
                                T(lambda e, b=b, ct=ct, j=j, ex=ex, s=s: e.matmul(ps[b][:, ct * 4:ct * 4 + 4], Sel[s][:, j, ct * 128:(ct + 1) * 128], G4_s[s][:, j * 16 + ex, :],
                                                                                 start=(j == 0), stop=(j == 15)), [("Sel%d" % s, j), "G4%d" % s], [pk(b)])
                        V(lambda e, b=b: e.tensor_copy(gtmp, ps[b][:, 0:8]), [pk(b)], ["gtmp"])
                        g3 = gtmp.rearrange("p (a b) -> p a b", b=4)
                        V(lambda e, g3=g3, pb=pb, s=s: e.tensor_tensor(gcol[pb][s], g3[:, :, 0], g3[:, :, 1], ALU.add), ["gtmp"], ["gcol%d%d" % (pb, s)])
                        V(lambda e, g3=g3: e.scalar_tensor_tensor(idxf, g3[:, :, 3], 128.0, g3[:, :, 2], ALU.mult, ALU.add), ["gtmp"], ["idxf"])
                        V(lambda e, pb=pb, s=s: e.tensor_copy(idxi[pb][s], idxf), ["idxf"], ["idxi%d%d" % (pb, s)])

                def gather(ex):
                    pb, p3 = ex % 2, ex % 3
                    for s in range(n_seq):
                        for ct in range(2):
                            tr.op(POOL, lambda e, ct=ct, pb=pb, p3=p3, s=s: e.indirect_dma_start(
                                out=xe_tm[pb][s][:, ct, :], out_offset=None, in_=h2_dram[s],
                                in_offset=bass.IndirectOffsetOnAxis(ap=idxi[p3][s][:, ct:ct + 1], axis=0)),
                                reads=["h2d%d" % s, "idxi%d%d" % (p3, s)], writes=[("xe_tm%d%d" % (pb, s), ct)], dma=s_g[pb])

                def prep_tr(ex):
                    pb = ex % 2
                    for s in range(n_seq):
                        for cp in range(4):
                            b = nb()
                            for c in (2 * cp, 2 * cp + 1):
                                for ct in range(2):
                                    T(lambda e, b=b, c=c, ct=ct, pb=pb, s=s: e.matmul(ps[b][:, (c % 2) * 256 + ct * 128:(c % 2) * 256 + (ct + 1) * 128],
                                                                                   xe_tm[pb][s][:, ct, c * 128:(c + 1) * 128], identb, start=True, stop=True),
                                      [("xe_tm%d%d" % (pb, s), ct), "identb"], [pk(b)])
                            dst = xeT[pb][:, 2 * cp:2 * cp + 2, s * 256:(s + 1) * 256]
                            if cp % 2 == 0:
                                A(lambda e, b=b, dst=dst: e.activation(dst, ps[b][:, :].rearrange("p (a b) -> p a b", a=2), AF.Copy), [pk(b)], [("xeT%d" % pb, cp)])
                            else:
                                V(lambda e, b=b, dst=dst: e.tensor_copy(dst, ps[b][:, :].rearrange("p (a b) -> p a b", a=2)), [pk(b)], [("xeT%d" % pb, cp)])

                def ffn_gu(ex):
                    pb = ex % 2
                    widx = wix["i"]
                    issue_w(widx + NSLOT)
                    for fc in range(8):
                        bg = nb()
                        bu = nb()
                        for (w0, b) in ((widx, bg), (widx + 2, bu)):
                            for c in range(8):
                                wsel = (w0 + c // 4) % NSLOT
                                T(lambda e, b=b, wsel=wsel, c=c, fc=fc, pb=pb: e.matmul(ps[b][:, 0:NSL], wsl[wsel][:, c % 4, fc * 128:(fc + 1) * 128], xeT[pb][:, c, :],
                                                                                     start=(c == 0), stop=(c == 7)), ["wsl%d" % wsel, ("xeT%d" % pb, c // 2)], [pk(b)])
                        st_ = sgt[fc % 2]
                        A(lambda e, bg=bg, st_=st_: e.activation(st_, ps[bg][:, 0:NSL], AF.Silu), [pk(bg)], ["sgt%d" % (fc % 2)])
                        V(lambda e, bu=bu, st_=st_, fc=fc: e.tensor_tensor(hidT[:, fc, :], ps[bu][:, 0:NSL], st_, ALU.mult), [pk(bu), "sgt%d" % (fc % 2)], [("hidT", fc)])
                    wix["i"] += 4

                def ffn_down(ex):
                    pb = ex % 2
                    p3 = ex % 3
                    widx = wix["i"]
                    issue_w(widx + NSLOT)
                    for nd in range(2):
                        for q in range(2 * n_seq):
                            s, ct = q // 2, q % 2
                            b = nb()
                            for fc in range(8):
                                sd_ = (widx + fc // 4) % NSLOT
                                T(lambda e, b=b, fc=fc, q=q, sd_=sd_, nd=nd: e.matmul(ps[b][:, :], hidT[:, fc, q * 128:(q + 1) * 128], wsl[sd_][:, fc % 4, nd * 512:(nd + 1) * 512],
                                                                                   start=(fc == 0), stop=(fc == 7)),
                                  [("hidT", fc), "wsl%d" % sd_], [pk(b)])
                            A(lambda e, b=b, pb=pb, p3=p3, q=q, nd=nd, s=s, ct=ct: e.activation(yef[pb][q][:, nd * 512:(nd + 1) * 512], ps[b][:, :], AF.Copy, scale=gcol[p3][s][:, ct:ct + 1]),
                              [pk(b), "gcol%d%d" % (p3, s)], ["yef%d%d" % (pb, q)])
                    wix["i"] += 2
                    for q in range(2 * n_seq):
                        s, ct = q // 2, q % 2
                        tr.op(POOL, lambda e, ct=ct, pb=pb, p3=p3, s=s, q=q: e.indirect_dma_start(
                            out=acc_dram[s][ct], out_offset=bass.IndirectOffsetOnAxis(ap=idxi[p3][s][:, ct:ct + 1], axis=0),
                            in_=yef[pb][q], in_offset=None, bounds_check="BCREG", oob_is_err=True, compute_op=ALU.add),
                            reads=["yef%d%d" % (pb, q), "idxi%d%d" % (p3, s), "accd%d%d" % (s, ct)], writes=["accd%d%d" % (s, ct)], dma=s_acc[s][ct])

                prep_sel(0)
                prep_idx(0)
                gather(0)
                prep_sel(1)
                prep_idx(1)
                prep_tr(0)
                for ex in range(NE):
                    if ex + 1 < NE:
                        gather(ex + 1)
                    if ex + 2 < NE:
                        prep_sel(ex + 2)
                    ffn_gu(ex)
                    if ex + 2 < NE:
                        prep_idx(ex + 2)
                    ffn_down(ex)
                    if ex + 1 < NE:
                        prep_tr(ex + 1)
                tr.barrier()
                EC = Region(0)
                xch = [EC.a([128, 8, 512], F32) for _ in range(2)]
                accb = [[EC.a([128, 1024], F32) for _ in range(2)] for _ in range(4)]
                assert EC.off <= 16384
                kx = 0
                for s in range(n_seq):
                    accv = [acc_dram[s][ct].rearrange("(j p) d -> p j d", p=128) for ct in range(2)]
                    xsv = xs_dram[s].rearrange("(c p) t -> p c t", p=128)
                    dstv = (out_d[s] if l == L - 1 else xs_dram[s]).rearrange("(c p) t -> p c t", p=128)
                    for tc in range(4):
                        xc = xch[kx % 2]
                        xk = "xch%d" % (kx % 2)
                        tr.op(SP, lambda e, xc=xc, xsv=xsv, tc=tc: e.dma_start(out=xc, in_=xsv[:, :, tc * 512:(tc + 1) * 512]),
                              reads=[("xs%d" % s, tc)], writes=[xk], dma=s_xc[kx % 2])
                        for j4 in range(4):
                            j = tc * 4 + j4
                            ab = accb[j % 4]
                            for ct in range(2):
                                tr.op(SP if ct == 0 else ACT, lambda e, j=j, ab=ab, accv=accv, ct=ct: e.dma_start(out=ab[ct], in_=accv[ct][:, j, :]), reads=["accd%d%d" % (s, ct)],
                                      writes=["accb%d_%d" % (j % 4, ct)], dma=s_ab2[j % 4][ct])
                            V(lambda e, ab=ab: e.tensor_tensor(ab[0], ab[0], ab[1], ALU.add), ["accb%d_0" % (j % 4), "accb%d_1" % (j % 4)], ["accb%d_0" % (j % 4)])
                            for hf in range(2):
                                b = nb()
                                for c4 in range(4):
                                    c = hf * 4 + c4
                                    T(lambda e, b=b, c=c, c4=c4, ab=ab: e.matmul(ps[b][:, c4 * 128:(c4 + 1) * 128], ab[0][:, c * 128:(c + 1) * 128], identf, start=True, stop=True),
                                      ["accb%d_0" % (j % 4), "identf"], [pk(b)])
                                for c4 in range(4):
                                    c = hf * 4 + c4
                                    V(lambda e, b=b, c=c, c4=c4, j4=j4, xc=xc, s=s: e.scalar_tensor_tensor(xc[:, c, j4 * 128:(j4 + 1) * 128], ps[b][:, c4 * 128:(c4 + 1) * 128], mcs[s][:, 40 + c:41 + c],
                                                                                                      xc[:, c, j4 * 128:(j4 + 1) * 128], ALU.mult, ALU.add),
                                      [pk(b), "mc", xk], [xk])
                        tr.op(POOL, lambda e, xc=xc, dstv=dstv, tc=tc: e.dma_start(out=dstv[:, :, tc * 512:(tc + 1) * 512], in_=xc),
                              reads=[xk], writes=[("xs%d" % s, tc)], dma=s_out)
                        kx += 1
        try:
            main_body()
        except _Stop:
            pass
        tr.emit(nc)
    return nc


def _consts():
    c = np.zeros((128, NCST), np.float32)
    c[:, 0:128] = np.eye(128, dtype=np.float32)
    tp = np.arange(128)
    c[:, 128:256] = (tp[:, None] < tp[None, :]).astype(np.float32)
    c[:, 256:512] = np.arange(1, 257, dtype=np.float32)[None, :]
    c[:, 512] = tp + 1
    c[:, 513] = tp + 129
    inv_freq = (1.0 / (np.float32(10000.0) ** (np.arange(0, 64, 2, dtype=np.float32) / np.float32(64)))).astype(np.float32)
    c[0:64, 514] = np.concatenate([inv_freq, inv_freq])
    c[0:32, 515] = -1.0
    c[32:64, 515] = 1.0
    c[:, 516] = EPS
    c[:, 517] = tp
    c[:, 520:776] = (np.arange(256) // 16).astype(np.float32)[None, :]
    return c


def _col(v, nch):
    return np.ascontiguousarray(np.asarray(v, np.float32).reshape(nch, 128).T)


def _pack_layer_inputs(inp, L):
    vec = np.zeros((L, 128, NV), np.float32)
    for l in range(L):
        v = vec[l]
        v[:, 0:8] = _col(inp["norm1_g"][l], 8)
        v[:, 8:16] = _col(inp["norm2_g"][l], 8)
        v[:, 16:18] = _col(inp["q_latent_g"][l], 2)
        v[:, 18:19] = _col(inp["kv_latent_g"][l], 1)
        qg = np.asarray(inp["q_head_g"][l], np.float32)
        kg = np.asarray(inp["k_head_g"][l], np.float32)
        v[:, 19] = qg[0:128]
        v[0:64, 20] = qg[128:192]
        v[0:64, 21] = np.concatenate([qg[160:192], qg[128:160]])
        v[:, 22] = kg[0:128]
        v[0:64, 23] = kg[128:192]
        v[0:64, 24] = np.concatenate([kg[160:192], kg[128:160]])
        v[:, 25:29] = _col(inp["conv_b"][l], 4)
        v[:, 29:33] = _col(inp["conv_norm_g"][l], 4)
        v[:, 33:37] = _col(inp["conv_norm_b"][l], 4)
        cw = np.asarray(inp["conv_w"][l], np.float32)
        for cc in range(4):
            v[:, 37 + cc * 31:37 + (cc + 1) * 31] = cw[:, cc * 128:(cc + 1) * 128].T
    bada = np.stack([_col(inp["b_ada"][l], 48) for l in range(L)])
    wr = np.stack([np.ascontiguousarray(np.asarray(inp["w_router"][l], np.float32).reshape(8, 128, NE).transpose(1, 0, 2)).reshape(128, 8 * NE)
                   for l in range(L)])
    return vec, bada, wr


def make_in_maps(inp, n_cores, n_seq, L, batch_ids=None):
    f32 = lambda a: np.ascontiguousarray(np.asarray(a, np.float32))
    vec, bada, wr = _pack_layer_inputs(inp, L)
    cst = _consts()
    shared = {
        "cst": cst, "vec": vec, "bada": bada, "w_router": wr,
        "w_ada": f32(inp["w_ada"][:L]), "w_in": f32(inp["w_in"][:L]), "w_uq": f32(inp["w_uq"][:L]),
        "w_ukv": f32(inp["w_ukv"][:L]), "w_out": f32(inp["w_out"][:L]),
        "w_gate": f32(inp["w_gate"][:L]), "w_up": f32(inp["w_up"][:L]), "w_down": f32(inp["w_down"][:L]),
    }
    x = np.asarray(inp["x"], np.float32)
    c = np.asarray(inp["c"], np.float32)
    pos = np.asarray(inp["positions"], np.int32)
    maps = []
    for core in range(n_cores):
        ids = batch_ids[core] if batch_ids is not None else list(range(core * n_seq, (core + 1) * n_seq))
        xT = np.ascontiguousarray(np.stack([x[b].T for b in ids]))
        cT = np.zeros((128, 8 * n_seq), np.float32)
        for si, b in enumerate(ids):
            cT.reshape(128, 8, n_seq)[:, :, si] = c[b].reshape(8, 128).T
        posr = np.ascontiguousarray(np.stack([np.broadcast_to(pos[b][None, :], (64, S_LEN)) for b in ids])).astype(np.int32)
        m = dict(shared)
        m.update({"xT": xT, "cT": cT, "posr": posr})
        maps.append(m)
    return maps


_NC_CACHE = {}


def kernel(**inputs):
    n_cores, n_seq, L = 8, 2, 2
    key = (n_seq, L)
    if key not in _NC_CACHE:
        _NC_CACHE[key] = build_program(n_seq, L)
    nc = _NC_CACHE[key]
    maps = make_in_maps(inputs, n_cores, n_seq, L)
    res = run_bass_kernel_spmd(nc, maps, core_ids=list(range(n_cores)))
    out = np.empty((n_cores * n_seq, S_LEN, D), np.float32)
    for core in range(n_cores):
        oT = res.results[core]["outT"]
        for si in range(n_seq):
            out[core * n_seq + si] = oT[si].T
    return out
```
